# Optimizing a Trainium2 kernel written in Bass

```python
import math
import jax, jax.numpy as jnp
from jax import lax
import numpy as np

D_MODEL = 2048
BATCH = 1
SEQ = 16384
DEPTH = 1

D_SSM = D_MODEL // 2
D_ATTN = D_MODEL // 2
D_MIX = D_SSM + D_ATTN
SSM_GROUP = 16
N_SSM_GROUPS = D_SSM // SSM_GROUP
SSM_STATE = 64
DT_MIN = 1e-3
DT_MAX = 1e-1

HEAD_DIM = 128
N_HEADS = D_ATTN // HEAD_DIM
N_KV = 2
HPG = N_HEADS // N_KV
CMP_LEN = 32
CMP_STRIDE = 16
CMP_HIDDEN = 256
SLC_LEN = 64
SLC_TOPK = 16
WINDOW = 512
Q_BLOCK = 128
N_BRANCH = 3

N_BUCKETS = 32
MAX_DISTANCE = 1024

PEER_HEADS = 8
PEER_NKEYS = 128
PEER_EXPERTS = PEER_NKEYS * PEER_NKEYS
PEER_DKEY = 256
PEER_TOPK = 16
PEER_CHUNK = 128

KV_COLS = N_BRANCH * 2 * N_KV * HEAD_DIM
GATE_COLS = N_BRANCH * N_HEADS
D_IN = D_SSM + D_ATTN + KV_COLS + GATE_COLS
N_ADA = 6
EPS = 1e-6
NEG = -1e30
FORCED = 1e6

kernel_name = 'hybrid_s5_nsa_peer_block'


def rmsnorm(x, g):
    xf = x.astype(jnp.float32)
    return xf * lax.rsqrt(jnp.mean(xf * xf, axis=-1, keepdims=True) + EPS) * g


def t5_bucket(dist):
    n = jnp.maximum(dist, 0)
    max_exact = N_BUCKETS // 2
    scaled = jnp.log(jnp.maximum(n, max_exact).astype(jnp.float32) / max_exact) / math.log(MAX_DISTANCE / max_exact)
    large = max_exact + (scaled * (N_BUCKETS - max_exact)).astype(jnp.int32)
    return jnp.where(n < max_exact, n, jnp.minimum(large, N_BUCKETS - 1))


def s5_mixer(u, lam_re, lam_im, log_dt, b_re, b_im, c_re, c_im, d_skip, w_glu, b_glu):
    B, L, _ = u.shape
    f32 = jnp.float32
    uf = u.astype(f32).reshape(B, L, N_SSM_GROUPS, SSM_GROUP)
    dt = jnp.exp(log_dt.astype(f32))[:, None]
    lr = lam_re.astype(f32)
    li = lam_im.astype(f32)
    mag = jnp.exp(lr * dt)
    ab_re = mag * jnp.cos(li * dt)
    ab_im = mag * jnp.sin(li * dt)
    den = lr * lr + li * li
    nr = ab_re - 1.0
    ni = ab_im
    w_re = (nr * lr + ni * li) / den
    w_im = (ni * lr - nr * li) / den
    br = b_re.astype(f32)
    bi = b_im.astype(f32)
    bb_re = w_re[..., None] * br - w_im[..., None] * bi
    bb_im = w_re[..., None] * bi + w_im[..., None] * br
    xr = jnp.einsum('gph,blgh->blgp', bb_re, uf)
    xi = jnp.einsum('gph,blgh->blgp', bb_im, uf)
    ar = jnp.broadcast_to(ab_re, xr.shape)
    ai = jnp.broadcast_to(ab_im, xi.shape)

    def combine(e1, e2):
        a1r, a1i, b1r, b1i = e1
        a2r, a2i, b2r, b2i = e2
        return (a1r * a2r - a1i * a2i, a1r * a2i + a1i * a2r,
                a2r * b1r - a2i * b1i + b2r, a2r * b1i + a2i * b1r + b2i)

    _, _, sr, si = lax.associative_scan(combine, (ar, ai, xr, xi), axis=1)
    y = (jnp.einsum('ghp,blgp->blgh', c_re.astype(f32), sr)
         - jnp.einsum('ghp,blgp->blgh', c_im.astype(f32), si)
         + d_skip.astype(f32) * uf)
    z = jax.nn.gelu(y.reshape(B, L, D_SSM))
    return z * jax.nn.sigmoid(z @ w_glu + b_glu)


def compress(kv, pe, w1, w2):
    B, L, G, dh = kv.shape
    ch = kv.reshape(B, L // CMP_STRIDE, CMP_STRIDE, G, dh)
    blocks = jnp.concatenate([ch[:, :-1], ch[:, 1:]], axis=2)
    blocks = blocks + pe[None, None, :, None, :]
    n_cmp = blocks.shape[1]
    flat = blocks.transpose(0, 1, 3, 2, 4).reshape(B, n_cmp, G, CMP_LEN * dh)
    return jax.nn.gelu(flat @ w1) @ w2


def nsa_attention(q, k_cmp, v_cmp, k_slc, v_slc, k_win, v_win, gates, rel_bias,
                  pe_k, w1_k, w2_k, pe_v, w1_v, w2_v):
    B, L = q.shape[:2]
    f32 = jnp.float32
    scale = HEAD_DIM ** -0.5
    kc = compress(k_cmp, pe_k, w1_k, w2_k)
    vc = compress(v_cmp, pe_v, w1_v, w2_v)
    n_cmp = kc.shape[1]
    n_slc = L // SLC_LEN
    topk = min(SLC_TOPK, n_slc)
    cmp_start = jnp.arange(n_cmp) * CMP_STRIDE
    cmp_end = cmp_start + CMP_LEN - 1
    slc_start = jnp.arange(n_slc) * SLC_LEN
    ov = (jnp.minimum(cmp_start[:, None] + CMP_LEN, slc_start[None, :] + SLC_LEN)
          - jnp.maximum(cmp_start[:, None], slc_start[None, :]))
    overlap = jnp.maximum(ov, 0).astype(f32) / CMP_STRIDE
    ks_blk = k_slc.reshape(B, n_slc, SLC_LEN, N_KV, HEAD_DIM).transpose(0, 3, 1, 2, 4)
    vs_blk = v_slc.reshape(B, n_slc, SLC_LEN, N_KV, HEAD_DIM).transpose(0, 3, 1, 2, 4)
    pad = ((0, 0), (WINDOW, 0), (0, 0), (0, 0))
    kw = jnp.pad(k_win, pad)
    vw = jnp.pad(v_win, pad)
    n_qb = L // Q_BLOCK
    qb = q.reshape(B, n_qb, Q_BLOCK, N_KV, HPG, HEAD_DIM).transpose(1, 0, 2, 3, 4, 5)
    gb = gates.reshape(B, n_qb, Q_BLOCK, N_BRANCH, N_KV, HPG).transpose(1, 0, 2, 3, 4, 5)
    bias_g = rel_bias.reshape(N_BUCKETS, N_KV, HPG)
    bidx = jnp.arange(B)[:, None, None, None]
    gidx = jnp.arange(N_KV)[None, None, :, None]
    gidx5 = jnp.arange(N_KV)[None, None, :, None, None]
    jblk = jnp.arange(n_slc)

    def block_fn(args):
        qi, gi, bi = args
        t = bi * Q_BLOCK + jnp.arange(Q_BLOCK)
        lc = jnp.einsum('bqgpd,bngd->bgpqn', qi, kc).astype(f32) * scale
        dist_c = t[:, None] - cmp_end[None, :]
        valid_c = dist_c >= 0
        lc = lc + bias_g[t5_bucket(dist_c)].transpose(2, 3, 0, 1)[None]
        lc = jnp.where(valid_c, lc, NEG)
        pc = jax.nn.softmax(lc, axis=-1) * valid_c
        oc = jnp.einsum('bgpqn,bngd->bqgpd', pc.astype(vc.dtype), vc)
        imp = jnp.einsum('bgqn,nj->bgqj', pc.sum(axis=2), overlap)
        qblk = t // SLC_LEN
        forced = ((jblk[None, :] == 0) | (jblk[None, :] == qblk[:, None])
                  | (jblk[None, :] == qblk[:, None] - 1))
        causal_s = slc_start[None, :] <= t[:, None]
        score = jnp.where(causal_s, jnp.where(forced, FORCED, imp), NEG)
        sel_val, sel_idx = lax.top_k(score, topk)
        idx = sel_idx.transpose(0, 2, 1, 3)
        ok = (sel_val > NEG * 0.5).transpose(0, 2, 1, 3)
        ks = ks_blk[bidx, gidx, idx]
        vs = vs_blk[bidx, gidx, idx]
        pos = idx[..., None] * SLC_LEN + jnp.arange(SLC_LEN)
        dist_s = t[None, :, None, None, None] - pos
        valid_s = ok[..., None] & (dist_s >= 0)
        ls = jnp.einsum('bqgpd,bqgskd->bqgskp', qi, ks).astype(f32) * scale
        ls = ls + bias_g[t5_bucket(dist_s), gidx5]
        ls = jnp.where(valid_s[..., None], ls, NEG)
        ps = jax.nn.softmax(ls, axis=(3, 4))
        osl = jnp.einsum('bqgskp,bqgskd->bqgpd', ps.astype(vs.dtype), vs)
        q0 = bi * Q_BLOCK
        kwin = lax.dynamic_slice_in_dim(kw, q0, WINDOW + Q_BLOCK, axis=1)
        vwin = lax.dynamic_slice_in_dim(vw, q0, WINDOW + Q_BLOCK, axis=1)
        spos = q0 - WINDOW + jnp.arange(WINDOW + Q_BLOCK)
        dist_w = t[:, None] - spos[None, :]
        valid_w = (dist_w >= 0) & (dist_w < WINDOW) & (spos[None, :] >= 0)
        lw = jnp.einsum('bqgpd,bkgd->bgpqk', qi, kwin).astype(f32) * scale
        lw = lw + bias_g[t5_bucket(dist_w)].transpose(2, 3, 0, 1)[None]
        lw = jnp.where(valid_w, lw, NEG)
        pw = jax.nn.softmax(lw, axis=-1)
        ow = jnp.einsum('bgpqk,bkgd->bqgpd', pw.astype(vwin.dtype), vwin)
        return (gi[:, :, 0, :, :, None] * oc + gi[:, :, 1, :, :, None] * osl
                + gi[:, :, 2, :, :, None] * ow)

    out = lax.map(block_fn, (qb, gb, jnp.arange(n_qb)))
    return out.transpose(1, 0, 2, 3, 4, 5).reshape(B, L, D_ATTN)


def peer(h, w_q, sub_k1, sub_k2, exp_u, exp_v):
    B, L, D = h.shape
    T = B * L
    hf = h.reshape(T, D)
    q = (hf @ w_q).reshape(T, PEER_HEADS, 2, PEER_DKEY // 2)
    s1 = jnp.einsum('thd,nd->thn', q[:, :, 0], sub_k1).astype(jnp.float32)
    s2 = jnp.einsum('thd,nd->thn', q[:, :, 1], sub_k2).astype(jnp.float32)
    v1, i1 = lax.top_k(s1, PEER_TOPK)
    v2, i2 = lax.top_k(s2, PEER_TOPK)
    cand = (v1[..., :, None] + v2[..., None, :]).reshape(T, PEER_HEADS, PEER_TOPK * PEER_TOPK)
    sv, si = lax.top_k(cand, PEER_TOPK)
    e1 = jnp.take_along_axis(i1, si // PEER_TOPK, axis=-1)
    e2 = jnp.take_along_axis(i2, si % PEER_TOPK, axis=-1)
    expert = e1 * PEER_NKEYS + e2
    gate = jax.nn.softmax(sv, axis=-1)
    n_chunks = T // PEER_CHUNK

    def chunk_fn(args):
        xc, ec, gc = args
        u = exp_u[ec]
        a = jax.nn.gelu(jnp.einsum('cd,chkd->chk', xc, u).astype(jnp.float32))
        w = (gc * a).astype(exp_v.dtype)
        return jnp.einsum('chk,chkd->cd', w, exp_v[ec])

    out = lax.map(chunk_fn, (hf.reshape(n_chunks, PEER_CHUNK, D),
                             expert.reshape(n_chunks, PEER_CHUNK, PEER_HEADS, PEER_TOPK),
                             gate.reshape(n_chunks, PEER_CHUNK, PEER_HEADS, PEER_TOPK)))
    return out.reshape(B, L, D)


def setup_inputs(seed: int = 0) -> dict:
    key = jax.random.key(seed)
    ks = jax.random.split(key, 33)
    f32 = jnp.float32
    D = D_MODEL
    G, P, H = N_SSM_GROUPS, SSM_STATE, SSM_GROUP

    def nrm(k, shape, s):
        return jax.random.normal(k, shape, f32) * s

    lam_im_base = jnp.pi * jnp.arange(P, dtype=f32)
    return {
        'x': nrm(ks[0], (BATCH, SEQ, D), 1.0),
        'c': nrm(ks[1], (BATCH, D), 1.0),
        'w_ada': nrm(ks[2], (DEPTH, D, N_ADA * D), 0.5 * D ** -0.5),
        'b_ada': nrm(ks[3], (DEPTH, N_ADA * D), 0.02),
        'norm1_g': 1.0 + nrm(ks[4], (DEPTH, D), 0.02),
        'norm2_g': 1.0 + nrm(ks[5], (DEPTH, D), 0.02),
        'w_in': nrm(ks[6], (DEPTH, D, D_IN), D ** -0.5),
        'w_out': nrm(ks[7], (DEPTH, D_MIX, D), D_MIX ** -0.5),
        'beta_ssm': 1.0 + nrm(ks[8], (DEPTH, D_SSM), 0.02),
        'beta_attn': 1.0 + nrm(ks[9], (DEPTH, D_ATTN), 0.02),
        'ssm_lam_re': -0.5 + nrm(ks[10], (DEPTH, G, P), 0.01),
        'ssm_lam_im': lam_im_base + nrm(ks[11], (DEPTH, G, P), 0.01),
        'ssm_log_dt': jax.random.uniform(ks[12], (DEPTH, G), f32, math.log(DT_MIN), math.log(DT_MAX)),
        'ssm_b_re': nrm(ks[13], (DEPTH, G, P, H), (2 * H) ** -0.5),
        'ssm_b_im': nrm(ks[14], (DEPTH, G, P, H), (2 * H) ** -0.5),
        'ssm_c_re': nrm(ks[15], (DEPTH, G, H, P), (2 * P) ** -0.5 * 4.0),
        'ssm_c_im': nrm(ks[16], (DEPTH, G, H, P), (2 * P) ** -0.5 * 4.0),
        'ssm_d': nrm(ks[17], (DEPTH, G, H), 1.0),
        'ssm_w_glu': nrm(ks[18], (DEPTH, D_SSM, D_SSM), D_SSM ** -0.5),
        'ssm_b_glu': nrm(ks[19], (DEPTH, D_SSM), 0.02),
        'cmp_pe_k': nrm(ks[20], (DEPTH, CMP_LEN, HEAD_DIM), 0.02),
        'cmp_w1_k': nrm(ks[21], (DEPTH, CMP_LEN * HEAD_DIM, CMP_HIDDEN), (CMP_LEN * HEAD_DIM) ** -0.5),
        'cmp_w2_k': nrm(ks[22], (DEPTH, CMP_HIDDEN, HEAD_DIM), CMP_HIDDEN ** -0.5 * 2.0),
        'cmp_pe_v': nrm(ks[23], (DEPTH, CMP_LEN, HEAD_DIM), 0.02),
        'cmp_w1_v': nrm(ks[24], (DEPTH, CMP_LEN * HEAD_DIM, CMP_HIDDEN), (CMP_LEN * HEAD_DIM) ** -0.5),
        'cmp_w2_v': nrm(ks[25], (DEPTH, CMP_HIDDEN, HEAD_DIM), CMP_HIDDEN ** -0.5 * 2.0),
        'rel_bias': nrm(ks[26], (N_BUCKETS, N_HEADS), 0.1),
        'peer_w_q': nrm(ks[27], (DEPTH, D, PEER_HEADS * PEER_DKEY), D ** -0.5),
        'peer_k1': nrm(ks[28], (DEPTH, PEER_NKEYS, PEER_DKEY // 2), (PEER_DKEY // 2) ** -0.5),
        'peer_k2': nrm(ks[29], (DEPTH, PEER_NKEYS, PEER_DKEY // 2), (PEER_DKEY // 2) ** -0.5),
        'peer_u': nrm(ks[30], (DEPTH, PEER_EXPERTS, D), D ** -0.5),
        'peer_v': nrm(ks[31], (DEPTH, PEER_EXPERTS, D), PEER_HEADS ** -0.5),
        'final_g': 1.0 + nrm(ks[32], (D,), 0.02),
    }


def reference(x, c, w_ada, b_ada, norm1_g, norm2_g, w_in, w_out, beta_ssm, beta_attn,
              ssm_lam_re, ssm_lam_im, ssm_log_dt, ssm_b_re, ssm_b_im, ssm_c_re, ssm_c_im,
              ssm_d, ssm_w_glu, ssm_b_glu, cmp_pe_k, cmp_w1_k, cmp_w2_k, cmp_pe_v, cmp_w1_v,
              cmp_w2_v, rel_bias, peer_w_q, peer_k1, peer_k2, peer_u, peer_v, final_g):
    B, L, _ = x.shape
    for l in range(DEPTH):
        cond = jax.nn.silu(c) @ w_ada[l] + b_ada[l]
        sh1, sc1, g1, sh2, sc2, g2 = [m[:, None, :] for m in jnp.split(cond, N_ADA, axis=-1)]
        h = rmsnorm(x, norm1_g[l]) * (1.0 + sc1) + sh1
        proj = h @ w_in[l]
        u_ssm, q, kv, gate_logits = jnp.split(
            proj, [D_SSM, D_SSM + D_ATTN, D_SSM + D_ATTN + KV_COLS], axis=-1)
        q = q.reshape(B, L, N_HEADS, HEAD_DIM)
        kv = kv.reshape(B, L, N_BRANCH, 2, N_KV, HEAD_DIM)
        gates = jax.nn.sigmoid(gate_logits.reshape(B, L, N_BRANCH, N_HEADS))
        y_ssm = s5_mixer(u_ssm, ssm_lam_re[l], ssm_lam_im[l], ssm_log_dt[l], ssm_b_re[l], ssm_b_im[l],
                         ssm_c_re[l], ssm_c_im[l], ssm_d[l], ssm_w_glu[l], ssm_b_glu[l])
        y_att = nsa_attention(q, kv[:, :, 0, 0], kv[:, :, 0, 1], kv[:, :, 1, 0], kv[:, :, 1, 1],
                              kv[:, :, 2, 0], kv[:, :, 2, 1], gates, rel_bias,
                              cmp_pe_k[l], cmp_w1_k[l], cmp_w2_k[l], cmp_pe_v[l], cmp_w1_v[l], cmp_w2_v[l])
        mixed = jnp.concatenate([rmsnorm(y_ssm, beta_ssm[l]), rmsnorm(y_att, beta_attn[l])], axis=-1)
        x = x + g1 * (mixed @ w_out[l])
        h2 = rmsnorm(x, norm2_g[l]) * (1.0 + sc2) + sh2
        x = x + g2 * peer(h2, peer_w_q[l], peer_k1[l], peer_k2[l], peer_u[l], peer_v[l])
    return rmsnorm(x, final_g)
```

```python
import numpy as np
from contextlib import ExitStack
import concourse.bass as bass
import concourse.mybir as mybir
from concourse.bass_utils import run_bass_kernel_spmd

F32 = mybir.dt.float32
BF16 = mybir.dt.bfloat16
I32 = mybir.dt.int32
AF = mybir.ActivationFunctionType
ALU = mybir.AluOpType
AX = mybir.AxisListType


class Buf:
    __slots__ = ("name", "ap", "lw", "rd")

    def __init__(self, name, ap):
        self.name = name
        self.ap = ap
        self.lw = None
        self.rd = []

    def __getitem__(self, idx):
        return self.ap[idx]


class Prog:
    COMPUTE = ("pe", "act", "dve", "pool")
    ALL = ("pe", "act", "dve", "pool", "sp")
    NDSEM = 16

    def __init__(self, nc, stack):
        self.nc = nc
        self.st = stack
        self.rec = {e: [] for e in self.ALL}
        self.sem = {}
        self.cnt = {}
        for e in self.COMPUTE:
            self.sem[e] = stack.enter_context(nc.semaphore("s_" + e))
            self.cnt[e] = 0
        self.dsem = {}
        self.dcnt = {}
        self.dnext = {}
        for q in ("sp", "act", "pool"):
            self.dsem[q] = [stack.enter_context(nc.semaphore(f"d_{q}{i}")) for i in range(self.NDSEM)]
            self.dcnt[q] = [0] * self.NDSEM
            self.dnext[q] = 0
        self.waited = {e: {} for e in self.ALL}
        self.semobj = {}
        self.nbuf = 0

    def sb(self, name, shape, dt=F32):
        self.nbuf += 1
        t = self.st.enter_context(self.nc.sbuf_tensor(f"sb_{name}_{self.nbuf}", list(shape), dt))
        return Buf(name, t)

    def ps(self, name, shape, dt=F32):
        self.nbuf += 1
        t = self.st.enter_context(self.nc.psum_tensor(f"ps_{name}_{self.nbuf}", list(shape), dt))
        return Buf(name, t)

    def view(self, name, ap):
        return Buf(name, ap)

    def _events(self, reads, writes):
        evs = []
        for b in reads:
            if b.lw is not None:
                evs.append(b.lw)
        for b in writes:
            if b.lw is not None:
                evs.append(b.lw)
            evs.extend(b.rd)
        return evs

    def _emit_waits(self, e, evs):
        need = {}
        for (sk, v) in evs:
            if sk == e and e == "pe":
                continue
            if self.waited[e].get(sk, 0) < v:
                if need.get(sk, 0) < v:
                    need[sk] = v
        for sk, v in need.items():
            self.waited[e][sk] = v
            self.rec[e].append(("w", sk, v))

    def _semof(self, sk):
        if isinstance(sk, str):
            return self.sem[sk]
        q, i = sk
        return self.dsem[q][i]

    def _mark(self, ev, reads, writes):
        for b in writes:
            b.lw = ev
            b.rd = []
        for b in reads:
            if b not in writes:
                b.rd.append(ev)
                if len(b.rd) > 64:
                    last = {}
                    for (sk, v) in b.rd:
                        if last.get(sk, 0) < v:
                            last[sk] = v
                    b.rd = list(last.items())

    def op(self, e, fn, reads=(), writes=()):
        reads = list(reads)
        writes = list(writes)
        self._emit_waits(e, self._events(reads, writes))
        self.cnt[e] += 1
        ev = (e, self.cnt[e])
        self.rec[e].append(("o", fn, e, 1))
        self._mark(ev, reads, writes)
        return ev

    def dma(self, q, out_ap, in_ap, reads=(), writes=(), **kw):
        reads = list(reads)
        writes = list(writes)
        i = self.dnext[q]
        evs = self._events(reads, writes)
        if self.dcnt[q][i]:
            evs.append(((q, i), self.dcnt[q][i]))
        self._emit_waits(q, evs)
        self.dnext[q] = (i + 1) % self.NDSEM
        self.dcnt[q][i] += 16
        ev = ((q, i), self.dcnt[q][i])
        self.rec[q].append(("o", lambda eng: eng.dma_start(out=out_ap, in_=in_ap, **kw), (q, i), 16))
        self._mark(ev, reads, writes)
        return ev

    def dmalike(self, q, fn, reads=(), writes=()):
        reads = list(reads)
        writes = list(writes)
        i = self.dnext[q]
        evs = self._events(reads, writes)
        if self.dcnt[q][i]:
            evs.append(((q, i), self.dcnt[q][i]))
        self._emit_waits(q, evs)
        self.dnext[q] = (i + 1) % self.NDSEM
        self.dcnt[q][i] += 16
        ev = ((q, i), self.dcnt[q][i])
        self.rec[q].append(("o", fn, (q, i), 16))
        self._mark(ev, reads, writes)
        return ev

    def barrier(self):
        evs = self._all_events()
        for e in self.ALL:
            self._emit_waits(e, evs)

    def scope(self):
        prog = self

        class _S:
            def __enter__(self_s):
                self_s.old = prog.st
                self_s.new = ExitStack()
                prog.st = self_s.new
                return self_s

            def __exit__(self_s, *a):
                prog.barrier()
                prog.st = self_s.old
                self_s.new.close()
                return False
        return _S()

    def _all_events(self):
        evs = []
        for q in self.dsem:
            for i in range(self.NDSEM):
                if self.dcnt[q][i]:
                    evs.append(((q, i), self.dcnt[q][i]))
        for e in self.COMPUTE:
            if self.cnt[e]:
                evs.append((e, self.cnt[e]))
        return evs

    def finish(self, waiter="sp"):
        evs = []
        for q in self.dsem:
            for i in range(self.NDSEM):
                if self.dcnt[q][i]:
                    evs.append(((q, i), self.dcnt[q][i]))
        for e in self.COMPUTE:
            if self.cnt[e]:
                evs.append((e, self.cnt[e]))
        self._emit_waits(waiter, evs)

    def replay(self):
        nc = self.nc

        def run(ename):
            def f(eng):
                for item in self.rec[ename]:
                    if item[0] == "w":
                        eng.wait_ge(self._semof(item[1]), item[2])
                    else:
                        ins = item[1](eng)
                        ins.then_inc(self._semof(item[2]), item[3])
            return f

        with nc.Block() as block:
            block.sync(run("sp"))
            block.tensor(run("pe"))
            block.scalar(run("act"))
            block.vector(run("dve"))
            block.gpsimd(run("pool"))

    def ninstr(self):
        return {e: len(self.rec[e]) for e in self.ALL}


def _bufs(*items):
    out = []
    for it in items:
        if it is None:
            continue
        if isinstance(it, Buf):
            out.append(it)
        elif isinstance(it, (list, tuple)):
            out.extend(_bufs(*it))
    return out


def _sc(x):
    if isinstance(x, tuple):
        return x[1], [x[0]]
    return x, []


def mm(p, ob, oap, lb, lap, rb, rap, start=True, stop=True):
    return p.op("pe", lambda e: e.matmul(oap, lhsT=lap, rhs=rap, start=start, stop=stop), reads=_bufs(lb, rb), writes=[ob])


def tr(p, ob, oap, ib, iap, idb, idap):
    return p.op("pe", lambda e: e.transpose(out=oap, in_=iap, identity=idap), reads=[ib, idb], writes=[ob])


def act(p, ob, oap, ib, iap, func, bias=0.0, scale=1.0, accum=None, eng="act"):
    bv, br = _sc(bias)
    sv, sr = _sc(scale)
    kw = {}
    wr = [ob]
    if accum is not None:
        kw["accum_out"] = accum[1]
        wr.append(accum[0])
    return p.op("act", lambda e: e.activation(out=oap, in_=iap, func=func, bias=bv, scale=sv, **kw), reads=[ib] + br + sr, writes=wr)


def tt(p, eng, ob, oap, ab, aap, bb, bap, op):
    return p.op(eng, lambda e: e.tensor_tensor(out=oap, in0=aap, in1=bap, op=op), reads=[ab, bb], writes=[ob])


def ts(p, eng, ob, oap, ib, iap, s1, s2=None, op0=ALU.mult, op1=None, accum=None):
    v1, r1 = _sc(s1)
    v2, r2 = _sc(s2) if s2 is not None else (None, [])
    kw = {}
    wr = [ob]
    if op1 is not None:
        kw["op1"] = op1
    if accum is not None:
        kw["accum_out"] = accum[1]
        wr.append(accum[0])
    return p.op(eng, lambda e: e.tensor_scalar(out=oap, in0=iap, scalar1=v1, scalar2=v2, op0=op0, **kw), reads=[ib] + r1 + r2, writes=wr)


def stt(p, eng, ob, oap, ab, aap, s, bb, bap, op0, op1):
    v, r = _sc(s)
    return p.op(eng, lambda e: e.scalar_tensor_tensor(out=oap, in0=aap, scalar=v, in1=bap, op0=op0, op1=op1), reads=[ab, bb] + r, writes=[ob])


def cp(p, eng, ob, oap, ib, iap):
    if eng == "act":
        return p.op("act", lambda e: e.copy(out=oap, in_=iap), reads=[ib], writes=[ob])
    return p.op(eng, lambda e: e.tensor_copy(out=oap, in_=iap), reads=[ib], writes=[ob])


def mset(p, eng, ob, oap, val):
    return p.op(eng, lambda e: e.memset(oap, val), writes=[ob])


L = 16384
D = 2048
NCORES = 8
TPC = L // NCORES
D_IN = 3608
FM_CHUNKS = list(range(16)) + [16, 17, 18, 19, 20, 21, 24, 25]
TM_COLS = [(22, 2), (26, 2)]
QSCALE = 128.0 ** -0.5


def build_A():
    nc = bass.Bass("TRN2", target_bir_lowering=False)
    x = nc.dram_tensor("x", [TPC, D], F32, kind="ExternalInput").ap()
    cT = None
    n1T = nc.dram_tensor("n1T", [128, 16], F32, kind="ExternalInput").ap()
    wada = nc.dram_tensor("sh1T", [128, 16], F32, kind="ExternalInput").ap()
    bada = nc.dram_tensor("sc1T", [128, 16], F32, kind="ExternalInput").ap()
    w_in = nc.dram_tensor("w_in", [D, D_IN], F32, kind="ExternalInput").ap()
    ident = nc.dram_tensor("ident", [128, 128], F32, kind="ExternalInput").ap()
    uT_o = nc.dram_tensor("uT_o", [1024, TPC], F32, kind="ExternalOutput").ap()
    fm_o = nc.dram_tensor("fm_o", [16 * 128, TPC], BF16, kind="ExternalOutput").ap()
    vtm_o = nc.dram_tensor("vtm_o", [TPC, 512], BF16, kind="ExternalOutput").ap()
    gates_o = nc.dram_tensor("gates_o", [TPC, 24], F32, kind="ExternalOutput").ap()

    with ExitStack() as st:
        p = Prog(nc, st)
        emit_A(p, x, cT, n1T, wada, bada, w_in, ident, uT_o, fm_o, vtm_o, gates_o)
        p.finish()
        p.replay()
        print("phase A instrs", p.ninstr())
    return nc


def emit_cond(p, cT, wada, badaT, ncols, pcond, condT, idf, R=None, dlo=0, nd=None):
    nch = ncols // 128
    if nd is None:
        nd = nch
    ct = p.sb("ct", [128, 16])
    sc = p.sb("silc", [128, 16])
    screp = p.sb("screp", [128, 16, 128])
    bT = p.sb("badaT", [128, nd])
    if R is None:
        R = p.sb("condR", [128, ncols])
    dg = p.sb("conddg", [128, nd, 128])
    slabs = [p.sb(f"wslab{i}", [128, 16, 512]) for i in range(2)]
    p.dma("sp", ct[:], cT, writes=[ct])
    p.dma("sp", bT[:], badaT, writes=[bT])
    act(p, sc, sc[:], ct, ct[:], AF.Silu)
    for kc in range(16):
        cp(p, "dve", screp, screp[:, kc, :], sc, sc[:, kc:kc + 1].to_broadcast([128, 128]))
    wv = wada.rearrange("(k p) n -> p k n", p=128)
    for cg in range(ncols // 512):
        sl = slabs[cg % 2]
        p.dma("sp", sl[:], wv[:, :, cg * 512:(cg + 1) * 512], writes=[sl])
        for kc in range(16):
            mm(p, pcond, pcond[:], screp, screp[:, kc, :], sl, sl[:, kc, :], start=(kc == 0), stop=(kc == 15))
        cp(p, "dve", R, R[:, cg * 512:(cg + 1) * 512], pcond, pcond[:])
    tt(p, "dve", dg, dg[:], R, R[:, dlo * 128:(dlo + nd) * 128].rearrange("p (c k) -> p c k", k=128), idf, idf[:, None, :].to_broadcast([128, nd, 128]), ALU.mult)
    p.op("dve", lambda e: e.tensor_reduce(out=condT[:, 0:nd], in_=dg[:], axis=AX.X, op=ALU.add), reads=[dg], writes=[condT])
    tt(p, "dve", condT, condT[:, 0:nd], condT, condT[:, 0:nd], bT, bT[:], ALU.add)
    return R


def emit_norm_transpose(p, xt, hT, hTk, tcol, gam, sh, idb, scr, ssq, rstd, xn, ptr, evac_engs=("dve", "act")):
    p.op("act", lambda e: e.activation(out=scr[:], in_=xt[:], func=AF.Square, accum_out=ssq[:]), reads=[xt], writes=[scr, ssq])
    p.op("dve", lambda e: e.tensor_scalar(out=rstd[:], in0=ssq[:], scalar1=1.0 / D, scalar2=1e-6, op0=ALU.mult, op1=ALU.add),
         reads=[ssq], writes=[rstd])
    p.op("act", lambda e: e.sqrt(out=rstd[:], in_=rstd[:]), reads=[rstd], writes=[rstd])
    p.op("dve", lambda e: e.reciprocal(out=rstd[:], in_=rstd[:]), reads=[rstd], writes=[rstd])
    p.op("dve", lambda e: e.tensor_scalar(out=xn[:], in0=xt[:], scalar1=rstd[:, 0:1], scalar2=None, op0=ALU.mult), reads=[xt, rstd], writes=[xn])
    for half in range(2):
        pt = ptr[half]
        for k8 in range(8):
            kc = half * 8 + k8
            p.op("pe", lambda e, kc=kc, k8=k8, pt=pt: e.transpose(out=pt[:, k8 * 128:(k8 + 1) * 128], in_=xn[:, kc * 128:(kc + 1) * 128],
                                                                    identity=idb[:]),
                 reads=[xn, idb], writes=[pt])
        for k8 in range(8):
            kc = half * 8 + k8
            eng = evac_engs[half % len(evac_engs)]
            if eng == "act":
                p.op("act", lambda e, kc=kc, k8=k8, pt=pt: e.activation(out=hT[:, kc, tcol:tcol + 128], in_=pt[:, k8 * 128:(k8 + 1) * 128],
                                                                          func=AF.Identity, bias=sh[:, kc:kc + 1], scale=gam[:, kc:kc + 1]),
                     reads=[pt, gam, sh], writes=[hTk[kc]])
            else:
                p.op(eng, lambda e, kc=kc, k8=k8, pt=pt: e.tensor_scalar(out=hT[:, kc, tcol:tcol + 128], in0=pt[:, k8 * 128:(k8 + 1) * 128],
                                                                          scalar1=gam[:, kc:kc + 1], scalar2=sh[:, kc:kc + 1],
                                                                          op0=ALU.mult, op1=ALU.add),
                     reads=[pt, gam, sh], writes=[hTk[kc]])


def emit_A(p, x, cT, n1T, wada, bada, w_in, ident, uT_o, fm_o, vtm_o, gates_o):
    pcond = p.ps("pcond", [128, 512])
    ptr = [p.ps(f"ptr{i}", [128, 1024], BF16) for i in range(2)]
    pmm = [p.ps(f"pmm{i}", [128, 512]) for i in range(4)]

    idf = p.sb("idf", [128, 128])
    idb = p.sb("idb", [128, 128], BF16)
    p.dma("sp", idf[:], ident, writes=[idf])
    p.op("dve", lambda e: e.tensor_copy(out=idb[:], in_=idf[:]), reads=[idf], writes=[idb])

    sh = p.sb("sh1", [128, 16]); sc1 = p.sb("sc1", [128, 16])
    p.dma("sp", sh[:], wada, writes=[sh]); p.dma("sp", sc1[:], bada, writes=[sc1])
    ng = p.sb("ng", [128, 16])
    gam = p.sb("gam", [128, 16])
    p.dma("sp", ng[:], n1T, writes=[ng])
    stt(p, "dve", gam, gam[:], sc1, sc1[:], 1.0, ng, ng[:], ALU.add, ALU.mult)
    import os
    STAGE = 99

    Wb = p.sb("Wb", [128, 16, D_IN], BF16)
    Wbk = [p.view(f"Wb{kc}", Wb[:, kc, :]) for kc in range(16)]
    with p.scope():
        wst = [p.sb(f"wst{i}", [128, D_IN]) for i in range(3)]
        for kc in range(16):
            s = wst[kc % 3]
            p.dma("sp", s[:], w_in[kc * 128:(kc + 1) * 128, :], writes=[s])
            eng = ("dve", "pool")[kc % 2]
            p.op(eng, lambda e, kc=kc, s=s: e.tensor_copy(out=Wb[:, kc, :], in_=s[:]), reads=[s], writes=[Wbk[kc]])

    if STAGE <= 2:
        return
    xts = [p.sb(f"xt{i}", [128, D]) for i in range(2)]
    scr = p.sb("sqscr", [128, D], BF16)
    xn = [p.sb(f"xn{i}", [128, D], BF16) for i in range(2)]
    ssq = [p.sb(f"ssq{i}", [128, 1]) for i in range(2)]
    rstd = [p.sb(f"rstd{i}", [128, 1]) for i in range(2)]
    hTs = [p.sb(f"hT{i}", [128, 16, 512], BF16) for i in range(2)]
    hTks = [[p.view(f"hT{i}_{kc}", hTs[i][:, kc, :]) for kc in range(16)] for i in range(2)]
    ofm = [p.sb(f"ofm{i}", [128, 512], BF16) for i in range(3)]
    ofu = [p.sb(f"ofu{i}", [128, 512]) for i in range(2)]
    ovt = [p.sb(f"ovt{i}", [128, 512], BF16) for i in range(2)]
    ogt = [p.sb(f"ogt{i}", [128, 24]) for i in range(2)]

    nfm = 0
    nu = 0
    nv = 0
    for tg in range(TPC // 512):
        hT = hTs[tg % 2]
        hTk = hTks[tg % 2]
        for ti in range(4):
            t = tg * 4 + ti
            xt = xts[t % 2]
            p.dma("sp", xt[:], x[t * 128:(t + 1) * 128, :], writes=[xt])
            emit_norm_transpose(p, xt, hT, hTk, ti * 128, gam, sh, idb, scr, ssq[t % 2], rstd[t % 2], xn[t % 2], ptr)
        if STAGE <= 3:
            break
        for j, ch in enumerate(FM_CHUNKS):
            pm = pmm[j % 3]
            for kc in range(16):
                p.op("pe", lambda e, kc=kc, ch=ch, pm=pm, hT=hT: e.matmul(pm[:], lhsT=Wb[:, kc, ch * 128:(ch + 1) * 128], rhs=hT[:, kc, :],
                                                                          start=(kc == 0), stop=(kc == 15)),
                     reads=[Wbk[kc], hTk[kc]], writes=[pm])
            if ch < 8:
                o = ofu[nu % 2]
                nu += 1
                p.op("dve", lambda e, o=o, pm=pm: e.tensor_copy(out=o[:], in_=pm[:]), reads=[pm], writes=[o])
                p.dma("act", uT_o[ch * 128:(ch + 1) * 128, tg * 512:(tg + 1) * 512], o[:], reads=[o])
            else:
                o = ofm[nfm % 3]
                nfm += 1
                scale = QSCALE if ch < 16 else 1.0
                p.op("act", lambda e, o=o, pm=pm, scale=scale: e.activation(out=o[:], in_=pm[:], func=AF.Copy, scale=scale),
                     reads=[pm], writes=[o])
                p.dma("act", fm_o[(j - 8) * 128:(j - 7) * 128, tg * 512:(tg + 1) * 512], o[:], reads=[o])
        if STAGE <= 4:
            break
        for ti in range(4):
            t = tg * 4 + ti
            o = ovt[nv % 2]
            og = ogt[nv % 2]
            nv += 1
            for i, (c0, nchk) in enumerate(TM_COLS):
                pm = pmm[3]
                for kc in range(16):
                    p.op("pe", lambda e, kc=kc, c0=c0, nchk=nchk, pm=pm, hT=hT, ti=ti: e.matmul(
                        pm[:, 0:nchk * 128], lhsT=hT[:, kc, ti * 128:(ti + 1) * 128], rhs=Wb[:, kc, c0 * 128:(c0 + nchk) * 128],
                        start=(kc == 0), stop=(kc == 15)), reads=[Wbk[kc], hTk[kc]], writes=[pm])
                p.op("dve", lambda e, o=o, pm=pm, i=i: e.tensor_copy(out=o[:, i * 256:(i + 1) * 256], in_=pm[:, 0:256]), reads=[pm], writes=[o])
            p.dma("act", vtm_o[t * 128:(t + 1) * 128, :], o[:], reads=[o])
            pm = pmm[3]
            for kc in range(16):
                p.op("pe", lambda e, kc=kc, pm=pm, hT=hT, ti=ti: e.matmul(pm[:, 0:24], lhsT=hT[:, kc, ti * 128:(ti + 1) * 128], rhs=Wb[:, kc, 3584:3608],
                                                                        start=(kc == 0), stop=(kc == 15)), reads=[Wbk[kc], hTk[kc]], writes=[pm])
            p.op("act", lambda e, og=og, pm=pm: e.activation(out=og[:], in_=pm[:, 0:24], func=AF.Sigmoid), reads=[pm], writes=[og])
            p.dma("act", gates_o[t * 128:(t + 1) * 128, :], og[:], reads=[og])


NC0 = 1536


def build_L0():
    nc = bass.Bass("TRN2", target_bir_lowering=False)
    cT = nc.dram_tensor("cT", [128, 16], F32, kind="ExternalInput").ap()
    wada = nc.dram_tensor("wada", [D, NC0], F32, kind="ExternalInput").ap()
    badaT = nc.dram_tensor("badaT", [128, NC0 // 128], F32, kind="ExternalInput").ap()
    ident = nc.dram_tensor("ident", [128, 128], F32, kind="ExternalInput").ap()
    condT_o = nc.dram_tensor("condT_o", [128, NC0 // 128], F32, kind="ExternalOutput").ap()
    with ExitStack() as st:
        p = Prog(nc, st)
        pcond = p.ps("pcond", [128, 512])
        idf = p.sb("idf", [128, 128])
        p.dma("sp", idf[:], ident, writes=[idf])
        condT = p.sb("condT", [128, NC0 // 128])
        emit_cond(p, cT, wada, badaT, NC0, pcond, condT, idf)
        p.dma("act", condT_o, condT[:], reads=[condT])
        p.finish()
        p.replay()
    return nc

import math

L = 16384
NCH = L // 128
TWO_PI = 2.0 * math.pi


_SC_N = [0]


def emit_sincos(p, ang, cos_o, sin_o, tmp, eng="dve"):
    (ab, aap), (cb, cap), (sb_, sap), (tb, tap) = ang, cos_o, sin_o, tmp
    shape = list(tb.ap.shape)
    _SC_N[0] += 1
    ni = p.sb(f"sc_ni{_SC_N[0]}", shape, I32)
    nf = p.sb(f"sc_nf{_SC_N[0]}", shape)
    cm = p.sb(f"sc_cm{_SC_N[0]}", shape)
    C1 = 6.28125
    C2 = TWO_PI - C1
    for shift, (ob, oap) in ((0.0, (sb_, sap)), (0.5 * math.pi, (cb, cap))):
        ts(p, eng, tb, tap, ab, aap, shift, None, op0=ALU.add)
        ts(p, eng, nf, nf[:], tb, tap, 1.0 / TWO_PI, None, op0=ALU.mult)
        cp(p, eng, ni, ni[:], nf, nf[:])
        cp(p, eng, nf, nf[:], ni, ni[:])
        stt(p, eng, tb, tap, nf, nf[:], -C1, tb, tap, ALU.mult, ALU.add)
        stt(p, eng, tb, tap, nf, nf[:], -C2, tb, tap, ALU.mult, ALU.add)
        ts(p, eng, cm, cm[:], tb, tap, math.pi, None, op0=ALU.is_gt)
        stt(p, eng, tb, tap, cm, cm[:], -TWO_PI, tb, tap, ALU.mult, ALU.add)
        ts(p, eng, cm, cm[:], tb, tap, -math.pi, None, op0=ALU.is_lt)
        stt(p, eng, tb, tap, cm, cm[:], TWO_PI, tb, tap, ALU.mult, ALU.add)
        ts(p, eng, tb, tap, tb, tap, math.pi, -math.pi, op0=ALU.min, op1=ALU.max)
        act(p, ob, oap, tb, tap, AF.Sin)


NEGPI = [None]


def emit_S5(p, uT_d, prm, yT_o):
    negpi = p.sb("negpi", [128, 1])
    p.op("dve", lambda e: e.memset(negpi[:], -math.pi), writes=[negpi])
    NEGPI[0] = negpi

    def load(name, shape):
        b = p.sb("s5_" + name, shape)
        p.dma("sp", b[:], prm[name], writes=[b])
        return b

    jcol = load("jcol", [128, 1])
    irow = load("irow", [128, 129])
    triT = load("triT", [128, 128])
    maskBD = load("maskBD", [128, 8])
    d_ch = load("d_ch", [128, 1])
    CTre = load("CTre", [128, 4, 128])
    CTim = load("CTim", [128, 4, 128])
    p.op("pool", lambda e: e.tensor_scalar(out=CTim[:], in0=CTim[:], scalar1=-1.0, scalar2=None, op0=ALU.mult), reads=[CTim], writes=[CTim])

    BD = p.sb("BD", [128, 4, 2, 2, 64])
    Er = p.sb("Er", [128, 4, 128])
    Ei = p.sb("Ei", [128, 4, 128])
    Fr = p.sb("Fr", [128, 4, 129])
    Fi = p.sb("Fi", [128, 4, 129])

    with p.scope():
        lr = load("lr_ch", [128, 64]); li = load("li_ch", [128, 64]); ldt = load("ldt_ch", [128, 1])
        bre = load("bre_ch", [128, 64]); bim = load("bim_ch", [128, 64])
        dt = p.sb("dt_ch", [128, 1])
        p.op("act", lambda e: e.activation(out=dt[:], in_=ldt[:], func=AF.Exp), reads=[ldt], writes=[dt])
        lrdt = p.sb("lrdt", [128, 64]); th = p.sb("th", [128, 64]); mag = p.sb("mag", [128, 64])
        cs = p.sb("cs", [128, 64]); sn = p.sb("sn", [128, 64]); tmp = p.sb("tmpc", [128, 64])
        p.op("dve", lambda e: e.tensor_scalar(out=lrdt[:], in0=lr[:], scalar1=dt[:, 0:1], scalar2=None, op0=ALU.mult), reads=[lr, dt], writes=[lrdt])
        p.op("dve", lambda e: e.tensor_scalar(out=th[:], in0=li[:], scalar1=dt[:, 0:1], scalar2=None, op0=ALU.mult), reads=[li, dt], writes=[th])
        p.op("act", lambda e: e.activation(out=mag[:], in_=lrdt[:], func=AF.Exp), reads=[lrdt], writes=[mag])
        emit_sincos(p, (th, th[:]), (cs, cs[:]), (sn, sn[:]), (tmp, tmp[:]))
        nr = p.sb("nr", [128, 64]); ni = p.sb("ni", [128, 64]); den = p.sb("den", [128, 64]); t2 = p.sb("t2", [128, 64])
        wre = p.sb("wre", [128, 64]); wim = p.sb("wim", [128, 64]); bbr = p.sb("bbr", [128, 64]); bbi = p.sb("bbi", [128, 64])
        V = "dve"
        p.op(V, lambda e: e.tensor_tensor(out=nr[:], in0=mag[:], in1=cs[:], op=ALU.mult), reads=[mag, cs], writes=[nr])
        p.op(V, lambda e: e.tensor_scalar(out=nr[:], in0=nr[:], scalar1=-1.0, scalar2=None, op0=ALU.add), reads=[nr], writes=[nr])
        p.op(V, lambda e: e.tensor_tensor(out=ni[:], in0=mag[:], in1=sn[:], op=ALU.mult), reads=[mag, sn], writes=[ni])
        p.op(V, lambda e: e.tensor_tensor(out=den[:], in0=lr[:], in1=lr[:], op=ALU.mult), reads=[lr], writes=[den])
        p.op(V, lambda e: e.tensor_tensor(out=t2[:], in0=li[:], in1=li[:], op=ALU.mult), reads=[li], writes=[t2])
        p.op(V, lambda e: e.tensor_tensor(out=den[:], in0=den[:], in1=t2[:], op=ALU.add), reads=[den, t2], writes=[den])
        p.op(V, lambda e: e.reciprocal(out=den[:], in_=den[:]), reads=[den], writes=[den])
        p.op(V, lambda e: e.tensor_tensor(out=wre[:], in0=nr[:], in1=lr[:], op=ALU.mult), reads=[nr, lr], writes=[wre])
        p.op(V, lambda e: e.tensor_tensor(out=t2[:], in0=ni[:], in1=li[:], op=ALU.mult), reads=[ni, li], writes=[t2])
        p.op(V, lambda e: e.tensor_tensor(out=wre[:], in0=wre[:], in1=t2[:], op=ALU.add), reads=[wre, t2], writes=[wre])
        p.op(V, lambda e: e.tensor_tensor(out=wre[:], in0=wre[:], in1=den[:], op=ALU.mult), reads=[wre, den], writes=[wre])
        p.op(V, lambda e: e.tensor_tensor(out=wim[:], in0=ni[:], in1=lr[:], op=ALU.mult), reads=[ni, lr], writes=[wim])
        p.op(V, lambda e: e.tensor_tensor(out=t2[:], in0=nr[:], in1=li[:], op=ALU.mult), reads=[nr, li], writes=[t2])
        p.op(V, lambda e: e.tensor_tensor(out=wim[:], in0=wim[:], in1=t2[:], op=ALU.subtract), reads=[wim, t2], writes=[wim])
        p.op(V, lambda e: e.tensor_tensor(out=wim[:], in0=wim[:], in1=den[:], op=ALU.mult), reads=[wim, den], writes=[wim])
        p.op(V, lambda e: e.tensor_tensor(out=bbr[:], in0=wre[:], in1=bre[:], op=ALU.mult), reads=[wre, bre], writes=[bbr])
        p.op(V, lambda e: e.tensor_tensor(out=t2[:], in0=wim[:], in1=bim[:], op=ALU.mult), reads=[wim, bim], writes=[t2])
        p.op(V, lambda e: e.tensor_tensor(out=bbr[:], in0=bbr[:], in1=t2[:], op=ALU.subtract), reads=[bbr, t2], writes=[bbr])
        p.op(V, lambda e: e.tensor_tensor(out=bbi[:], in0=wre[:], in1=bim[:], op=ALU.mult), reads=[wre, bim], writes=[bbi])
        p.op(V, lambda e: e.tensor_tensor(out=t2[:], in0=wim[:], in1=bre[:], op=ALU.mult), reads=[wim, bre], writes=[t2])
        p.op(V, lambda e: e.tensor_tensor(out=bbi[:], in0=bbi[:], in1=t2[:], op=ALU.add), reads=[bbi, t2], writes=[bbi])
        mview = maskBD[:].rearrange("c (q g) -> c q g", g=2)
        for ri, bb in ((0, bbr), (1, bbi)):
            for q in range(4):
                for gg in range(2):
                    p.op(V, lambda e, ri=ri, bb=bb, q=q, gg=gg: e.tensor_scalar(out=BD[:, q, ri, gg, :], in0=bb[:], scalar1=maskBD[:, q * 2 + gg:q * 2 + gg + 1],
                                                                               scalar2=None, op0=ALU.mult), reads=[bb, maskBD], writes=[BD])

        lr_r = load("lr_row", [128, 512]); li_r = load("li_row", [128, 512]); ldt_r = load("ldt_row", [128, 512])
        dt_r = p.sb("dt_r", [128, 512]); ang = p.sb("ang_r", [128, 512]); mg = p.sb("mg_r", [128, 512])
        cs_r = p.sb("cs_r", [128, 512]); sn_r = p.sb("sn_r", [128, 512]); tmp_r = p.sb("tmp_r", [128, 512])
        negj = p.sb("negj", [128, 1])
        p.op(V, lambda e: e.tensor_scalar(out=negj[:], in0=jcol[:], scalar1=-1.0, scalar2=None, op0=ALU.mult), reads=[jcol], writes=[negj])
        p.op("act", lambda e: e.activation(out=dt_r[:], in_=ldt_r[:], func=AF.Exp), reads=[ldt_r], writes=[dt_r])
        p.op(V, lambda e: e.tensor_tensor(out=lr_r[:], in0=lr_r[:], in1=dt_r[:], op=ALU.mult), reads=[lr_r, dt_r], writes=[lr_r])
        p.op(V, lambda e: e.tensor_tensor(out=li_r[:], in0=li_r[:], in1=dt_r[:], op=ALU.mult), reads=[li_r, dt_r], writes=[li_r])
        p.op("act", lambda e: e.activation(out=mg[:], in_=lr_r[:], func=AF.Exp, scale=negj[:, 0:1]), reads=[lr_r, negj], writes=[mg])
        p.op(V, lambda e: e.tensor_scalar(out=ang[:], in0=li_r[:], scalar1=jcol[:, 0:1], scalar2=None, op0=ALU.mult), reads=[li_r, jcol], writes=[ang])
        emit_sincos(p, (ang, ang[:]), (cs_r, cs_r[:]), (sn_r, sn_r[:]), (tmp_r, tmp_r[:]))
        p.op(V, lambda e: e.tensor_tensor(out=Er[:], in0=mg[:].rearrange("c (q s) -> c q s", q=4), in1=cs_r[:].rearrange("c (q s) -> c q s", q=4), op=ALU.mult),
             reads=[mg, cs_r], writes=[Er])
        p.op(V, lambda e: e.scalar_tensor_tensor(out=Ei[:], in0=mg[:].rearrange("c (q s) -> c q s", q=4), scalar=-1.0,
                                                 in1=sn_r[:].rearrange("c (q s) -> c q s", q=4), op0=ALU.mult, op1=ALU.mult),
             reads=[mg, sn_r], writes=[Ei])

        lr_s = load("lr_sp", [128, 4]); li_s = load("li_sp", [128, 4]); ldt_s = load("ldt_sp", [128, 4])
        dt_s = p.sb("dt_s", [128, 4])
        p.op("act", lambda e: e.activation(out=dt_s[:], in_=ldt_s[:], func=AF.Exp), reads=[ldt_s], writes=[dt_s])
        p.op(V, lambda e: e.tensor_tensor(out=lr_s[:], in0=lr_s[:], in1=dt_s[:], op=ALU.mult), reads=[lr_s, dt_s], writes=[lr_s])
        p.op(V, lambda e: e.tensor_tensor(out=li_s[:], in0=li_s[:], in1=dt_s[:], op=ALU.mult), reads=[li_s, dt_s], writes=[li_s])
        ang_s = p.sb("ang_s", [128, 4, 129]); mg_s = p.sb("mg_s", [128, 4, 129]); cs_s = p.sb("cs_s", [128, 4, 129]); sn_s = p.sb("sn_s", [128, 4, 129])
        tmp_s = p.sb("tmp_s", [128, 4, 129])
        for q in range(4):
            p.op("act", lambda e, q=q: e.activation(out=mg_s[:, q, :], in_=irow[:], func=AF.Exp, scale=lr_s[:, q:q + 1]), reads=[irow, lr_s], writes=[mg_s])
            p.op(V, lambda e, q=q: e.tensor_scalar(out=ang_s[:, q, :], in0=irow[:], scalar1=li_s[:, q:q + 1], scalar2=None, op0=ALU.mult),
                 reads=[irow, li_s], writes=[ang_s])
        emit_sincos(p, (ang_s, ang_s[:]), (cs_s, cs_s[:]), (sn_s, sn_s[:]), (tmp_s, tmp_s[:]))
        p.op(V, lambda e: e.tensor_tensor(out=Fr[:], in0=mg_s[:], in1=cs_s[:], op=ALU.mult), reads=[mg_s, cs_s], writes=[Fr])
        p.op(V, lambda e: e.tensor_tensor(out=Fi[:], in0=mg_s[:], in1=sn_s[:], op=ALU.mult), reads=[mg_s, sn_s], writes=[Fi])

    with p.scope():
        uT = p.sb("uT_sb", [128, L])
        NLD = 8
        uTv = [p.view(f"uT{i}", uT[:, i * (L // NLD):(i + 1) * (L // NLD)]) for i in range(NLD)]
        for i in range(NLD):
            p.dma("sp", uT[:, i * (L // NLD):(i + 1) * (L // NLD)], uT_d[:, i * (L // NLD):(i + 1) * (L // NLD)], writes=[uTv[i]])
        pX = [p.ps(f"pX{i}", [128, 4, 2, 128]) for i in range(1)]
        pZ = [p.ps(f"pZ{i}", [128, 4, 2, 128]) for i in range(1)]
        pY = [p.ps(f"pY{i}", [128, 128]) for i in range(2)]
        Xs = p.sb("Xs", [128, 4, 2, 128])
        Wt = p.sb("Wt", [128, 4, 2, 128])
        Zs = p.sb("Zs", [128, 4, 2, 128])
        Sr = p.sb("Sr", [128, 4, 128]); Si = p.sb("Si", [128, 4, 128])
        m1 = p.sb("m1", [128, 4, 128]); m2 = p.sb("m2", [128, 4, 128]); m3 = p.sb("m3", [128, 4, 128]); m4 = p.sb("m4", [128, 4, 128])
        cpr = p.sb("cpr", [128, 4]); cpi = p.sb("cpi", [128, 4])
        ca = p.sb("ca", [128, 4]); cb = p.sb("cb", [128, 4])
        p.op("dve", lambda e: e.memset(cpr[:], 0.0), writes=[cpr])
        p.op("dve", lambda e: e.memset(cpi[:], 0.0), writes=[cpi])
        yst = [p.sb(f"yst{i}", [128, 512]) for i in range(2)]
        BDf = BD[:].rearrange("c q r g s -> c (q r g s)")
        for c in range(NCH):
            uc = uT[:, c * 128:(c + 1) * 128]
            ub = uTv[c * 128 // (L // NLD)]
            px = pX[0]; pz = pZ[0]; py = pY[c % 2]
            pxf = px[:].rearrange("c q r s -> c (q r s)")
            for h in range(2):
                p.op("pe", lambda e, h=h, uc=uc, pxf=pxf: e.matmul(pxf[:, h * 512:(h + 1) * 512], lhsT=uc, rhs=BDf[:, h * 512:(h + 1) * 512], start=True, stop=True),
                     reads=[ub, BD], writes=[px])
            p.op("act", lambda e, px=px: e.activation(out=Xs[:], in_=px[:], func=AF.Copy), reads=[px], writes=[Xs])
            p.op("dve", lambda e: e.tensor_tensor(out=m1[:], in0=Xs[:, :, 0, :], in1=Er[:], op=ALU.mult), reads=[Xs, Er], writes=[m1])
            p.op("dve", lambda e: e.tensor_tensor(out=m2[:], in0=Xs[:, :, 1, :], in1=Ei[:], op=ALU.mult), reads=[Xs, Ei], writes=[m2])
            p.op("dve", lambda e: e.tensor_tensor(out=Wt[:, :, 0, :], in0=m1[:], in1=m2[:], op=ALU.subtract), reads=[m1, m2], writes=[Wt])
            p.op("pool", lambda e: e.tensor_tensor(out=m3[:], in0=Xs[:, :, 0, :], in1=Ei[:], op=ALU.mult), reads=[Xs, Ei], writes=[m3])
            p.op("pool", lambda e: e.tensor_tensor(out=m4[:], in0=Xs[:, :, 1, :], in1=Er[:], op=ALU.mult), reads=[Xs, Er], writes=[m4])
            p.op("pool", lambda e: e.tensor_tensor(out=Wt[:, :, 1, :], in0=m3[:], in1=m4[:], op=ALU.add), reads=[m3, m4, Wt], writes=[Wt])
            for q in range(4):
                for ri in range(2):
                    p.op("pe", lambda e, q=q, ri=ri, pz=pz: e.matmul(pz[:, q, ri, :], lhsT=Wt[:, q, ri, :], rhs=triT[:], start=True, stop=True),
                         reads=[Wt, triT], writes=[pz])
            for q in range(4):
                p.op("act", lambda e, q=q, pz=pz: e.activation(out=Zs[:, q, 0, :], in_=pz[:, q, 0, :], func=AF.Identity, bias=cpr[:, q:q + 1], scale=1.0),
                     reads=[pz, cpr], writes=[Zs])
                p.op("act", lambda e, q=q, pz=pz: e.activation(out=Zs[:, q, 1, :], in_=pz[:, q, 1, :], func=AF.Identity, bias=cpi[:, q:q + 1], scale=1.0),
                     reads=[pz, cpi], writes=[Zs])
            if c + 1 < NCH:
                p.op("dve", lambda e: e.tensor_tensor(out=ca[:], in0=Zs[:, :, 0, 127], in1=Fr[:, :, 128], op=ALU.mult), reads=[Zs, Fr], writes=[ca])
                p.op("dve", lambda e: e.tensor_tensor(out=cb[:], in0=Zs[:, :, 1, 127], in1=Fi[:, :, 128], op=ALU.mult), reads=[Zs, Fi], writes=[cb])
                p.op("dve", lambda e: e.tensor_tensor(out=cpr[:], in0=ca[:], in1=cb[:], op=ALU.subtract), reads=[ca, cb], writes=[cpr])
                p.op("dve", lambda e: e.tensor_tensor(out=ca[:], in0=Zs[:, :, 0, 127], in1=Fi[:, :, 128], op=ALU.mult), reads=[Zs, Fi], writes=[ca])
                p.op("dve", lambda e: e.tensor_tensor(out=cb[:], in0=Zs[:, :, 1, 127], in1=Fr[:, :, 128], op=ALU.mult), reads=[Zs, Fr], writes=[cb])
                p.op("dve", lambda e: e.tensor_tensor(out=cpi[:], in0=ca[:], in1=cb[:], op=ALU.add), reads=[ca, cb], writes=[cpi])
            p.op("dve", lambda e: e.tensor_tensor(out=m1[:], in0=Zs[:, :, 0, :], in1=Fr[:, :, 0:128], op=ALU.mult), reads=[Zs, Fr], writes=[m1])
            p.op("dve", lambda e: e.tensor_tensor(out=m2[:], in0=Zs[:, :, 1, :], in1=Fi[:, :, 0:128], op=ALU.mult), reads=[Zs, Fi], writes=[m2])
            p.op("dve", lambda e: e.tensor_tensor(out=Sr[:], in0=m1[:], in1=m2[:], op=ALU.subtract), reads=[m1, m2], writes=[Sr])
            p.op("pool", lambda e: e.tensor_tensor(out=m3[:], in0=Zs[:, :, 0, :], in1=Fi[:, :, 0:128], op=ALU.mult), reads=[Zs, Fi], writes=[m3])
            p.op("pool", lambda e: e.tensor_tensor(out=m4[:], in0=Zs[:, :, 1, :], in1=Fr[:, :, 0:128], op=ALU.mult), reads=[Zs, Fr], writes=[m4])
            p.op("pool", lambda e: e.tensor_tensor(out=Si[:], in0=m3[:], in1=m4[:], op=ALU.add), reads=[m3, m4], writes=[Si])
            n = 0
            for q in range(4):
                for ri, (CT, S) in enumerate(((CTre, Sr), (CTim, Si))):
                    p.op("pe", lambda e, q=q, CT=CT, S=S, py=py, n=n: e.matmul(py[:], lhsT=CT[:, q, :], rhs=S[:, q, :], start=(n == 0), stop=(n == 7)),
                         reads=[CT, S], writes=[py])
                    n += 1
            ys = yst[(c // 4) % 2]
            p.op("dve", lambda e, uc=uc, py=py, ys=ys, c=c: e.scalar_tensor_tensor(out=ys[:, (c % 4) * 128:(c % 4 + 1) * 128], in0=uc, scalar=d_ch[:, 0:1], in1=py[:],
                                                                             op0=ALU.mult, op1=ALU.add), reads=[ub, d_ch, py], writes=[ys])
            if c % 4 == 3:
                p.dma("sp", yT_o[:, (c - 3) * 128:(c + 1) * 128], ys[:], reads=[ys])


def s5_host_params(inp, j):
    f = np.float32
    gs = slice(8 * j, 8 * j + 8)
    lr = inp["ssm_lam_re"][0][gs]; li = inp["ssm_lam_im"][0][gs]; ldt = inp["ssm_log_dt"][0][gs]
    bre = inp["ssm_b_re"][0][gs]; bim = inp["ssm_b_im"][0][gs]
    cre = inp["ssm_c_re"][0][gs]; cim = inp["ssm_c_im"][0][gs]
    d = inp["ssm_d"][0][gs]
    prm = {}
    prm["lr_ch"] = np.repeat(lr, 16, axis=0); prm["li_ch"] = np.repeat(li, 16, axis=0)
    prm["ldt_ch"] = np.repeat(ldt, 16)[:, None]
    prm["bre_ch"] = bre.transpose(0, 2, 1).reshape(128, 64); prm["bim_ch"] = bim.transpose(0, 2, 1).reshape(128, 64)
    prm["d_ch"] = d.reshape(128, 1)
    prm["lr_row"] = np.broadcast_to(lr.reshape(1, 512), (128, 512)); prm["li_row"] = np.broadcast_to(li.reshape(1, 512), (128, 512))
    prm["ldt_row"] = np.broadcast_to(np.repeat(ldt, 64).reshape(1, 512), (128, 512))
    def sp(a):
        return a.reshape(4, 2, 64).transpose(1, 2, 0).reshape(128, 4)
    prm["lr_sp"] = sp(lr); prm["li_sp"] = sp(li); prm["ldt_sp"] = sp(np.repeat(ldt[:, None], 64, axis=1))
    CTre = np.zeros((128, 4, 128), f); CTim = np.zeros((128, 4, 128), f)
    for q in range(4):
        for gg in range(2):
            g = 2 * q + gg
            CTre[gg * 64:(gg + 1) * 64, q, 16 * g:16 * g + 16] = cre[g].T
            CTim[gg * 64:(gg + 1) * 64, q, 16 * g:16 * g + 16] = cim[g].T
    prm["CTre"] = CTre; prm["CTim"] = CTim
    prm["jcol"] = np.arange(128, dtype=f)[:, None]
    prm["irow"] = np.broadcast_to(np.arange(129, dtype=f)[None, :], (128, 129))
    prm["triT"] = np.triu(np.ones((128, 128), f))
    m = np.zeros((128, 8), f)
    for g in range(8):
        m[16 * g:16 * g + 16, g] = 1.0
    prm["maskBD"] = m
    return {k: np.ascontiguousarray(v, dtype=f) for k, v in prm.items()}


S5_SHAPES = dict(lr_ch=[128, 64], li_ch=[128, 64], ldt_ch=[128, 1], bre_ch=[128, 64], bim_ch=[128, 64], d_ch=[128, 1],
                 lr_row=[128, 512], li_row=[128, 512], ldt_row=[128, 512], lr_sp=[128, 4], li_sp=[128, 4], ldt_sp=[128, 4],
                 CTre=[128, 4, 128], CTim=[128, 4, 128], jcol=[128, 1], irow=[128, 129], triT=[128, 128], maskBD=[128, 8])


def build_S5():
    nc = bass.Bass("TRN2", target_bir_lowering=False)
    uT = nc.dram_tensor("uT", [128, L], F32, kind="ExternalInput").ap()
    prm = {k: nc.dram_tensor(k, s, F32, kind="ExternalInput").ap() for k, s in S5_SHAPES.items()}
    yT_o = nc.dram_tensor("yT_o", [128, L], F32, kind="ExternalOutput").ap()
    with ExitStack() as st:
        p = Prog(nc, st)
        emit_S5(p, uT, prm, yT_o)
        p.finish()
        p.replay()
        print("S5 instrs", p.ninstr())
    return nc

import math

L = 16384
NEGM = -30000.0
NQG = L // 512


def t5_bucket_np(dist):
    n = np.maximum(dist, 0)
    me = 16
    scaled = np.log(np.maximum(n, me).astype(np.float32) / np.float32(me)) / np.float32(math.log(1024 / 16))
    large = me + (scaled * np.float32(16)).astype(np.int32)
    return np.where(n < me, n, np.minimum(large, 31))


def nsa_tables(rel_bias, heads4):
    f = np.float32
    own = heads4[0]
    k = np.arange(128)[:, None]
    cols = np.arange(11 * 128)[None, :]
    dist = (cols // 128 - 3) * 128 + (cols % 128) - k
    bw = np.where((dist >= 0) & (dist < 512), rel_bias[t5_bucket_np(dist), own], f(NEGM)).astype(f)
    cols = np.arange(14 * 128)[None, :]
    dist = (cols // 128 - 3) * 128 + (cols % 128) - k
    bs = np.where(dist >= 0, rel_bias[t5_bucket_np(dist), own], f(NEGM)).astype(f)
    bc = np.zeros((128, 4, 6, 512), f)
    n_ = np.arange(128)[:, None]
    q_ = np.arange(512)[None, :]
    for hi, h in enumerate(heads4):
        for dl in range(6):
            dist = 512 * dl + q_ - 16 * n_ - 31
            bc[:, hi, dl, :] = np.where(dist >= 0, rel_bias[t5_bucket_np(dist), h], f(NEGM))
    b31 = np.broadcast_to(rel_bias[31, heads4][None, :], (128, 4)).astype(f)
    return dict(biasW=np.ascontiguousarray(bw), biasS=np.ascontiguousarray(bs), biasC=bc, b31c=np.ascontiguousarray(b31))


def nsa_consts():
    import ml_dtypes
    f = np.float32
    bf = ml_dtypes.bfloat16
    EXP = np.zeros((128, 64, 128), f)
    for kk in range(64):
        EXP[2 * kk, kk, 0:64] = 1.0
        EXP[2 * kk + 1, kk, 64:128] = 1.0
    ov = np.zeros((1024, 256), f)
    for n in range(1023):
        for j in range(max(0, (16 * n) // 64 - 1), min(256, (16 * n + 32) // 64 + 1)):
            o = min(16 * n + 32, 64 * j + 64) - max(16 * n, 64 * j)
            if o > 0:
                ov[n, j] = o / 16.0
    ovx = np.zeros((128, 8, 257), f)
    ovx[:, :, :256] = ov.reshape(8, 128, 256).transpose(1, 0, 2)
    ovx[:, :, 256] = 1.0
    pmul = np.zeros((128, 3), f)
    padd = np.zeros((128, 3), f)
    padd[:64] = [1e6, 2e6, -1.0]
    pmul[64:, 0] = 1.0
    padd[64:] = [0.0, 1e6, 2e6]
    return dict(EXP=EXP.astype(bf), ovx=ovx.astype(bf), pmul=pmul, padd=padd, ident=np.eye(128, dtype=f))


NSA_IN = dict(q4T=([4, 128, L], BF16), kcmpT=([128, L], BF16), vcmpT=([128, L], BF16), kslcT=([128, L], BF16), kwinT=([128, L], BF16),
              vslc=([L, 128], BF16), vwin=([L, 128], BF16), gates3=([L, 3], F32),
              w1k=([4096, 256], F32), w1v=([4096, 256], F32), w2k=([256, 128], F32), w2v=([256, 128], F32),
              pekT=([128, 32], F32), pevT=([128, 32], F32),
              biasW=([128, 11 * 128], F32), biasS=([128, 14 * 128], F32), biasC=([128, 4, 6, 512], F32), b31c=([128, 4], F32),
              EXP=([128, 64, 128], BF16), ovx=([128, 8, 257], BF16), pmul=([128, 3], F32), padd=([128, 3], F32), ident=([128, 128], F32))


def emit_NSA(p, d, out_T, nq=NQG):
    pS = [p.ps(f"pS{i}", [128, 512]) for i in range(2)]
    pU = [p.ps(f"pU{i}", [128, 512]) for i in range(2)]
    pO = [p.ps(f"pO{i}", [128, 512]) for i in range(4)]

    def load(name, shape, dt=F32, q="sp"):
        b = p.sb("n_" + name, shape, dt)
        p.dma(q, b[:], d[name], writes=[b])
        return b

    idf = load("ident", [128, 128])
    idb = p.sb("n_idb", [128, 128], BF16)
    cp(p, "dve", idb, idb[:], idf, idf[:])
    pmul = load("pmul", [128, 3])
    padd = load("padd", [128, 3])
    b31c = load("b31c", [128, 4])
    kcT = p.sb("kcT", [128, 1024], BF16)
    RC = p.sb("RC", [128, 8, 385], BF16)
    p.dma("sp", RC[:, :, 0:257], d["ovx"], writes=[RC])
    biasC = p.sb("biasC", [128, 4, 6, 512], BF16)
    biasS = p.sb("biasS", [128, 14 * 128], BF16)
    biasW = p.sb("biasW", [128, 11 * 128], BF16)

    with p.scope():
        stg = [p.sb(f"tstg{i}", [128, 3072]) for i in range(2)]
        n = 0
        for hh in range(4):
            s = stg[n % 2]; n += 1
            p.dma("sp", s[:], d["biasC"][:, hh].rearrange("p a b -> p (a b)"), writes=[s])
            cp(p, ("dve", "pool")[n % 2], biasC, biasC[:, hh].rearrange("p a b -> p (a b)"), s, s[:])
        s = stg[n % 2]; n += 1
        p.dma("sp", s[:, 0:1792], d["biasS"], writes=[s])
        cp(p, "dve", biasS, biasS[:], s, s[:, 0:1792])
        s = stg[n % 2]; n += 1
        p.dma("sp", s[:, 0:1408], d["biasW"], writes=[s])
        cp(p, "pool", biasW, biasW[:], s, s[:, 0:1408])

        kext = p.sb("kext", [128, L + 32], BF16)
        kview = kext[:].rearrange("d (n s) -> d s n", s=16)
        w1b = p.sb("w1b", [128, 32, 256], BF16)
        w1s = [p.sb(f"w1s{i}", [128, 8, 256]) for i in range(2)]
        w2s = p.sb("w2s", [128, 2, 128])
        w2b = p.sb("w2b", [128, 2, 128], BF16)
        pes = p.sb("pes", [128, 32])
        peRep = p.sb("peRep", [128, 32, 128], BF16)
        biasH = p.sb("biasH", [128, 2])
        hTb = p.sb("hTb", [128, 2, 1024], BF16)
        mset(p, "pool", kext, kext[:, L:L + 32], 0.0)
        for which in range(2):
            kname, w1n, w2n, pen = (("kcmpT", "w1k", "w2k", "pekT"), ("vcmpT", "w1v", "w2v", "pevT"))[which]
            for c4 in range(4):
                p.dma("sp", kext[:, c4 * 4096:(c4 + 1) * 4096], d[kname][:, c4 * 4096:(c4 + 1) * 4096], writes=[kext])
            w1v_ = d[w1n].rearrange("(pos d) h -> d pos h", d=128)
            for c4 in range(4):
                s = w1s[c4 % 2]
                p.dma("sp", s[:], w1v_[:, c4 * 8:(c4 + 1) * 8, :], writes=[s])
                cp(p, ("dve", "pool")[c4 % 2], w1b, w1b[:, c4 * 8:(c4 + 1) * 8, :], s, s[:])
            p.dma("sp", w2s[:], d[w2n].rearrange("(c h) d -> h c d", h=128), writes=[w2s])
            cp(p, "dve", w2b, w2b[:], w2s, w2s[:])
            p.dma("sp", pes[:], d[pen], writes=[pes])
            for pos in range(32):
                cp(p, "dve", peRep, peRep[:, pos, :], pes, pes[:, pos:pos + 1].to_broadcast([128, 128]))
            for hc in range(2):
                ps = pS[hc % 2]
                for pos in range(32):
                    mm(p, ps, ps[:, 0:128], w1b, w1b[:, pos, hc * 128:(hc + 1) * 128], peRep, peRep[:, pos, :], start=(pos == 0), stop=(pos == 31))
                act(p, biasH, biasH[:, hc:hc + 1], ps, ps[:, 0:1], AF.Copy)
            for hc in range(2):
                for nh in range(2):
                    ps = pS[(hc * 2 + nh) % 2]
                    for pos in range(32):
                        n0 = pos // 16 + nh * 512
                        mm(p, ps, ps[:], w1b, w1b[:, pos, hc * 128:(hc + 1) * 128], kext, kview[:, pos % 16, n0:n0 + 512], start=(pos == 0), stop=(pos == 31))
                    act(p, hTb, hTb[:, hc, nh * 512:(nh + 1) * 512], ps, ps[:], AF.Gelu_apprx_tanh, bias=(biasH, biasH[:, hc:hc + 1]))
            if which == 0:
                for nh in range(2):
                    ps = pS[nh % 2]
                    for hc in range(2):
                        mm(p, ps, ps[:], w2b, w2b[:, hc, :], hTb, hTb[:, hc, nh * 512:(nh + 1) * 512], start=(hc == 0), stop=(hc == 1))
                    cp(p, "act", kcT, kcT[:, nh * 512:(nh + 1) * 512], ps, ps[:])
                mset(p, "dve", kcT, kcT[:, 1023:1024], 0.0)
            else:
                for m in range(8):
                    ps = pS[m % 2]
                    for hc in range(2):
                        mm(p, ps, ps[:, 0:128], hTb, hTb[:, hc, m * 128:(m + 1) * 128], w2b, w2b[:, hc, :], start=(hc == 0), stop=(hc == 1))
                    cp(p, "act", RC, RC[:, m, 257:385], ps, ps[:, 0:128])

    EXP = p.sb("EXPm", [128, 64, 128], BF16)
    p.dma("sp", EXP[:], d["EXP"], writes=[EXP])
    kslc = p.sb("kslc", [128, L], BF16)
    vslc1 = p.sb("vslc1", [128, 128, 129], BF16)
    mset(p, "pool", vslc1, vslc1[:, :, 128:129], 1.0)
    nld = max(1, (nq * 512) // 2048)
    vsv = d["vslc"].rearrange("(n p) d -> p n d", p=128)
    for c in range(nld):
        p.dma("sp", kslc[:, c * 2048:(c + 1) * 2048], d["kslcT"][:, c * 2048:(c + 1) * 2048], writes=[kslc])
        p.dma("sp", vslc1[:, c * 16:(c + 1) * 16, 0:128], vsv[:, c * 16:(c + 1) * 16, :], writes=[vslc1])
    q4s = [p.sb(f"q4_{i}", [128, 4, 512], BF16) for i in range(2)]
    kwins = [p.sb(f"kwin{i}", [128, 1024], BF16) for i in range(2)]
    vwins = [p.sb(f"vwin{i}", [128, 8, 129], BF16) for i in range(2)]
    gts = [p.sb(f"gts{i}", [128, 4, 3]) for i in range(2)]
    for i in range(2):
        mset(p, "pool", vwins[i], vwins[i][:, :, 128:129], 1.0)
    PcT = [p.sb(f"PcT{i}", [128, 8, 512], BF16) for i in range(2)]
    PsT = [p.sb(f"PsT{i}", [128, 512], BF16) for i in range(3)]
    sc = p.sb("scr_sc", [128, 4, 256])
    scz = p.sb("scr_z", [128, 256])
    acc = p.sb("accO", [128, 4, 128])
    maddf = p.sb("maddf", [128, 256])
    maddT = p.sb("maddT", [128, 2, 512], BF16)
    m8 = p.sb("m8", [128, 16])
    thr = p.sb("thr", [128, 1])
    rden = p.sb("rden", [128, 1])
    wsc = p.sb("wsc", [128, 1])
    ost = [p.sb(f"ost{i}", [128, 512]) for i in range(2)]
    q4v = d["q4T"].rearrange("h d t -> d h t")
    vwv = d["vwin"].rearrange("(n p) d -> p n d", p=128)
    gv = d["gates3"].rearrange("(n p) g -> p n g", p=128)
    nps = 0
    npt = 0

    for Q in range(nq):
        q4 = q4s[Q % 2]; kwin = kwins[Q % 2]; vwin = vwins[Q % 2]; gt = gts[Q % 2]
        p.dma("sp", q4[:], q4v[:, :, Q * 512:(Q + 1) * 512], writes=[q4])
        t0 = max(0, 4 * Q - 4)
        woff = t0 - (4 * Q - 4)
        p.dma("sp", kwin[:, woff * 128:1024], d["kwinT"][:, t0 * 128:(4 * Q + 4) * 128], writes=[kwin])
        p.dma("sp", vwin[:, woff:8, 0:128], vwv[:, t0:4 * Q + 4, :], writes=[vwin])
        p.dma("sp", gt[:], gv[:, 4 * Q:4 * Q + 4, :], writes=[gt])
        nm = Q // 4 + 1
        for hh in range(4):
            pc = PcT[hh % 2]
            for m in range(nm):
                dl = Q - 4 * m
                ps = pS[nps % 2]; nps += 1
                mm(p, ps, ps[:], kcT, kcT[:, m * 128:(m + 1) * 128], q4, q4[:, hh, :], start=True, stop=(dl >= 6))
                if dl < 6:
                    mm(p, ps, ps[:], idb, idb[:], biasC, biasC[:, hh, dl, :], start=False, stop=True)
                    act(p, pc, pc[:, m, :], ps, ps[:], AF.Exp)
                else:
                    act(p, pc, pc[:, m, :], ps, ps[:], AF.Exp, bias=(b31c, b31c[:, hh:hh + 1]))
            for i in range(4):
                pu = pU[(hh * 4 + i) % 2]
                N = 385 if hh == 0 else 257
                for m in range(nm):
                    mm(p, pu, pu[:, 0:N], pc, pc[:, m, i * 128:(i + 1) * 128], RC, RC[:, m, 0:N], start=(m == 0), stop=(m == nm - 1))
                ts(p, "dve", rden, rden[:], pu, pu[:, 256:257], 1e-30, None, op0=ALU.add)
                p.op("dve", lambda e: e.reciprocal(out=rden[:], in_=rden[:]), reads=[rden], writes=[rden])
                if hh == 0:
                    ts(p, "dve", sc, sc[:, i, :], pu, pu[:, 0:256], (rden, rden[:, 0:1]), None, op0=ALU.mult)
                    tt(p, "dve", wsc, wsc[:], rden, rden[:], gt, gt[:, i, 0:1], ALU.mult)
                    ts(p, "dve", acc, acc[:, i, :], pu, pu[:, 257:385], (wsc, wsc[:, 0:1]), None, op0=ALU.mult)
                else:
                    stt(p, "dve", sc, sc[:, i, :], pu, pu[:, 0:256], (rden, rden[:, 0:1]), sc, sc[:, i, :], ALU.mult, ALU.add)
        for i in range(4):
            T = 4 * Q + i
            if 2 * T + 2 < 256:
                mset(p, "dve", sc, sc[:, i, 2 * T + 2:256], -1.0)
            if T >= 1:
                a = 2 * T - 1
                tt(p, "dve", sc, sc[:, i, a:a + 3], sc, sc[:, i, a:a + 3], pmul, pmul[:], ALU.mult)
                tt(p, "dve", sc, sc[:, i, a:a + 3], sc, sc[:, i, a:a + 3], padd, padd[:], ALU.add)
            else:
                tt(p, "dve", sc, sc[:, i, 0:2], sc, sc[:, i, 0:2], pmul, pmul[:, 1:3], ALU.mult)
                tt(p, "dve", sc, sc[:, i, 0:2], sc, sc[:, i, 0:2], padd, padd[:, 1:3], ALU.add)
            mset(p, "dve", sc, sc[:, i, 0:1], 3e6)
            p.op("dve", lambda e, i=i: e.max(out=m8[:, 0:8], in_=sc[:, i, :]), reads=[sc], writes=[m8])
            p.op("dve", lambda e, i=i: e.match_replace(out=scz[:], in_to_replace=m8[:, 0:8], in_values=sc[:, i, :], imm_value=-2.0),
                 reads=[sc, m8], writes=[scz])
            p.op("dve", lambda e: e.max(out=m8[:, 8:16], in_=scz[:]), reads=[scz], writes=[m8])
            ts(p, "dve", thr, thr[:], m8, m8[:, 15:16], 0.0, None, op0=ALU.max)
            ts(p, "dve", maddf, maddf[:], sc, sc[:, i, :], (thr, thr[:, 0:1]), None, op0=ALU.is_ge)
            ts(p, "dve", maddf, maddf[:], maddf, maddf[:], -NEGM, NEGM, op0=ALU.mult, op1=ALU.add)
            for hf in range(2):
                pt = pU[npt % 2]; npt += 1
                tr(p, pt, pt[:, 0:128], maddf, maddf[:, hf * 128:(hf + 1) * 128], idf, idf[:])
                cp(p, "dve", maddT, maddT[:, hf, i * 128:(i + 1) * 128], pt, pt[:, 0:128])
        for kt in range(4 * Q + 4):
            i0 = max(0, kt - 4 * Q)
            c0 = i0 * 128
            near = (4 * Q - kt) <= 7
            ps = pS[nps % 2]; nps += 1
            pst = PsT[nps % 3]
            mm(p, ps, ps[:, c0:512], kslc, kslc[:, kt * 128:(kt + 1) * 128], q4, q4[:, 0, c0:512], start=True, stop=False)
            mm(p, ps, ps[:, c0:512], EXP, EXP[:, kt % 64, :], maddT, maddT[:, kt // 64, c0:512], start=False, stop=(not near))
            if near:
                b0 = (4 * Q - kt + 3) * 128
                mm(p, ps, ps[:, c0:512], idb, idb[:], biasS, biasS[:, b0 + c0:b0 + 512], start=False, stop=True)
                act(p, pst, pst[:, c0:512], ps, ps[:, c0:512], AF.Exp)
            else:
                act(p, pst, pst[:, c0:512], ps, ps[:, c0:512], AF.Exp, bias=(b31c, b31c[:, 0:1]))
            for i in range(i0, 4):
                po = pO[i]
                mm(p, po, po[:, 0:129], pst, pst[:, i * 128:(i + 1) * 128], vslc1, vslc1[:, kt, :], start=(kt == 0), stop=(kt == 4 * Q + i))
        for i in range(4):
            po = pO[i]
            p.op("dve", lambda e, po=po: e.reciprocal(out=rden[:], in_=po[:, 128:129]), reads=[po], writes=[rden])
            tt(p, "dve", wsc, wsc[:], rden, rden[:], gt, gt[:, i, 1:2], ALU.mult)
            stt(p, "dve", acc, acc[:, i, :], po, po[:, 0:128], (wsc, wsc[:, 0:1]), acc, acc[:, i, :], ALU.mult, ALU.add)
        for kt in range(t0, 4 * Q + 4):
            i0 = max(0, kt - 4 * Q)
            i1 = min(3, kt + 4 - 4 * Q)
            c0, c1 = i0 * 128, (i1 + 1) * 128
            slot = kt - (4 * Q - 4)
            ps = pS[nps % 2]; nps += 1
            pst = PsT[nps % 3]
            b0 = (4 * Q - kt + 3) * 128
            mm(p, ps, ps[:, c0:c1], kwin, kwin[:, slot * 128:(slot + 1) * 128], q4, q4[:, 0, c0:c1], start=True, stop=False)
            mm(p, ps, ps[:, c0:c1], idb, idb[:], biasW, biasW[:, b0 + c0:b0 + c1], start=False, stop=True)
            act(p, pst, pst[:, c0:c1], ps, ps[:, c0:c1], AF.Exp)
            for i in range(i0, i1 + 1):
                po = pO[i]
                mm(p, po, po[:, 0:129], pst, pst[:, i * 128:(i + 1) * 128], vwin, vwin[:, slot, :],
                   start=(kt == max(0, 4 * Q + i - 4)), stop=(kt == 4 * Q + i))
        o = ost[Q % 2]
        for i in range(4):
            po = pO[i]
            p.op("dve", lambda e, po=po: e.reciprocal(out=rden[:], in_=po[:, 128:129]), reads=[po], writes=[rden])
            tt(p, "dve", wsc, wsc[:], rden, rden[:], gt, gt[:, i, 2:3], ALU.mult)
            stt(p, "dve", acc, acc[:, i, :], po, po[:, 0:128], (wsc, wsc[:, 0:1]), acc, acc[:, i, :], ALU.mult, ALU.add)
            pt = pU[npt % 2]; npt += 1
            tr(p, pt, pt[:, 0:128], acc, acc[:, i, :], idf, idf[:])
            cp(p, "dve", o, o[:, i * 128:(i + 1) * 128], pt, pt[:, 0:128])
        p.dma("act", out_T[:, Q * 512:(Q + 1) * 512], o[:], reads=[o])


def build_NSA(nq=NQG):
    nc = bass.Bass("TRN2", target_bir_lowering=False)
    d = {k: nc.dram_tensor(k, s, dt, kind="ExternalInput").ap() for k, (s, dt) in NSA_IN.items()}
    out_T = nc.dram_tensor("yattT_o", [128, L], F32, kind="ExternalOutput").ap()
    with ExitStack() as st:
        p = Prog(nc, st)
        emit_NSA(p, d, out_T, nq)
        p.finish()
        p.replay()
        print("NSA instrs", p.ninstr())
    return nc


def nsa_host_inputs(inp, A, j):
    g = j // 4
    heads4 = [j] + [h for h in range(4 * g, 4 * g + 4) if h != j]
    fm = A["fm"]
    m = {}
    m["q4T"] = np.ascontiguousarray(np.stack([fm[h * 128:(h + 1) * 128] for h in heads4]))
    base = 1024
    m["kcmpT"] = np.ascontiguousarray(fm[base + (0 + g) * 128: base + (1 + g) * 128])
    m["vcmpT"] = np.ascontiguousarray(fm[base + (2 + g) * 128: base + (3 + g) * 128])
    m["kslcT"] = np.ascontiguousarray(fm[base + (4 + g) * 128: base + (5 + g) * 128])
    m["kwinT"] = np.ascontiguousarray(fm[base + (6 + g) * 128: base + (7 + g) * 128])
    m["vslc"] = np.ascontiguousarray(A["vtm"][:, g * 128:(g + 1) * 128])
    m["vwin"] = np.ascontiguousarray(A["vtm"][:, 256 + g * 128:256 + (g + 1) * 128])
    m["gates3"] = np.ascontiguousarray(A["gates"].reshape(L, 3, 8)[:, :, j])
    m["w1k"] = inp["cmp_w1_k"][0]; m["w1v"] = inp["cmp_w1_v"][0]; m["w2k"] = inp["cmp_w2_k"][0]; m["w2v"] = inp["cmp_w2_v"][0]
    m["pekT"] = np.ascontiguousarray(inp["cmp_pe_k"][0].T); m["pevT"] = np.ascontiguousarray(inp["cmp_pe_v"][0].T)
    m.update(nsa_tables(inp["rel_bias"], heads4))
    m.update(nsa_consts())
    return m


DS = 1024


def build_C1():
    nc = bass.Bass("TRN2", target_bir_lowering=False)
    I = {}
    def din(name, shape, dt=F32):
        I[name] = nc.dram_tensor(name, shape, dt, kind="ExternalInput").ap()
    din("x", [TPC, D]); din("ypreT", [DS, TPC]); din("yattT", [DS, TPC])
    din("g1row", [128, D]); din("sh2T", [128, 16]); din("sc2T", [128, 16])
    din("n2T", [128, 16]); din("wglu", [DS, DS]); din("bgluT", [128, 8]); din("bsT", [128, 8]); din("baT", [128, 8])
    din("wout", [2 * DS, D]); din("ident", [128, 128])
    O = {}
    O["x1_o"] = nc.dram_tensor("x1_o", [TPC, D], F32, kind="ExternalOutput").ap()
    O["h2T_o"] = nc.dram_tensor("h2T_o", [D, TPC], BF16, kind="ExternalOutput").ap()
    with ExitStack() as st:
        p = Prog(nc, st)
        emit_C1(p, I, O)
        p.finish()
        p.replay()
        print("C1 instrs", p.ninstr())
    return nc


def emit_C1(p, I, O):
    ptr = [p.ps(f"ptr{i}", [128, 1024], BF16) for i in range(2)]
    pg = p.ps("pg", [128, 512])
    pA = p.ps("pA", [128, 512])
    pB = p.ps("pB", [128, 512])
    pq = p.ps("pq", [128, 512])

    idf = p.sb("idf", [128, 128]); idb = p.sb("idb", [128, 128], BF16)
    p.dma("sp", idf[:], I["ident"], writes=[idf])
    cp(p, "dve", idb, idb[:], idf, idf[:])
    g1row = p.sb("g1row", [128, D])
    p.dma("sp", g1row[:], I["g1row"], writes=[g1row])
    sh = p.sb("sh2", [128, 16]); sc2 = p.sb("sc2", [128, 16])
    p.dma("sp", sh[:], I["sh2T"], writes=[sh]); p.dma("sp", sc2[:], I["sc2T"], writes=[sc2])
    ng = p.sb("ng2", [128, 16]); gam = p.sb("gam2", [128, 16])
    p.dma("sp", ng[:], I["n2T"], writes=[ng])
    stt(p, "dve", gam, gam[:], sc2, sc2[:], 1.0, ng, ng[:], ALU.add, ALU.mult)
    bglu = p.sb("bglu", [128, 8]); bsT = p.sb("bsT", [128, 8]); baT = p.sb("baT", [128, 8])
    p.dma("sp", bglu[:], I["bgluT"], writes=[bglu]); p.dma("sp", bsT[:], I["bsT"], writes=[bsT]); p.dma("sp", baT[:], I["baT"], writes=[baT])
    ones = p.sb("ones32", [128, 32])
    mset(p, "dve", ones, ones[:], 1.0)

    woutb = p.sb("woutb", [128, 16, D], BF16)
    wglub = p.sb("wglub", [128, 8, DS], BF16)
    with p.scope():
        stg = [p.sb(f"wstg{i}", [128, D]) for i in range(2)]
        for kc in range(16):
            s = stg[kc % 2]
            p.dma("sp", s[:], I["wout"][kc * 128:(kc + 1) * 128, :], writes=[s])
            cp(p, ("dve", "pool")[kc % 2], woutb, woutb[:, kc, :], s, s[:])
        for kc in range(8):
            s = stg[kc % 2]
            p.dma("sp", s[:, 0:DS], I["wglu"][kc * 128:(kc + 1) * 128, :], writes=[s])
            cp(p, ("dve", "pool")[kc % 2], wglub, wglub[:, kc, :], s, s[:, 0:DS])

    ybuf = p.sb("ybuf", [128, 8, 512])
    zf = p.sb("zf", [128, 8, 512])
    zb = p.sb("zb", [128, 8, 512], BF16)
    sq = p.sb("sqb", [128, 8, 512])
    sig = p.sb("sig", [128, 512])
    s5ob = p.sb("s5ob", [128, 8, 512], BF16)
    attb = p.sb("attb", [128, 8, 512], BF16)
    rs = p.sb("rs_s", [128, 4]); ra = p.sb("rs_a", [128, 4])
    xt = p.sb("xt", [128, D]); x1 = p.sb("x1", [128, D])
    t1 = p.sb("t1c", [128, 512])
    scr = p.sb("sqscr", [128, D], BF16); xn = p.sb("xn", [128, D], BF16)
    ssq = p.sb("ssq", [128, 1]); rstd = p.sb("rstd", [128, 1])
    hT = p.sb("hT", [128, 16, 512], BF16)
    hTk = [p.view(f"hT_{kc}", hT[:, kc, :]) for kc in range(16)]
    ypv = I["ypreT"].rearrange("(c p) t -> p c t", p=128)
    yav = I["yattT"].rearrange("(c p) t -> p c t", p=128)
    h2v = O["h2T_o"].rearrange("(c p) t -> p c t", p=128)

    def rms_rstd(src, dst):
        tt(p, "pool", sq, sq[:], src, src[:], src, src[:], ALU.mult)
        for i in range(4):
            for cc in range(8):
                mm(p, pq, pq[:, 0:32], sq, sq[:, cc, i * 128:(i + 1) * 128], ones, ones[:], start=(cc == 0), stop=(cc == 7))
            ts(p, "dve", dst, dst[:, i:i + 1], pq, pq[:, 0:1], 1.0 / DS, 1e-6, op0=ALU.mult, op1=ALU.add)
        p.op("act", lambda e: e.sqrt(out=dst[:], in_=dst[:]), reads=[dst], writes=[dst])
        p.op("dve", lambda e: e.reciprocal(out=dst[:], in_=dst[:]), reads=[dst], writes=[dst])

    for tg in range(TPC // 512):
        ts_ = slice(tg * 512, (tg + 1) * 512)
        p.dma("sp", ybuf[:], ypv[:, :, ts_], writes=[ybuf])
        act(p, zf, zf[:], ybuf, ybuf[:], AF.Gelu_apprx_tanh)
        cp(p, "pool", zb, zb[:], zf, zf[:])
        for cc in range(8):
            for kc in range(8):
                mm(p, pg, pg[:], wglub, wglub[:, kc, cc * 128:(cc + 1) * 128], zb, zb[:, kc, :], start=(kc == 0), stop=(kc == 7))
            act(p, sig, sig[:], pg, pg[:], AF.Sigmoid, bias=(bglu, bglu[:, cc:cc + 1]))
            tt(p, "dve", zf, zf[:, cc, :], zf, zf[:, cc, :], sig, sig[:], ALU.mult)
        rms_rstd(zf, rs)
        for cc in range(8):
            ts(p, "dve", s5ob, s5ob[:, cc, :], zf, zf[:, cc, :], (bsT, bsT[:, cc:cc + 1]), None, op0=ALU.mult)
        p.dma("sp", ybuf[:], yav[:, :, ts_], writes=[ybuf])
        rms_rstd(ybuf, ra)
        for cc in range(8):
            ts(p, "dve", attb, attb[:, cc, :], ybuf, ybuf[:, cc, :], (baT, baT[:, cc:cc + 1]), None, op0=ALU.mult)
        for i in range(4):
            t = tg * 4 + i
            p.dma("sp", xt[:], I["x"][t * 128:(t + 1) * 128, :], writes=[xt])
            for cg in range(4):
                cs = slice(cg * 512, (cg + 1) * 512)
                for c in range(8):
                    mm(p, pA, pA[:], s5ob, s5ob[:, c, i * 128:(i + 1) * 128], woutb, woutb[:, c, cs], start=(c == 0), stop=(c == 7))
                for c in range(8):
                    mm(p, pB, pB[:], attb, attb[:, c, i * 128:(i + 1) * 128], woutb, woutb[:, 8 + c, cs], start=(c == 0), stop=(c == 7))
                ts(p, "dve", t1, t1[:], pA, pA[:], (rs, rs[:, i:i + 1]), None, op0=ALU.mult)
                stt(p, "dve", t1, t1[:], pB, pB[:], (ra, ra[:, i:i + 1]), t1, t1[:], ALU.mult, ALU.add)
                tt(p, "pool", t1, t1[:], t1, t1[:], g1row, g1row[:, cs], ALU.mult)
                tt(p, "pool", x1, x1[:, cs], t1, t1[:], xt, xt[:, cs], ALU.add)
            p.dma("act", O["x1_o"][t * 128:(t + 1) * 128, :], x1[:], reads=[x1])
            emit_norm_transpose(p, x1, hT, hTk, i * 128, gam, sh, idb, scr, ssq, rstd, xn, ptr)
        p.dma("act", h2v[:, :, ts_], hT[:], reads=hTk)


NE = 16384
EG = 256
NEG_ = NE // EG
GTH_RELAX = 1.0 - 1e-4


def build_C2(npass=4, neg=NEG_):
    nc = bass.Bass("TRN2", target_bir_lowering=False)
    I = {}
    def din(name, shape, dt=F32):
        I[name] = nc.dram_tensor(name, shape, dt, kind="ExternalInput").ap()
    din("x1", [TPC, D]); din("h2T", [D, TPC], BF16); din("g2row", [128, D]); din("fgrow", [128, D])
    din("wq", [D, D]); din("k1T", [128, 128]); din("k2T", [128, 128]); din("pu", [NE, D]); din("pv", [NE, D]); din("ident", [128, 128])
    out = nc.dram_tensor("out", [TPC, D], F32, kind="ExternalOutput").ap()
    with ExitStack() as st:
        p = Prog(nc, st)
        emit_C2(p, I, out, npass, neg)
        p.finish()
        p.replay()
        print("C2 instrs", p.ninstr())
    return nc


def emit_C2(p, I, out, npass=4, neg=NEG_):
    psc = p.ps("psc", [128, 512])
    ptr = p.ps("ptrU", [128, 1024], BF16)
    ptw = p.ps("ptrW", [128, 1024], BF16)
    pv = [p.ps(f"pv{i}", [128, 512]) for i in range(4)]

    idf = p.sb("idf", [128, 128]); idb = p.sb("idb", [128, 128], BF16)
    p.dma("sp", idf[:], I["ident"], writes=[idf])
    cp(p, "dve", idb, idb[:], idf, idf[:])
    k1b = p.sb("k1b", [128, 128], BF16); k2b = p.sb("k2b", [128, 128], BF16)
    kst = p.sb("kst", [128, 128])
    p.dma("sp", kst[:], I["k1T"], writes=[kst]); cp(p, "dve", k1b, k1b[:], kst, kst[:])
    p.dma("sp", kst[:], I["k2T"], writes=[kst]); cp(p, "dve", k2b, k2b[:], kst, kst[:])
    g2row = p.sb("g2row", [128, D]); fgrow = p.sb("fgrow", [128, D])
    p.dma("sp", g2row[:], I["g2row"], writes=[g2row]); p.dma("sp", fgrow[:], I["fgrow"], writes=[fgrow])

    h2T = p.sb("h2T", [128, 16, 512], BF16)
    acc = p.sb("acc", [128, 4, D])
    E2 = p.sb("E2", [128, 4, 8, 128]); Rg = p.sb("Rg", [128, 4, 8, 128]); gth = p.sb("gth", [128, 4, 8])
    h2v = I["h2T"].rearrange("(c p) t -> p c t", p=128)
    wqv = I["wq"].rearrange("(k p) n -> p k n", p=128)
    uv = I["pu"].rearrange("(g a p) d -> g p a d", a=2, p=128)
    vv = I["pv"].rearrange("(g a p) d -> g p a d", a=2, p=128)

    for ps_ in range(npass):
        tsl = slice(ps_ * 512, (ps_ + 1) * 512)
        p.dma("sp", h2T[:], h2v[:, :, tsl], writes=[h2T])
        with p.scope():
            qpT = p.sb("qpT", [128, 16, 512], BF16)
            wqs = [p.sb(f"wqs{i}", [128, 16, 128]) for i in range(2)]
            wqb = [p.sb(f"wqb{i}", [128, 16, 128], BF16) for i in range(2)]
            s12 = p.sb("s12", [128, 2, 128]); scz = p.sb("sczp", [128, 256]); m16 = p.sb("m16", [128, 2, 16])
            cand = p.sb("cand", [128, 16, 16]); c16 = p.sb("c16", [128, 16]); ez = p.sb("ez", [128, 16])
            mm_ = p.sb("mmx", [128, 1]); th = p.sb("thx", [128, 1]); negm = p.sb("negm", [128, 1]); Z = p.sb("Zx", [128, 1]); lnZ = p.sb("lnZ", [128, 1])
            m1 = p.sb("m1x", [128, 1]); m2 = p.sb("m2x", [128, 1]); b1 = p.sb("b1x", [128, 1]); b2 = p.sb("b2x", [128, 1]); bg = p.sb("bgx", [128, 1])
            for ch in range(16):
                s = wqs[ch % 2]; sb_ = wqb[ch % 2]
                p.dma("sp", s[:], wqv[:, :, ch * 128:(ch + 1) * 128], writes=[s])
                cp(p, "pool", sb_, sb_[:], s, s[:])
                for kc in range(16):
                    mm(p, psc, psc[:], sb_, sb_[:, kc, :], h2T, h2T[:, kc, :], start=(kc == 0), stop=(kc == 15))
                cp(p, "act", qpT, qpT[:, ch, :], psc, psc[:])
            for i in range(4):
                for h in range(8):
                    mm(p, psc, psc[:, 0:128], qpT, qpT[:, 2 * h, i * 128:(i + 1) * 128], k1b, k1b[:])
                    mm(p, psc, psc[:, 128:256], qpT, qpT[:, 2 * h + 1, i * 128:(i + 1) * 128], k2b, k2b[:])
                    cp(p, "act", s12, s12[:].rearrange("p a b -> p (a b)"), psc, psc[:, 0:256])
                    for a in range(2):
                        p.op("dve", lambda e, a=a: e.max(out=m16[:, a, 0:8], in_=s12[:, a, :]), reads=[s12], writes=[m16])
                        p.op("dve", lambda e, a=a: e.match_replace(out=scz[:, 0:128], in_to_replace=m16[:, a, 0:8], in_values=s12[:, a, :], imm_value=-1e30),
                             reads=[s12, m16], writes=[scz])
                        p.op("dve", lambda e, a=a: e.max(out=m16[:, a, 8:16], in_=scz[:, 0:128]), reads=[scz], writes=[m16])
                    tt(p, "dve", cand, cand[:], m16, m16[:, 0, :, None].to_broadcast([128, 16, 16]), m16, m16[:, 1, None, :].to_broadcast([128, 16, 16]), ALU.add)
                    cflat = cand[:].rearrange("p a b -> p (a b)")
                    p.op("dve", lambda e: e.max(out=c16[:, 0:8], in_=cflat), reads=[cand], writes=[c16])
                    p.op("dve", lambda e: e.match_replace(out=scz[:], in_to_replace=c16[:, 0:8], in_values=cflat, imm_value=-1e30), reads=[cand, c16], writes=[scz])
                    p.op("dve", lambda e: e.max(out=c16[:, 8:16], in_=scz[:]), reads=[scz], writes=[c16])
                    p.op("dve", lambda e: e.tensor_reduce(out=mm_[:], in_=c16[:, 0:8], axis=AX.X, op=ALU.max), reads=[c16], writes=[mm_])
                    p.op("dve", lambda e: e.tensor_reduce(out=th[:], in_=c16[:, 8:16], axis=AX.X, op=ALU.min), reads=[c16], writes=[th])
                    p.op("dve", lambda e: e.tensor_reduce(out=m1[:], in_=m16[:, 0, 0:8], axis=AX.X, op=ALU.max), reads=[m16], writes=[m1])
                    p.op("dve", lambda e: e.tensor_reduce(out=m2[:], in_=m16[:, 1, 0:8], axis=AX.X, op=ALU.max), reads=[m16], writes=[m2])
                    ts(p, "dve", negm, negm[:], mm_, mm_[:], -1.0, None, op0=ALU.mult)
                    act(p, ez, ez[:], c16, c16[:], AF.Exp, bias=(negm, negm[:, 0:1]), accum=(Z, Z[:]))
                    act(p, lnZ, lnZ[:], Z, Z[:], AF.Ln)
                    ts(p, "dve", b2, b2[:], m2, m2[:], -1.0, None, op0=ALU.mult)
                    stt(p, "dve", b1, b1[:], m1, m1[:], -1.0, lnZ, lnZ[:], ALU.mult, ALU.subtract)
                    tt(p, "dve", bg, bg[:], th, th[:], mm_, mm_[:], ALU.subtract)
                    tt(p, "dve", bg, bg[:], bg, bg[:], lnZ, lnZ[:], ALU.subtract)
                    act(p, E2, E2[:, i, h, :], s12, s12[:, 1, :], AF.Exp, bias=(b2, b2[:, 0:1]))
                    act(p, Rg, Rg[:, i, h, :], s12, s12[:, 0, :], AF.Exp, bias=(b1, b1[:, 0:1]))
                    act(p, gth, gth[:, i, h:h + 1], bg, bg[:], AF.Exp)
            ts(p, "dve", gth, gth[:], gth, gth[:], GTH_RELAX, None, op0=ALU.mult)
        with p.scope():
            ust = p.sb("ust", [128, 2, D]); vst = p.sb("vst", [128, 2, D])
            ub = p.sb("ub", [128, 2, D], BF16); vb = p.sb("vb", [128, 2, D], BF16)
            uT = p.sb("uT", [128, 16, EG], BF16)
            A = p.sb("Agelu", [128, 2, 128])
            G = p.sb("Ggrid", [128, 8, 2, 128]); M = p.sb("Mgrid", [128, 8, 2, 128])
            Wg = p.sb("Wg", [128, 2, 128])
            WA = p.sb("WA", [128, 2, 128], BF16)
            WAT = p.sb("WAT", [128, 2, 128], BF16)
            for eg in range(neg):
                p.dma("sp", ust[:], uv[eg], writes=[ust])
                cp(p, "pool", ub, ub[:], ust, ust[:])
                for a in range(2):
                    for half in range(2):
                        for k8 in range(8):
                            kc = half * 8 + k8
                            tr(p, ptr, ptr[:, k8 * 128:(k8 + 1) * 128], ub, ub[:, a, kc * 128:(kc + 1) * 128], idb, idb[:])
                        cp(p, "act", uT, uT[:, half * 8:(half + 1) * 8, a * 128:(a + 1) * 128], ptr, ptr[:].rearrange("p (k e) -> p k e", e=128))
                p.dma("sp", vst[:], vv[eg], writes=[vst])
                cp(p, "pool", vb, vb[:], vst, vst[:])
                for i in range(4):
                    for kc in range(16):
                        mm(p, psc, psc[:, 0:EG], h2T, h2T[:, kc, i * 128:(i + 1) * 128], uT, uT[:, kc, :], start=(kc == 0), stop=(kc == 15))
                    act(p, A, A[:].rearrange("p a b -> p (a b)"), psc, psc[:, 0:EG], AF.Gelu_apprx_tanh)
                    tt(p, "pool", G, G[:], Rg, Rg[:, i, :, 2 * eg:2 * eg + 2, None].to_broadcast([128, 8, 2, 128]),
                       E2, E2[:, i, :, None, :].to_broadcast([128, 8, 2, 128]), ALU.mult)
                    tt(p, "dve", M, M[:], G, G[:], gth, gth[:, i, :, None, None].to_broadcast([128, 8, 2, 128]), ALU.is_ge)
                    tt(p, "pool", M, M[:], M, M[:], G, G[:], ALU.mult)
                    p.op("dve", lambda e: e.tensor_reduce(out=Wg[:], in_=M[:].rearrange("p h a b -> p a b h"), axis=AX.X, op=ALU.add), reads=[M], writes=[Wg])
                    tt(p, "dve", WA, WA[:], A, A[:], Wg, Wg[:], ALU.mult)
                    for a in range(2):
                        tr(p, ptw, ptw[:, a * 128:(a + 1) * 128], WA, WA[:, a, :], idb, idb[:])
                    cp(p, "act", WAT, WAT[:].rearrange("p a b -> p (a b)"), ptw, ptw[:, 0:256])
                    for cg in range(4):
                        for a in range(2):
                            mm(p, pv[cg], pv[cg][:], WAT, WAT[:, a, :], vb, vb[:, a, cg * 512:(cg + 1) * 512], start=(a == 0), stop=(a == 1))
                        if eg == 0:
                            cp(p, "dve", acc, acc[:, i, cg * 512:(cg + 1) * 512], pv[cg], pv[cg][:])
                        else:
                            tt(p, "dve", acc, acc[:, i, cg * 512:(cg + 1) * 512], pv[cg], pv[cg][:], acc, acc[:, i, cg * 512:(cg + 1) * 512], ALU.add)
        with p.scope():
            x1t = p.sb("x1t", [128, D]); x2 = p.sb("x2", [128, D]); scr = p.sb("fscr", [128, D], BF16)
            ssq = p.sb("fssq", [128, 1]); rstd = p.sb("frstd", [128, 1])
            for i in range(4):
                t = ps_ * 4 + i
                p.dma("sp", x1t[:], I["x1"][t * 128:(t + 1) * 128, :], writes=[x1t])
                tt(p, "pool", x2, x2[:], acc, acc[:, i, :], g2row, g2row[:], ALU.mult)
                tt(p, "pool", x2, x2[:], x2, x2[:], x1t, x1t[:], ALU.add)
                act(p, scr, scr[:], x2, x2[:], AF.Square, accum=(ssq, ssq[:]))
                ts(p, "dve", rstd, rstd[:], ssq, ssq[:], 1.0 / D, 1e-6, op0=ALU.mult, op1=ALU.add)
                p.op("act", lambda e: e.sqrt(out=rstd[:], in_=rstd[:]), reads=[rstd], writes=[rstd])
                p.op("dve", lambda e: e.reciprocal(out=rstd[:], in_=rstd[:]), reads=[rstd], writes=[rstd])
                ts(p, "dve", x2, x2[:], x2, x2[:], (rstd, rstd[:, 0:1]), None, op0=ALU.mult)
                tt(p, "pool", x2, x2[:], x2, x2[:], fgrow, fgrow[:], ALU.mult)
                p.dma("act", out[t * 128:(t + 1) * 128, :], x2[:], reads=[x2])


def _ac(a, dt=None):
    return np.ascontiguousarray(a if dt is None else np.asarray(a, dtype=dt))


def _run(nc, maps):
    return run_bass_kernel_spmd(nc, maps, core_ids=list(range(len(maps)))).results


def kernel(**inp):
    f = np.float32
    NCR = 8
    ident = np.eye(128, dtype=f)
    x = inp["x"][0]
    cTl = _ac(inp["c"][0].reshape(16, 128).T)
    T16 = lambda v: _ac(np.asarray(v).reshape(16, 128).T)
    wa = inp["w_ada"][0]
    ba = inp["b_ada"][0]
    m0 = [dict(cT=cTl, wada=_ac(wa[:, c * NC0:(c + 1) * NC0]), badaT=_ac(ba[c * NC0:(c + 1) * NC0].reshape(NC0 // 128, 128).T), ident=ident)
          for c in range(NCR)]
    r0 = _run(build_L0(), m0)
    cond = np.concatenate([np.asarray(r["condT_o"]).T.reshape(-1) for r in r0])
    sh1, sc1, g1, sh2, sc2, g2 = np.split(cond, 6)
    mA = [dict(x=_ac(x[c * TPC:(c + 1) * TPC]), n1T=T16(inp["norm1_g"][0]), sh1T=T16(sh1), sc1T=T16(sc1), w_in=inp["w_in"][0], ident=ident)
          for c in range(NCR)]
    rA = _run(build_A(), mA)
    fm = np.concatenate([np.asarray(r["fm_o"]) for r in rA], axis=1)
    uT = np.concatenate([np.asarray(r["uT_o"]) for r in rA], axis=1)
    vtm = np.concatenate([np.asarray(r["vtm_o"]) for r in rA], axis=0)
    gates = np.concatenate([np.asarray(r["gates_o"]) for r in rA], axis=0)
    del rA
    mS = [dict(s5_host_params(inp, j), uT=_ac(uT[128 * j:128 * (j + 1)])) for j in range(NCR)]
    rS = _run(build_S5(), mS)
    ypreT = np.concatenate([np.asarray(r["yT_o"]) for r in rS], axis=0)
    del rS, mS, uT
    Aout = dict(fm=fm, vtm=vtm, gates=gates)
    mN = [nsa_host_inputs(inp, Aout, j) for j in range(NCR)]
    rN = _run(build_NSA(), mN)
    yattT = np.concatenate([np.asarray(r["yattT_o"]) for r in rN], axis=0)
    del rN, mN, fm, vtm, gates, Aout
    g1row = _ac(np.broadcast_to(g1[None, :], (128, D)), f)
    mC1 = []
    for c in range(NCR):
        sl = slice(c * TPC, (c + 1) * TPC)
        mC1.append(dict(x=_ac(x[sl]), ypreT=_ac(ypreT[:, sl]), yattT=_ac(yattT[:, sl]), g1row=g1row, sh2T=T16(sh2), sc2T=T16(sc2),
                        n2T=T16(inp["norm2_g"][0]), wglu=inp["ssm_w_glu"][0], bgluT=_ac(inp["ssm_b_glu"][0].reshape(8, 128).T),
                        bsT=_ac(inp["beta_ssm"][0].reshape(8, 128).T), baT=_ac(inp["beta_attn"][0].reshape(8, 128).T),
                        wout=inp["w_out"][0], ident=ident))
    rC1 = _run(build_C1(), mC1)
    del mC1, ypreT, yattT
    g2row = _ac(np.broadcast_to(g2[None, :], (128, D)), f)
    fgrow = _ac(np.broadcast_to(inp["final_g"][None, :], (128, D)), f)
    k1T = _ac(inp["peer_k1"][0].T)
    k2T = _ac(inp["peer_k2"][0].T)
    mC2 = [dict(x1=np.asarray(rC1[c]["x1_o"]), h2T=np.asarray(rC1[c]["h2T_o"]), g2row=g2row, fgrow=fgrow, wq=inp["peer_w_q"][0],
                k1T=k1T, k2T=k2T, pu=inp["peer_u"][0], pv=inp["peer_v"][0], ident=ident) for c in range(NCR)]
    rC2 = _run(build_C2(), mC2)
    out = np.concatenate([np.asarray(r["out"]) for r in rC2], axis=0)
    return out.reshape(1, L, D).astype(np.float32)
```

```python
import numpy as np
from contextlib import ExitStack
import concourse.bass as bass
import concourse.mybir as mybir
from concourse.bass_utils import run_bass_kernel_spmd

F32 = mybir.dt.float32
BF16 = mybir.dt.bfloat16
I32 = mybir.dt.int32
AF = mybir.ActivationFunctionType
ALU = mybir.AluOpType
AX = mybir.AxisListType


class Buf:
    __slots__ = ("name", "ap", "lw", "rd")

    def __init__(self, name, ap):
        self.name = name
        self.ap = ap
        self.lw = None
        self.rd = []

    def __getitem__(self, idx):
        return self.ap[idx]


class Prog:
    COMPUTE = ("pe", "act", "dve", "pool")
    ALL = ("pe", "act", "dve", "pool", "sp")
    NDSEM = 16

    def __init__(self, nc, stack):
        self.nc = nc
        self.st = stack
        self.rec = {e: [] for e in self.ALL}
        self.sem = {}
        self.cnt = {}
        for e in self.COMPUTE:
            self.sem[e] = stack.enter_context(nc.semaphore("s_" + e))
            self.cnt[e] = 0
        self.dsem = {}
        self.dcnt = {}
        self.dnext = {}
        for q in ("sp", "act", "pool"):
            self.dsem[q] = [stack.enter_context(nc.semaphore(f"d_{q}{i}")) for i in range(self.NDSEM)]
            self.dcnt[q] = [0] * self.NDSEM
            self.dnext[q] = 0
        self.waited = {e: {} for e in self.ALL}
        self.semobj = {}
        self.nbuf = 0

    def sb(self, name, shape, dt=F32):
        self.nbuf += 1
        t = self.st.enter_context(self.nc.sbuf_tensor(f"sb_{name}_{self.nbuf}", list(shape), dt))
        return Buf(name, t)

    def ps(self, name, shape, dt=F32):
        self.nbuf += 1
        t = self.st.enter_context(self.nc.psum_tensor(f"ps_{name}_{self.nbuf}", list(shape), dt))
        return Buf(name, t)

    def view(self, name, ap):
        return Buf(name, ap)

    def _events(self, reads, writes):
        evs = []
        for b in reads:
            if b.lw is not None:
                evs.append(b.lw)
        for b in writes:
            if b.lw is not None:
                evs.append(b.lw)
            evs.extend(b.rd)
        return evs

    def _emit_waits(self, e, evs):
        need = {}
        for (sk, v) in evs:
            if sk == e and e == "pe":
                continue
            if self.waited[e].get(sk, 0) < v:
                if need.get(sk, 0) < v:
                    need[sk] = v
        for sk, v in need.items():
            self.waited[e][sk] = v
            self.rec[e].append(("w", sk, v))

    def _semof(self, sk):
        if isinstance(sk, str):
            return self.sem[sk]
        q, i = sk
        return self.dsem[q][i]

    def _mark(self, ev, reads, writes):
        for b in writes:
            b.lw = ev
            b.rd = []
        for b in reads:
            if b not in writes:
                b.rd.append(ev)
                if len(b.rd) > 64:
                    last = {}
                    for (sk, v) in b.rd:
                        if last.get(sk, 0) < v:
                            last[sk] = v
                    b.rd = list(last.items())

    def op(self, e, fn, reads=(), writes=()):
        reads = list(reads)
        writes = list(writes)
        self._emit_waits(e, self._events(reads, writes))
        self.cnt[e] += 1
        ev = (e, self.cnt[e])
        self.rec[e].append(("o", fn, e, 1))
        self._mark(ev, reads, writes)
        return ev

    def dma(self, q, out_ap, in_ap, reads=(), writes=(), **kw):
        reads = list(reads)
        writes = list(writes)
        i = self.dnext[q]
        evs = self._events(reads, writes)
        if self.dcnt[q][i]:
            evs.append(((q, i), self.dcnt[q][i]))
        self._emit_waits(q, evs)
        self.dnext[q] = (i + 1) % self.NDSEM
        self.dcnt[q][i] += 16
        ev = ((q, i), self.dcnt[q][i])
        self.rec[q].append(("o", lambda eng: eng.dma_start(out=out_ap, in_=in_ap, **kw), (q, i), 16))
        self._mark(ev, reads, writes)
        return ev

    def dmalike(self, q, fn, reads=(), writes=()):
        reads = list(reads)
        writes = list(writes)
        i = self.dnext[q]
        evs = self._events(reads, writes)
        if self.dcnt[q][i]:
            evs.append(((q, i), self.dcnt[q][i]))
        self._emit_waits(q, evs)
        self.dnext[q] = (i + 1) % self.NDSEM
        self.dcnt[q][i] += 16
        ev = ((q, i), self.dcnt[q][i])
        self.rec[q].append(("o", fn, (q, i), 16))
        self._mark(ev, reads, writes)
        return ev

    def barrier(self):
        evs = self._all_events()
        for e in self.ALL:
            self._emit_waits(e, evs)

    def scope(self):
        prog = self

        class _S:
            def __enter__(self_s):
                self_s.old = prog.st
                self_s.new = ExitStack()
                prog.st = self_s.new
                return self_s

            def __exit__(self_s, *a):
                prog.barrier()
                prog.st = self_s.old
                self_s.new.close()
                return False
        return _S()

    def _all_events(self):
        evs = []
        for q in self.dsem:
            for i in range(self.NDSEM):
                if self.dcnt[q][i]:
                    evs.append(((q, i), self.dcnt[q][i]))
        for e in self.COMPUTE:
            if self.cnt[e]:
                evs.append((e, self.cnt[e]))
        return evs

    def finish(self, waiter="sp"):
        evs = []
        for q in self.dsem:
            for i in range(self.NDSEM):
                if self.dcnt[q][i]:
                    evs.append(((q, i), self.dcnt[q][i]))
        for e in self.COMPUTE:
            if self.cnt[e]:
                evs.append((e, self.cnt[e]))
        self._emit_waits(waiter, evs)

    def replay(self):
        nc = self.nc

        def run(ename):
            def f(eng):
                for item in self.rec[ename]:
                    if item[0] == "w":
                        eng.wait_ge(self._semof(item[1]), item[2])
                    else:
                        ins = item[1](eng)
                        ins.then_inc(self._semof(item[2]), item[3])
            return f

        with nc.Block() as block:
            block.sync(run("sp"))
            block.tensor(run("pe"))
            block.scalar(run("act"))
            block.vector(run("dve"))
            block.gpsimd(run("pool"))

    def ninstr(self):
        return {e: len(self.rec[e]) for e in self.ALL}


def _bufs(*items):
    out = []
    for it in items:
        if it is None:
            continue
        if isinstance(it, Buf):
            out.append(it)
        elif isinstance(it, (list, tuple)):
            out.extend(_bufs(*it))
    return out


def _sc(x):
    if isinstance(x, tuple):
        return x[1], [x[0]]
    return x, []


def mm(p, ob, oap, lb, lap, rb, rap, start=True, stop=True):
    return p.op("pe", lambda e: e.matmul(oap, lhsT=lap, rhs=rap, start=start, stop=stop), reads=_bufs(lb, rb), writes=[ob])


def tr(p, ob, oap, ib, iap, idb, idap):
    return p.op("pe", lambda e: e.transpose(out=oap, in_=iap, identity=idap), reads=[ib, idb], writes=[ob])


def act(p, ob, oap, ib, iap, func, bias=0.0, scale=1.0, accum=None, eng="act"):
    bv, br = _sc(bias)
    sv, sr = _sc(scale)
    kw = {}
    wr = [ob]
    if accum is not None:
        kw["accum_out"] = accum[1]
        wr.append(accum[0])
    return p.op("act", lambda e: e.activation(out=oap, in_=iap, func=func, bias=bv, scale=sv, **kw), reads=[ib] + br + sr, writes=wr)


def tt(p, eng, ob, oap, ab, aap, bb, bap, op):
    return p.op(eng, lambda e: e.tensor_tensor(out=oap, in0=aap, in1=bap, op=op), reads=[ab, bb], writes=[ob])


def ts(p, eng, ob, oap, ib, iap, s1, s2=None, op0=ALU.mult, op1=None, accum=None):
    v1, r1 = _sc(s1)
    v2, r2 = _sc(s2) if s2 is not None else (None, [])
    kw = {}
    wr = [ob]
    if op1 is not None:
        kw["op1"] = op1
    if accum is not None:
        kw["accum_out"] = accum[1]
        wr.append(accum[0])
    return p.op(eng, lambda e: e.tensor_scalar(out=oap, in0=iap, scalar1=v1, scalar2=v2, op0=op0, **kw), reads=[ib] + r1 + r2, writes=wr)


def stt(p, eng, ob, oap, ab, aap, s, bb, bap, op0, op1):
    v, r = _sc(s)
    return p.op(eng, lambda e: e.scalar_tensor_tensor(out=oap, in0=aap, scalar=v, in1=bap, op0=op0, op1=op1), reads=[ab, bb] + r, writes=[ob])


def cp(p, eng, ob, oap, ib, iap):
    if eng == "act":
        return p.op("act", lambda e: e.copy(out=oap, in_=iap), reads=[ib], writes=[ob])
    return p.op(eng, lambda e: e.tensor_copy(out=oap, in_=iap), reads=[ib], writes=[ob])


def mset(p, eng, ob, oap, val):
    return p.op(eng, lambda e: e.memset(oap, val), writes=[ob])


L = 16384
D = 2048
NCORES = 8
TPC = L // NCORES
D_IN = 3608
FM_CHUNKS = list(range(16)) + [16, 17, 18, 19, 20, 21, 24, 25]
TM_COLS = [(22, 2), (26, 2)]
QSCALE = 128.0 ** -0.5


def build_A():
    nc = bass.Bass("TRN2", target_bir_lowering=False)
    x = nc.dram_tensor("x", [TPC, D], F32, kind="ExternalInput").ap()
    cT = None
    n1T = nc.dram_tensor("n1T", [128, 16], F32, kind="ExternalInput").ap()
    wada = nc.dram_tensor("sh1T", [128, 16], F32, kind="ExternalInput").ap()
    bada = nc.dram_tensor("sc1T", [128, 16], F32, kind="ExternalInput").ap()
    w_in = nc.dram_tensor("w_in", [D, D_IN], F32, kind="ExternalInput").ap()
    ident = nc.dram_tensor("ident", [128, 128], F32, kind="ExternalInput").ap()
    uT_o = nc.dram_tensor("uT_o", [1024, TPC], F32, kind="ExternalOutput").ap()
    fm_o = nc.dram_tensor("fm_o", [16 * 128, TPC], BF16, kind="ExternalOutput").ap()
    vtm_o = nc.dram_tensor("vtm_o", [TPC, 512], BF16, kind="ExternalOutput").ap()
    gates_o = nc.dram_tensor("gates_o", [TPC, 24], F32, kind="ExternalOutput").ap()

    with ExitStack() as st:
        p = Prog(nc, st)
        emit_A(p, x, cT, n1T, wada, bada, w_in, ident, uT_o, fm_o, vtm_o, gates_o)
        p.finish()
        p.replay()
        print("phase A instrs", p.ninstr())
    return nc


def emit_cond(p, cT, wada, badaT, ncols, pcond, condT, idf, R=None, dlo=0, nd=None):
    nch = ncols // 128
    if nd is None:
        nd = nch
    ct = p.sb("ct", [128, 16])
    sc = p.sb("silc", [128, 16])
    screp = p.sb("screp", [128, 16, 128])
    bT = p.sb("badaT", [128, nd])
    if R is None:
        R = p.sb("condR", [128, ncols])
    dg = p.sb("conddg", [128, nd, 128])
    slabs = [p.sb(f"wslab{i}", [128, 16, 512]) for i in range(2)]
    p.dma("sp", ct[:], cT, writes=[ct])
    p.dma("sp", bT[:], badaT, writes=[bT])
    act(p, sc, sc[:], ct, ct[:], AF.Silu)
    for kc in range(16):
        cp(p, "dve", screp, screp[:, kc, :], sc, sc[:, kc:kc + 1].to_broadcast([128, 128]))
    wv = wada.rearrange("(k p) n -> p k n", p=128)
    for cg in range(ncols // 512):
        sl = slabs[cg % 2]
        p.dma("sp", sl[:], wv[:, :, cg * 512:(cg + 1) * 512], writes=[sl])
        for kc in range(16):
            mm(p, pcond, pcond[:], screp, screp[:, kc, :], sl, sl[:, kc, :], start=(kc == 0), stop=(kc == 15))
        cp(p, "dve", R, R[:, cg * 512:(cg + 1) * 512], pcond, pcond[:])
    tt(p, "dve", dg, dg[:], R, R[:, dlo * 128:(dlo + nd) * 128].rearrange("p (c k) -> p c k", k=128), idf, idf[:, None, :].to_broadcast([128, nd, 128]), ALU.mult)
    p.op("dve", lambda e: e.tensor_reduce(out=condT[:, 0:nd], in_=dg[:], axis=AX.X, op=ALU.add), reads=[dg], writes=[condT])
    tt(p, "dve", condT, condT[:, 0:nd], condT, condT[:, 0:nd], bT, bT[:], ALU.add)
    return R


def emit_norm_transpose(p, xt, hT, hTk, tcol, gam, sh, idb, scr, ssq, rstd, xn, ptr, evac_engs=("dve", "act")):
    p.op("act", lambda e: e.activation(out=scr[:], in_=xt[:], func=AF.Square, accum_out=ssq[:]), reads=[xt], writes=[scr, ssq])
    p.op("dve", lambda e: e.tensor_scalar(out=rstd[:], in0=ssq[:], scalar1=1.0 / D, scalar2=1e-6, op0=ALU.mult, op1=ALU.add),
         reads=[ssq], writes=[rstd])
    p.op("act", lambda e: e.sqrt(out=rstd[:], in_=rstd[:]), reads=[rstd], writes=[rstd])
    p.op("dve", lambda e: e.reciprocal(out=rstd[:], in_=rstd[:]), reads=[rstd], writes=[rstd])
    p.op("dve", lambda e: e.tensor_scalar(out=xn[:], in0=xt[:], scalar1=rstd[:, 0:1], scalar2=None, op0=ALU.mult), reads=[xt, rstd], writes=[xn])
    for half in range(2):
        pt = ptr[half]
        for k8 in range(8):
            kc = half * 8 + k8
            p.op("pe", lambda e, kc=kc, k8=k8, pt=pt: e.transpose(out=pt[:, k8 * 128:(k8 + 1) * 128], in_=xn[:, kc * 128:(kc + 1) * 128],
                                                                    identity=idb[:]),
                 reads=[xn, idb], writes=[pt])
        for k8 in range(8):
            kc = half * 8 + k8
            eng = evac_engs[half % len(evac_engs)]
            if eng == "act":
                p.op("act", lambda e, kc=kc, k8=k8, pt=pt: e.activation(out=hT[:, kc, tcol:tcol + 128], in_=pt[:, k8 * 128:(k8 + 1) * 128],
                                                                          func=AF.Identity, bias=sh[:, kc:kc + 1], scale=gam[:, kc:kc + 1]),
                     reads=[pt, gam, sh], writes=[hTk[kc]])
            else:
                p.op(eng, lambda e, kc=kc, k8=k8, pt=pt: e.tensor_scalar(out=hT[:, kc, tcol:tcol + 128], in0=pt[:, k8 * 128:(k8 + 1) * 128],
                                                                          scalar1=gam[:, kc:kc + 1], scalar2=sh[:, kc:kc + 1],
                                                                          op0=ALU.mult, op1=ALU.add),
                     reads=[pt, gam, sh], writes=[hTk[kc]])


def emit_A(p, x, cT, n1T, wada, bada, w_in, ident, uT_o, fm_o, vtm_o, gates_o):
    pcond = p.ps("pcond", [128, 512])
    ptr = [p.ps(f"ptr{i}", [128, 1024], BF16) for i in range(2)]
    pmm = [p.ps(f"pmm{i}", [128, 512]) for i in range(4)]

    idf = p.sb("idf", [128, 128])
    idb = p.sb("idb", [128, 128], BF16)
    p.dma("sp", idf[:], ident, writes=[idf])
    p.op("dve", lambda e: e.tensor_copy(out=idb[:], in_=idf[:]), reads=[idf], writes=[idb])

    sh = p.sb("sh1", [128, 16]); sc1 = p.sb("sc1", [128, 16])
    p.dma("sp", sh[:], wada, writes=[sh]); p.dma("sp", sc1[:], bada, writes=[sc1])
    ng = p.sb("ng", [128, 16])
    gam = p.sb("gam", [128, 16])
    p.dma("sp", ng[:], n1T, writes=[ng])
    stt(p, "dve", gam, gam[:], sc1, sc1[:], 1.0, ng, ng[:], ALU.add, ALU.mult)
    import os
    STAGE = 99

    Wb = p.sb("Wb", [128, 16, D_IN], BF16)
    Wbk = [p.view(f"Wb{kc}", Wb[:, kc, :]) for kc in range(16)]
    with p.scope():
        wst = [p.sb(f"wst{i}", [128, D_IN]) for i in range(3)]
        for kc in range(16):
            s = wst[kc % 3]
            p.dma("sp", s[:], w_in[kc * 128:(kc + 1) * 128, :], writes=[s])
            eng = ("dve", "pool")[kc % 2]
            p.op(eng, lambda e, kc=kc, s=s: e.tensor_copy(out=Wb[:, kc, :], in_=s[:]), reads=[s], writes=[Wbk[kc]])

    if STAGE <= 2:
        return
    xts = [p.sb(f"xt{i}", [128, D]) for i in range(2)]
    scr = p.sb("sqscr", [128, D], BF16)
    xn = [p.sb(f"xn{i}", [128, D], BF16) for i in range(2)]
    ssq = [p.sb(f"ssq{i}", [128, 1]) for i in range(2)]
    rstd = [p.sb(f"rstd{i}", [128, 1]) for i in range(2)]
    hTs = [p.sb(f"hT{i}", [128, 16, 512], BF16) for i in range(2)]
    hTks = [[p.view(f"hT{i}_{kc}", hTs[i][:, kc, :]) for kc in range(16)] for i in range(2)]
    ofm = [p.sb(f"ofm{i}", [128, 512], BF16) for i in range(3)]
    ofu = [p.sb(f"ofu{i}", [128, 512]) for i in range(2)]
    ovt = [p.sb(f"ovt{i}", [128, 512], BF16) for i in range(2)]
    ogt = [p.sb(f"ogt{i}", [128, 24]) for i in range(2)]

    nfm = 0
    nu = 0
    nv = 0
    for tg in range(TPC // 512):
        hT = hTs[tg % 2]
        hTk = hTks[tg % 2]
        for ti in range(4):
            t = tg * 4 + ti
            xt = xts[t % 2]
            p.dma("sp", xt[:], x[t * 128:(t + 1) * 128, :], writes=[xt])
            emit_norm_transpose(p, xt, hT, hTk, ti * 128, gam, sh, idb, scr, ssq[t % 2], rstd[t % 2], xn[t % 2], ptr)
        if STAGE <= 3:
            break
        for j, ch in enumerate(FM_CHUNKS):
            pm = pmm[j % 3]
            for kc in range(16):
                p.op("pe", lambda e, kc=kc, ch=ch, pm=pm, hT=hT: e.matmul(pm[:], lhsT=Wb[:, kc, ch * 128:(ch + 1) * 128], rhs=hT[:, kc, :],
                                                                          start=(kc == 0), stop=(kc == 15)),
                     reads=[Wbk[kc], hTk[kc]], writes=[pm])
            if ch < 8:
                o = ofu[nu % 2]
                nu += 1
                p.op("dve", lambda e, o=o, pm=pm: e.tensor_copy(out=o[:], in_=pm[:]), reads=[pm], writes=[o])
                p.dma("act", uT_o[ch * 128:(ch + 1) * 128, tg * 512:(tg + 1) * 512], o[:], reads=[o])
            else:
                o = ofm[nfm % 3]
                nfm += 1
                scale = QSCALE if ch < 16 else 1.0
                p.op("act", lambda e, o=o, pm=pm, scale=scale: e.activation(out=o[:], in_=pm[:], func=AF.Copy, scale=scale),
                     reads=[pm], writes=[o])
                p.dma("act", fm_o[(j - 8) * 128:(j - 7) * 128, tg * 512:(tg + 1) * 512], o[:], reads=[o])
        if STAGE <= 4:
            break
        for ti in range(4):
            t = tg * 4 + ti
            o = ovt[nv % 2]
            og = ogt[nv % 2]
            nv += 1
            for i, (c0, nchk) in enumerate(TM_COLS):
                pm = pmm[3]
                for kc in range(16):
                    p.op("pe", lambda e, kc=kc, c0=c0, nchk=nchk, pm=pm, hT=hT, ti=ti: e.matmul(
                        pm[:, 0:nchk * 128], lhsT=hT[:, kc, ti * 128:(ti + 1) * 128], rhs=Wb[:, kc, c0 * 128:(c0 + nchk) * 128],
                        start=(kc == 0), stop=(kc == 15)), reads=[Wbk[kc], hTk[kc]], writes=[pm])
                p.op("dve", lambda e, o=o, pm=pm, i=i: e.tensor_copy(out=o[:, i * 256:(i + 1) * 256], in_=pm[:, 0:256]), reads=[pm], writes=[o])
            p.dma("act", vtm_o[t * 128:(t + 1) * 128, :], o[:], reads=[o])
            pm = pmm[3]
            for kc in range(16):
                p.op("pe", lambda e, kc=kc, pm=pm, hT=hT, ti=ti: e.matmul(pm[:, 0:24], lhsT=hT[:, kc, ti * 128:(ti + 1) * 128], rhs=Wb[:, kc, 3584:3608],
                                                                        start=(kc == 0), stop=(kc == 15)), reads=[Wbk[kc], hTk[kc]], writes=[pm])
            p.op("act", lambda e, og=og, pm=pm: e.activation(out=og[:], in_=pm[:, 0:24], func=AF.Sigmoid), reads=[pm], writes=[og])
            p.dma("act", gates_o[t * 128:(t + 1) * 128, :], og[:], reads=[og])


NC0 = 1536


def build_L0():
    nc = bass.Bass("TRN2", target_bir_lowering=False)
    cT = nc.dram_tensor("cT", [128, 16], F32, kind="ExternalInput").ap()
    wada = nc.dram_tensor("wada", [D, NC0], F32, kind="ExternalInput").ap()
    badaT = nc.dram_tensor("badaT", [128, NC0 // 128], F32, kind="ExternalInput").ap()
    ident = nc.dram_tensor("ident", [128, 128], F32, kind="ExternalInput").ap()
    condT_o = nc.dram_tensor("condT_o", [128, NC0 // 128], F32, kind="ExternalOutput").ap()
    with ExitStack() as st:
        p = Prog(nc, st)
        pcond = p.ps("pcond", [128, 512])
        idf = p.sb("idf", [128, 128])
        p.dma("sp", idf[:], ident, writes=[idf])
        condT = p.sb("condT", [128, NC0 // 128])
        emit_cond(p, cT, wada, badaT, NC0, pcond, condT, idf)
        p.dma("act", condT_o, condT[:], reads=[condT])
        p.finish()
        p.replay()
    return nc

import math

L = 16384
NCH = L // 128
TWO_PI = 2.0 * math.pi


_SC_N = [0]


def emit_sincos(p, ang, cos_o, sin_o, tmp, eng="dve"):
    (ab, aap), (cb, cap), (sb_, sap), (tb, tap) = ang, cos_o, sin_o, tmp
    shape = list(tb.ap.shape)
    _SC_N[0] += 1
    ni = p.sb(f"sc_ni{_SC_N[0]}", shape, I32)
    nf = p.sb(f"sc_nf{_SC_N[0]}", shape)
    cm = p.sb(f"sc_cm{_SC_N[0]}", shape)
    C1 = 6.28125
    C2 = TWO_PI - C1
    for shift, (ob, oap) in ((0.0, (sb_, sap)), (0.5 * math.pi, (cb, cap))):
        ts(p, eng, tb, tap, ab, aap, shift, None, op0=ALU.add)
        ts(p, eng, nf, nf[:], tb, tap, 1.0 / TWO_PI, None, op0=ALU.mult)
        cp(p, eng, ni, ni[:], nf, nf[:])
        cp(p, eng, nf, nf[:], ni, ni[:])
        stt(p, eng, tb, tap, nf, nf[:], -C1, tb, tap, ALU.mult, ALU.add)
        stt(p, eng, tb, tap, nf, nf[:], -C2, tb, tap, ALU.mult, ALU.add)
        ts(p, eng, cm, cm[:], tb, tap, math.pi, None, op0=ALU.is_gt)
        stt(p, eng, tb, tap, cm, cm[:], -TWO_PI, tb, tap, ALU.mult, ALU.add)
        ts(p, eng, cm, cm[:], tb, tap, -math.pi, None, op0=ALU.is_lt)
        stt(p, eng, tb, tap, cm, cm[:], TWO_PI, tb, tap, ALU.mult, ALU.add)
        ts(p, eng, tb, tap, tb, tap, math.pi, -math.pi, op0=ALU.min, op1=ALU.max)
        act(p, ob, oap, tb, tap, AF.Sin)


NEGPI = [None]


def emit_S5(p, uT_d, prm, yT_o):
    negpi = p.sb("negpi", [128, 1])
    p.op("dve", lambda e: e.memset(negpi[:], -math.pi), writes=[negpi])
    NEGPI[0] = negpi

    def load(name, shape):
        b = p.sb("s5_" + name, shape)
        p.dma("sp", b[:], prm[name], writes=[b])
        return b

    jcol = load("jcol", [128, 1])
    irow = load("irow", [128, 129])
    triT = load("triT", [128, 128])
    maskBD = load("maskBD", [128, 8])
    d_ch = load("d_ch", [128, 1])
    CTre = load("CTre", [128, 4, 128])
    CTim = load("CTim", [128, 4, 128])
    p.op("pool", lambda e: e.tensor_scalar(out=CTim[:], in0=CTim[:], scalar1=-1.0, scalar2=None, op0=ALU.mult), reads=[CTim], writes=[CTim])

    BD = p.sb("BD", [128, 4, 2, 2, 64])
    Er = p.sb("Er", [128, 4, 128])
    Ei = p.sb("Ei", [128, 4, 128])
    Fr = p.sb("Fr", [128, 4, 129])
    Fi = p.sb("Fi", [128, 4, 129])

    with p.scope():
        lr = load("lr_ch", [128, 64]); li = load("li_ch", [128, 64]); ldt = load("ldt_ch", [128, 1])
        bre = load("bre_ch", [128, 64]); bim = load("bim_ch", [128, 64])
        dt = p.sb("dt_ch", [128, 1])
        p.op("act", lambda e: e.activation(out=dt[:], in_=ldt[:], func=AF.Exp), reads=[ldt], writes=[dt])
        lrdt = p.sb("lrdt", [128, 64]); th = p.sb("th", [128, 64]); mag = p.sb("mag", [128, 64])
        cs = p.sb("cs", [128, 64]); sn = p.sb("sn", [128, 64]); tmp = p.sb("tmpc", [128, 64])
        p.op("dve", lambda e: e.tensor_scalar(out=lrdt[:], in0=lr[:], scalar1=dt[:, 0:1], scalar2=None, op0=ALU.mult), reads=[lr, dt], writes=[lrdt])
        p.op("dve", lambda e: e.tensor_scalar(out=th[:], in0=li[:], scalar1=dt[:, 0:1], scalar2=None, op0=ALU.mult), reads=[li, dt], writes=[th])
        p.op("act", lambda e: e.activation(out=mag[:], in_=lrdt[:], func=AF.Exp), reads=[lrdt], writes=[mag])
        emit_sincos(p, (th, th[:]), (cs, cs[:]), (sn, sn[:]), (tmp, tmp[:]))
        nr = p.sb("nr", [128, 64]); ni = p.sb("ni", [128, 64]); den = p.sb("den", [128, 64]); t2 = p.sb("t2", [128, 64])
        wre = p.sb("wre", [128, 64]); wim = p.sb("wim", [128, 64]); bbr = p.sb("bbr", [128, 64]); bbi = p.sb("bbi", [128, 64])
        V = "dve"
        p.op(V, lambda e: e.tensor_tensor(out=nr[:], in0=mag[:], in1=cs[:], op=ALU.mult), reads=[mag, cs], writes=[nr])
        p.op(V, lambda e: e.tensor_scalar(out=nr[:], in0=nr[:], scalar1=-1.0, scalar2=None, op0=ALU.add), reads=[nr], writes=[nr])
        p.op(V, lambda e: e.tensor_tensor(out=ni[:], in0=mag[:], in1=sn[:], op=ALU.mult), reads=[mag, sn], writes=[ni])
        p.op(V, lambda e: e.tensor_tensor(out=den[:], in0=lr[:], in1=lr[:], op=ALU.mult), reads=[lr], writes=[den])
        p.op(V, lambda e: e.tensor_tensor(out=t2[:], in0=li[:], in1=li[:], op=ALU.mult), reads=[li], writes=[t2])
        p.op(V, lambda e: e.tensor_tensor(out=den[:], in0=den[:], in1=t2[:], op=ALU.add), reads=[den, t2], writes=[den])
        p.op(V, lambda e: e.reciprocal(out=den[:], in_=den[:]), reads=[den], writes=[den])
        p.op(V, lambda e: e.tensor_tensor(out=wre[:], in0=nr[:], in1=lr[:], op=ALU.mult), reads=[nr, lr], writes=[wre])
        p.op(V, lambda e: e.tensor_tensor(out=t2[:], in0=ni[:], in1=li[:], op=ALU.mult), reads=[ni, li], writes=[t2])
        p.op(V, lambda e: e.tensor_tensor(out=wre[:], in0=wre[:], in1=t2[:], op=ALU.add), reads=[wre, t2], writes=[wre])
        p.op(V, lambda e: e.tensor_tensor(out=wre[:], in0=wre[:], in1=den[:], op=ALU.mult), reads=[wre, den], writes=[wre])
        p.op(V, lambda e: e.tensor_tensor(out=wim[:], in0=ni[:], in1=lr[:], op=ALU.mult), reads=[ni, lr], writes=[wim])
        p.op(V, lambda e: e.tensor_tensor(out=t2[:], in0=nr[:], in1=li[:], op=ALU.mult), reads=[nr, li], writes=[t2])
        p.op(V, lambda e: e.tensor_tensor(out=wim[:], in0=wim[:], in1=t2[:], op=ALU.subtract), reads=[wim, t2], writes=[wim])
        p.op(V, lambda e: e.tensor_tensor(out=wim[:], in0=wim[:], in1=den[:], op=ALU.mult), reads=[wim, den], writes=[wim])
        p.op(V, lambda e: e.tensor_tensor(out=bbr[:], in0=wre[:], in1=bre[:], op=ALU.mult), reads=[wre, bre], writes=[bbr])
        p.op(V, lambda e: e.tensor_tensor(out=t2[:], in0=wim[:], in1=bim[:], op=ALU.mult), reads=[wim, bim], writes=[t2])
        p.op(V, lambda e: e.tensor_tensor(out=bbr[:], in0=bbr[:], in1=t2[:], op=ALU.subtract), reads=[bbr, t2], writes=[bbr])
        p.op(V, lambda e: e.tensor_tensor(out=bbi[:], in0=wre[:], in1=bim[:], op=ALU.mult), reads=[wre, bim], writes=[bbi])
        p.op(V, lambda e: e.tensor_tensor(out=t2[:], in0=wim[:], in1=bre[:], op=ALU.mult), reads=[wim, bre], writes=[t2])
        p.op(V, lambda e: e.tensor_tensor(out=bbi[:], in0=bbi[:], in1=t2[:], op=ALU.add), reads=[bbi, t2], writes=[bbi])
        mview = maskBD[:].rearrange("c (q g) -> c q g", g=2)
        for ri, bb in ((0, bbr), (1, bbi)):
            for q in range(4):
                for gg in range(2):
                    p.op(V, lambda e, ri=ri, bb=bb, q=q, gg=gg: e.tensor_scalar(out=BD[:, q, ri, gg, :], in0=bb[:], scalar1=maskBD[:, q * 2 + gg:q * 2 + gg + 1],
                                                                               scalar2=None, op0=ALU.mult), reads=[bb, maskBD], writes=[BD])

        lr_r = load("lr_row", [128, 512]); li_r = load("li_row", [128, 512]); ldt_r = load("ldt_row", [128, 512])
        dt_r = p.sb("dt_r", [128, 512]); ang = p.sb("ang_r", [128, 512]); mg = p.sb("mg_r", [128, 512])
        cs_r = p.sb("cs_r", [128, 512]); sn_r = p.sb("sn_r", [128, 512]); tmp_r = p.sb("tmp_r", [128, 512])
        negj = p.sb("negj", [128, 1])
        p.op(V, lambda e: e.tensor_scalar(out=negj[:], in0=jcol[:], scalar1=-1.0, scalar2=None, op0=ALU.mult), reads=[jcol], writes=[negj])
        p.op("act", lambda e: e.activation(out=dt_r[:], in_=ldt_r[:], func=AF.Exp), reads=[ldt_r], writes=[dt_r])
        p.op(V, lambda e: e.tensor_tensor(out=lr_r[:], in0=lr_r[:], in1=dt_r[:], op=ALU.mult), reads=[lr_r, dt_r], writes=[lr_r])
        p.op(V, lambda e: e.tensor_tensor(out=li_r[:], in0=li_r[:], in1=dt_r[:], op=ALU.mult), reads=[li_r, dt_r], writes=[li_r])
        p.op("act", lambda e: e.activation(out=mg[:], in_=lr_r[:], func=AF.Exp, scale=negj[:, 0:1]), reads=[lr_r, negj], writes=[mg])
        p.op(V, lambda e: e.tensor_scalar(out=ang[:], in0=li_r[:], scalar1=jcol[:, 0:1], scalar2=None, op0=ALU.mult), reads=[li_r, jcol], writes=[ang])
        emit_sincos(p, (ang, ang[:]), (cs_r, cs_r[:]), (sn_r, sn_r[:]), (tmp_r, tmp_r[:]))
        p.op(V, lambda e: e.tensor_tensor(out=Er[:], in0=mg[:].rearrange("c (q s) -> c q s", q=4), in1=cs_r[:].rearrange("c (q s) -> c q s", q=4), op=ALU.mult),
             reads=[mg, cs_r], writes=[Er])
        p.op(V, lambda e: e.scalar_tensor_tensor(out=Ei[:], in0=mg[:].rearrange("c (q s) -> c q s", q=4), scalar=-1.0,
                                                 in1=sn_r[:].rearrange("c (q s) -> c q s", q=4), op0=ALU.mult, op1=ALU.mult),
             reads=[mg, sn_r], writes=[Ei])

        lr_s = load("lr_sp", [128, 4]); li_s = load("li_sp", [128, 4]); ldt_s = load("ldt_sp", [128, 4])
        dt_s = p.sb("dt_s", [128, 4])
        p.op("act", lambda e: e.activation(out=dt_s[:], in_=ldt_s[:], func=AF.Exp), reads=[ldt_s], writes=[dt_s])
        p.op(V, lambda e: e.tensor_tensor(out=lr_s[:], in0=lr_s[:], in1=dt_s[:], op=ALU.mult), reads=[lr_s, dt_s], writes=[lr_s])
        p.op(V, lambda e: e.tensor_tensor(out=li_s[:], in0=li_s[:], in1=dt_s[:], op=ALU.mult), reads=[li_s, dt_s], writes=[li_s])
        ang_s = p.sb("ang_s", [128, 4, 129]); mg_s = p.sb("mg_s", [128, 4, 129]); cs_s = p.sb("cs_s", [128, 4, 129]); sn_s = p.sb("sn_s", [128, 4, 129])
        tmp_s = p.sb("tmp_s", [128, 4, 129])
        for q in range(4):
            p.op("act", lambda e, q=q: e.activation(out=mg_s[:, q, :], in_=irow[:], func=AF.Exp, scale=lr_s[:, q:q + 1]), reads=[irow, lr_s], writes=[mg_s])
            p.op(V, lambda e, q=q: e.tensor_scalar(out=ang_s[:, q, :], in0=irow[:], scalar1=li_s[:, q:q + 1], scalar2=None, op0=ALU.mult),
                 reads=[irow, li_s], writes=[ang_s])
        emit_sincos(p, (ang_s, ang_s[:]), (cs_s, cs_s[:]), (sn_s, sn_s[:]), (tmp_s, tmp_s[:]))
        p.op(V, lambda e: e.tensor_tensor(out=Fr[:], in0=mg_s[:], in1=cs_s[:], op=ALU.mult), reads=[mg_s, cs_s], writes=[Fr])
        p.op(V, lambda e: e.tensor_tensor(out=Fi[:], in0=mg_s[:], in1=sn_s[:], op=ALU.mult), reads=[mg_s, sn_s], writes=[Fi])

    with p.scope():
        uT = p.sb("uT_sb", [128, L])
        NLD = 8
        uTv = [p.view(f"uT{i}", uT[:, i * (L // NLD):(i + 1) * (L // NLD)]) for i in range(NLD)]
        for i in range(NLD):
            p.dma("sp", uT[:, i * (L // NLD):(i + 1) * (L // NLD)], uT_d[:, i * (L // NLD):(i + 1) * (L // NLD)], writes=[uTv[i]])
        pX = [p.ps(f"pX{i}", [128, 4, 2, 128]) for i in range(1)]
        pZ = [p.ps(f"pZ{i}", [128, 4, 2, 128]) for i in range(1)]
        pY = [p.ps(f"pY{i}", [128, 128]) for i in range(2)]
        Xs = p.sb("Xs", [128, 4, 2, 128])
        Wt = p.sb("Wt", [128, 4, 2, 128])
        Zs = p.sb("Zs", [128, 4, 2, 128])
        Sr = p.sb("Sr", [128, 4, 128]); Si = p.sb("Si", [128, 4, 128])
        m1 = p.sb("m1", [128, 4, 128]); m2 = p.sb("m2", [128, 4, 128]); m3 = p.sb("m3", [128, 4, 128]); m4 = p.sb("m4", [128, 4, 128])
        cpr = p.sb("cpr", [128, 4]); cpi = p.sb("cpi", [128, 4])
        ca = p.sb("ca", [128, 4]); cb = p.sb("cb", [128, 4])
        p.op("dve", lambda e: e.memset(cpr[:], 0.0), writes=[cpr])
        p.op("dve", lambda e: e.memset(cpi[:], 0.0), writes=[cpi])
        yst = [p.sb(f"yst{i}", [128, 512]) for i in range(2)]
        BDf = BD[:].rearrange("c q r g s -> c (q r g s)")
        for c in range(NCH):
            uc = uT[:, c * 128:(c + 1) * 128]
            ub = uTv[c * 128 // (L // NLD)]
            px = pX[0]; pz = pZ[0]; py = pY[c % 2]
            pxf = px[:].rearrange("c q r s -> c (q r s)")
            for h in range(2):
                p.op("pe", lambda e, h=h, uc=uc, pxf=pxf: e.matmul(pxf[:, h * 512:(h + 1) * 512], lhsT=uc, rhs=BDf[:, h * 512:(h + 1) * 512], start=True, stop=True),
                     reads=[ub, BD], writes=[px])
            p.op("act", lambda e, px=px: e.activation(out=Xs[:], in_=px[:], func=AF.Copy), reads=[px], writes=[Xs])
            p.op("dve", lambda e: e.tensor_tensor(out=m1[:], in0=Xs[:, :, 0, :], in1=Er[:], op=ALU.mult), reads=[Xs, Er], writes=[m1])
            p.op("dve", lambda e: e.tensor_tensor(out=m2[:], in0=Xs[:, :, 1, :], in1=Ei[:], op=ALU.mult), reads=[Xs, Ei], writes=[m2])
            p.op("dve", lambda e: e.tensor_tensor(out=Wt[:, :, 0, :], in0=m1[:], in1=m2[:], op=ALU.subtract), reads=[m1, m2], writes=[Wt])
            p.op("pool", lambda e: e.tensor_tensor(out=m3[:], in0=Xs[:, :, 0, :], in1=Ei[:], op=ALU.mult), reads=[Xs, Ei], writes=[m3])
            p.op("pool", lambda e: e.tensor_tensor(out=m4[:], in0=Xs[:, :, 1, :], in1=Er[:], op=ALU.mult), reads=[Xs, Er], writes=[m4])
            p.op("pool", lambda e: e.tensor_tensor(out=Wt[:, :, 1, :], in0=m3[:], in1=m4[:], op=ALU.add), reads=[m3, m4, Wt], writes=[Wt])
            for q in range(4):
                for ri in range(2):
                    p.op("pe", lambda e, q=q, ri=ri, pz=pz: e.matmul(pz[:, q, ri, :], lhsT=Wt[:, q, ri, :], rhs=triT[:], start=True, stop=True),
                         reads=[Wt, triT], writes=[pz])
            for q in range(4):
                p.op("act", lambda e, q=q, pz=pz: e.activation(out=Zs[:, q, 0, :], in_=pz[:, q, 0, :], func=AF.Identity, bias=cpr[:, q:q + 1], scale=1.0),
                     reads=[pz, cpr], writes=[Zs])
                p.op("act", lambda e, q=q, pz=pz: e.activation(out=Zs[:, q, 1, :], in_=pz[:, q, 1, :], func=AF.Identity, bias=cpi[:, q:q + 1], scale=1.0),
                     reads=[pz, cpi], writes=[Zs])
            if c + 1 < NCH:
                p.op("dve", lambda e: e.tensor_tensor(out=ca[:], in0=Zs[:, :, 0, 127], in1=Fr[:, :, 128], op=ALU.mult), reads=[Zs, Fr], writes=[ca])
                p.op("dve", lambda e: e.tensor_tensor(out=cb[:], in0=Zs[:, :, 1, 127], in1=Fi[:, :, 128], op=ALU.mult), reads=[Zs, Fi], writes=[cb])
                p.op("dve", lambda e: e.tensor_tensor(out=cpr[:], in0=ca[:], in1=cb[:], op=ALU.subtract), reads=[ca, cb], writes=[cpr])
                p.op("dve", lambda e: e.tensor_tensor(out=ca[:], in0=Zs[:, :, 0, 127], in1=Fi[:, :, 128], op=ALU.mult), reads=[Zs, Fi], writes=[ca])
                p.op("dve", lambda e: e.tensor_tensor(out=cb[:], in0=Zs[:, :, 1, 127], in1=Fr[:, :, 128], op=ALU.mult), reads=[Zs, Fr], writes=[cb])
                p.op("dve", lambda e: e.tensor_tensor(out=cpi[:], in0=ca[:], in1=cb[:], op=ALU.add), reads=[ca, cb], writes=[cpi])
            p.op("dve", lambda e: e.tensor_tensor(out=m1[:], in0=Zs[:, :, 0, :], in1=Fr[:, :, 0:128], op=ALU.mult), reads=[Zs, Fr], writes=[m1])
            p.op("dve", lambda e: e.tensor_tensor(out=m2[:], in0=Zs[:, :, 1, :], in1=Fi[:, :, 0:128], op=ALU.mult), reads=[Zs, Fi], writes=[m2])
            p.op("dve", lambda e: e.tensor_tensor(out=Sr[:], in0=m1[:], in1=m2[:], op=ALU.subtract), reads=[m1, m2], writes=[Sr])
            p.op("pool", lambda e: e.tensor_tensor(out=m3[:], in0=Zs[:, :, 0, :], in1=Fi[:, :, 0:128], op=ALU.mult), reads=[Zs, Fi], writes=[m3])
            p.op("pool", lambda e: e.tensor_tensor(out=m4[:], in0=Zs[:, :, 1, :], in1=Fr[:, :, 0:128], op=ALU.mult), reads=[Zs, Fr], writes=[m4])
            p.op("pool", lambda e: e.tensor_tensor(out=Si[:], in0=m3[:], in1=m4[:], op=ALU.add), reads=[m3, m4], writes=[Si])
            n = 0
            for q in range(4):
                for ri, (CT, S) in enumerate(((CTre, Sr), (CTim, Si))):
                    p.op("pe", lambda e, q=q, CT=CT, S=S, py=py, n=n: e.matmul(py[:], lhsT=CT[:, q, :], rhs=S[:, q, :], start=(n == 0), stop=(n == 7)),
                         reads=[CT, S], writes=[py])
                    n += 1
            ys = yst[(c // 4) % 2]
            p.op("dve", lambda e, uc=uc, py=py, ys=ys, c=c: e.scalar_tensor_tensor(out=ys[:, (c % 4) * 128:(c % 4 + 1) * 128], in0=uc, scalar=d_ch[:, 0:1], in1=py[:],
                                                                             op0=ALU.mult, op1=ALU.add), reads=[ub, d_ch, py], writes=[ys])
            if c % 4 == 3:
                p.dma("sp", yT_o[:, (c - 3) * 128:(c + 1) * 128], ys[:], reads=[ys])


def s5_host_params(inp, j):
    f = np.float32
    gs = slice(8 * j, 8 * j + 8)
    lr = inp["ssm_lam_re"][0][gs]; li = inp["ssm_lam_im"][0][gs]; ldt = inp["ssm_log_dt"][0][gs]
    bre = inp["ssm_b_re"][0][gs]; bim = inp["ssm_b_im"][0][gs]
    cre = inp["ssm_c_re"][0][gs]; cim = inp["ssm_c_im"][0][gs]
    d = inp["ssm_d"][0][gs]
    prm = {}
    prm["lr_ch"] = np.repeat(lr, 16, axis=0); prm["li_ch"] = np.repeat(li, 16, axis=0)
    prm["ldt_ch"] = np.repeat(ldt, 16)[:, None]
    prm["bre_ch"] = bre.transpose(0, 2, 1).reshape(128, 64); prm["bim_ch"] = bim.transpose(0, 2, 1).reshape(128, 64)
    prm["d_ch"] = d.reshape(128, 1)
    prm["lr_row"] = np.broadcast_to(lr.reshape(1, 512), (128, 512)); prm["li_row"] = np.broadcast_to(li.reshape(1, 512), (128, 512))
    prm["ldt_row"] = np.broadcast_to(np.repeat(ldt, 64).reshape(1, 512), (128, 512))
    def sp(a):
        return a.reshape(4, 2, 64).transpose(1, 2, 0).reshape(128, 4)
    prm["lr_sp"] = sp(lr); prm["li_sp"] = sp(li); prm["ldt_sp"] = sp(np.repeat(ldt[:, None], 64, axis=1))
    CTre = np.zeros((128, 4, 128), f); CTim = np.zeros((128, 4, 128), f)
    for q in range(4):
        for gg in range(2):
            g = 2 * q + gg
            CTre[gg * 64:(gg + 1) * 64, q, 16 * g:16 * g + 16] = cre[g].T
            CTim[gg * 64:(gg + 1) * 64, q, 16 * g:16 * g + 16] = cim[g].T
    prm["CTre"] = CTre; prm["CTim"] = CTim
    prm["jcol"] = np.arange(128, dtype=f)[:, None]
    prm["irow"] = np.broadcast_to(np.arange(129, dtype=f)[None, :], (128, 129))
    prm["triT"] = np.triu(np.ones((128, 128), f))
    m = np.zeros((128, 8), f)
    for g in range(8):
        m[16 * g:16 * g + 16, g] = 1.0
    prm["maskBD"] = m
    return {k: np.ascontiguousarray(v, dtype=f) for k, v in prm.items()}


S5_SHAPES = dict(lr_ch=[128, 64], li_ch=[128, 64], ldt_ch=[128, 1], bre_ch=[128, 64], bim_ch=[128, 64], d_ch=[128, 1],
                 lr_row=[128, 512], li_row=[128, 512], ldt_row=[128, 512], lr_sp=[128, 4], li_sp=[128, 4], ldt_sp=[128, 4],
                 CTre=[128, 4, 128], CTim=[128, 4, 128], jcol=[128, 1], irow=[128, 129], triT=[128, 128], maskBD=[128, 8])


def build_S5():
    nc = bass.Bass("TRN2", target_bir_lowering=False)
    uT = nc.dram_tensor("uT", [128, L], F32, kind="ExternalInput").ap()
    prm = {k: nc.dram_tensor(k, s, F32, kind="ExternalInput").ap() for k, s in S5_SHAPES.items()}
    yT_o = nc.dram_tensor("yT_o", [128, L], F32, kind="ExternalOutput").ap()
    with ExitStack() as st:
        p = Prog(nc, st)
        emit_S5(p, uT, prm, yT_o)
        p.finish()
        p.replay()
        print("S5 instrs", p.ninstr())
    return nc

import math

L = 16384
NEGM = -30000.0
NQG = L // 512


def t5_bucket_np(dist):
    n = np.maximum(dist, 0)
    me = 16
    scaled = np.log(np.maximum(n, me).astype(np.float32) / np.float32(me)) / np.float32(math.log(1024 / 16))
    large = me + (scaled * np.float32(16)).astype(np.int32)
    return np.where(n < me, n, np.minimum(large, 31))


def nsa_tables(rel_bias, heads4):
    f = np.float32
    own = heads4[0]
    k = np.arange(128)[:, None]
    cols = np.arange(11 * 128)[None, :]
    dist = (cols // 128 - 3) * 128 + (cols % 128) - k
    bw = np.where((dist >= 0) & (dist < 512), rel_bias[t5_bucket_np(dist), own], f(NEGM)).astype(f)
    cols = np.arange(14 * 128)[None, :]
    dist = (cols // 128 - 3) * 128 + (cols % 128) - k
    bs = np.where(dist >= 0, rel_bias[t5_bucket_np(dist), own], f(NEGM)).astype(f)
    bc = np.zeros((128, 4, 6, 512), f)
    n_ = np.arange(128)[:, None]
    q_ = np.arange(512)[None, :]
    for hi, h in enumerate(heads4):
        for dl in range(6):
            dist = 512 * dl + q_ - 16 * n_ - 31
            bc[:, hi, dl, :] = np.where(dist >= 0, rel_bias[t5_bucket_np(dist), h], f(NEGM))
    b31 = np.broadcast_to(rel_bias[31, heads4][None, :], (128, 4)).astype(f)
    return dict(biasW=np.ascontiguousarray(bw), biasS=np.ascontiguousarray(bs), biasC=bc, b31c=np.ascontiguousarray(b31))


def nsa_consts():
    import ml_dtypes
    f = np.float32
    bf = ml_dtypes.bfloat16
    EXP = np.zeros((128, 64, 128), f)
    for kk in range(64):
        EXP[2 * kk, kk, 0:64] = 1.0
        EXP[2 * kk + 1, kk, 64:128] = 1.0
    ov = np.zeros((1024, 256), f)
    for n in range(1023):
        for j in range(max(0, (16 * n) // 64 - 1), min(256, (16 * n + 32) // 64 + 1)):
            o = min(16 * n + 32, 64 * j + 64) - max(16 * n, 64 * j)
            if o > 0:
                ov[n, j] = o / 16.0
    ovx = np.zeros((128, 8, 257), f)
    ovx[:, :, :256] = ov.reshape(8, 128, 256).transpose(1, 0, 2)
    ovx[:, :, 256] = 1.0
    pmul = np.zeros((128, 3), f)
    padd = np.zeros((128, 3), f)
    padd[:64] = [1e6, 2e6, -1.0]
    pmul[64:, 0] = 1.0
    padd[64:] = [0.0, 1e6, 2e6]
    return dict(EXP=EXP.astype(bf), ovx=ovx.astype(bf), pmul=pmul, padd=padd, ident=np.eye(128, dtype=f))


NSA_IN = dict(q4T=([4, 128, L], BF16), kcmpT=([128, L], BF16), vcmpT=([128, L], BF16), kslcT=([128, L], BF16), kwinT=([128, L], BF16),
              vslc=([L, 128], BF16), vwin=([L, 128], BF16), gates3=([L, 3], F32),
              w1k=([4096, 256], F32), w1v=([4096, 256], F32), w2k=([256, 128], F32), w2v=([256, 128], F32),
              pekT=([128, 32], F32), pevT=([128, 32], F32),
              biasW=([128, 11 * 128], F32), biasS=([128, 14 * 128], F32), biasC=([128, 4, 6, 512], F32), b31c=([128, 4], F32),
              EXP=([128, 64, 128], BF16), ovx=([128, 8, 257], BF16), pmul=([128, 3], F32), padd=([128, 3], F32), ident=([128, 128], F32))


def emit_NSA(p, d, out_T, nq=NQG):
    pS = [p.ps(f"pS{i}", [128, 512]) for i in range(2)]
    pU = [p.ps(f"pU{i}", [128, 512]) for i in range(2)]
    pO = [p.ps(f"pO{i}", [128, 512]) for i in range(4)]

    def load(name, shape, dt=F32, q="sp"):
        b = p.sb("n_" + name, shape, dt)
        p.dma(q, b[:], d[name], writes=[b])
        return b

    idf = load("ident", [128, 128])
    idb = p.sb("n_idb", [128, 128], BF16)
    cp(p, "dve", idb, idb[:], idf, idf[:])
    pmul = load("pmul", [128, 3])
    padd = load("padd", [128, 3])
    b31c = load("b31c", [128, 4])
    kcT = p.sb("kcT", [128, 1024], BF16)
    RC = p.sb("RC", [128, 8, 385], BF16)
    p.dma("sp", RC[:, :, 0:257], d["ovx"], writes=[RC])
    biasC = p.sb("biasC", [128, 4, 6, 512], BF16)
    biasS = p.sb("biasS", [128, 14 * 128], BF16)
    biasW = p.sb("biasW", [128, 11 * 128], BF16)

    with p.scope():
        stg = [p.sb(f"tstg{i}", [128, 3072]) for i in range(2)]
        n = 0
        for hh in range(4):
            s = stg[n % 2]; n += 1
            p.dma("sp", s[:], d["biasC"][:, hh].rearrange("p a b -> p (a b)"), writes=[s])
            cp(p, ("dve", "pool")[n % 2], biasC, biasC[:, hh].rearrange("p a b -> p (a b)"), s, s[:])
        s = stg[n % 2]; n += 1
        p.dma("sp", s[:, 0:1792], d["biasS"], writes=[s])
        cp(p, "dve", biasS, biasS[:], s, s[:, 0:1792])
        s = stg[n % 2]; n += 1
        p.dma("sp", s[:, 0:1408], d["biasW"], writes=[s])
        cp(p, "pool", biasW, biasW[:], s, s[:, 0:1408])

        kext = p.sb("kext", [128, L + 32], BF16)
        kview = kext[:].rearrange("d (n s) -> d s n", s=16)
        w1b = p.sb("w1b", [128, 32, 256], BF16)
        w1s = [p.sb(f"w1s{i}", [128, 8, 256]) for i in range(2)]
        w2s = p.sb("w2s", [128, 2, 128])
        w2b = p.sb("w2b", [128, 2, 128], BF16)
        pes = p.sb("pes", [128, 32])
        peRep = p.sb("peRep", [128, 32, 128], BF16)
        biasH = p.sb("biasH", [128, 2])
        hTb = p.sb("hTb", [128, 2, 1024], BF16)
        mset(p, "pool", kext, kext[:, L:L + 32], 0.0)
        for which in range(2):
            kname, w1n, w2n, pen = (("kcmpT", "w1k", "w2k", "pekT"), ("vcmpT", "w1v", "w2v", "pevT"))[which]
            for c4 in range(4):
                p.dma("sp", kext[:, c4 * 4096:(c4 + 1) * 4096], d[kname][:, c4 * 4096:(c4 + 1) * 4096], writes=[kext])
            w1v_ = d[w1n].rearrange("(pos d) h -> d pos h", d=128)
            for c4 in range(4):
                s = w1s[c4 % 2]
                p.dma("sp", s[:], w1v_[:, c4 * 8:(c4 + 1) * 8, :], writes=[s])
                cp(p, ("dve", "pool")[c4 % 2], w1b, w1b[:, c4 * 8:(c4 + 1) * 8, :], s, s[:])
            p.dma("sp", w2s[:], d[w2n].rearrange("(c h) d -> h c d", h=128), writes=[w2s])
            cp(p, "dve", w2b, w2b[:], w2s, w2s[:])
            p.dma("sp", pes[:], d[pen], writes=[pes])
            for pos in range(32):
                cp(p, "dve", peRep, peRep[:, pos, :], pes, pes[:, pos:pos + 1].to_broadcast([128, 128]))
            for hc in range(2):
                ps = pS[hc % 2]
                for pos in range(32):
                    mm(p, ps, ps[:, 0:128], w1b, w1b[:, pos, hc * 128:(hc + 1) * 128], peRep, peRep[:, pos, :], start=(pos == 0), stop=(pos == 31))
                act(p, biasH, biasH[:, hc:hc + 1], ps, ps[:, 0:1], AF.Copy)
            for hc in range(2):
                for nh in range(2):
                    ps = pS[(hc * 2 + nh) % 2]
                    for pos in range(32):
                        n0 = pos // 16 + nh * 512
                        mm(p, ps, ps[:], w1b, w1b[:, pos, hc * 128:(hc + 1) * 128], kext, kview[:, pos % 16, n0:n0 + 512], start=(pos == 0), stop=(pos == 31))
                    act(p, hTb, hTb[:, hc, nh * 512:(nh + 1) * 512], ps, ps[:], AF.Gelu_apprx_tanh, bias=(biasH, biasH[:, hc:hc + 1]))
            if which == 0:
                for nh in range(2):
                    ps = pS[nh % 2]
                    for hc in range(2):
                        mm(p, ps, ps[:], w2b, w2b[:, hc, :], hTb, hTb[:, hc, nh * 512:(nh + 1) * 512], start=(hc == 0), stop=(hc == 1))
                    cp(p, "act", kcT, kcT[:, nh * 512:(nh + 1) * 512], ps, ps[:])
                mset(p, "dve", kcT, kcT[:, 1023:1024], 0.0)
            else:
                for m in range(8):
                    ps = pS[m % 2]
                    for hc in range(2):
                        mm(p, ps, ps[:, 0:128], hTb, hTb[:, hc, m * 128:(m + 1) * 128], w2b, w2b[:, hc, :], start=(hc == 0), stop=(hc == 1))
                    cp(p, "act", RC, RC[:, m, 257:385], ps, ps[:, 0:128])

    EXP = p.sb("EXPm", [128, 64, 128], BF16)
    p.dma("sp", EXP[:], d["EXP"], writes=[EXP])
    kslc = p.sb("kslc", [128, L], BF16)
    vslc1 = p.sb("vslc1", [128, 128, 129], BF16)
    mset(p, "pool", vslc1, vslc1[:, :, 128:129], 1.0)
    nld = max(1, (nq * 512) // 2048)
    vsv = d["vslc"].rearrange("(n p) d -> p n d", p=128)
    for c in range(nld):
        p.dma("sp", kslc[:, c * 2048:(c + 1) * 2048], d["kslcT"][:, c * 2048:(c + 1) * 2048], writes=[kslc])
        p.dma("sp", vslc1[:, c * 16:(c + 1) * 16, 0:128], vsv[:, c * 16:(c + 1) * 16, :], writes=[vslc1])
    q4s = [p.sb(f"q4_{i}", [128, 4, 512], BF16) for i in range(2)]
    kwins = [p.sb(f"kwin{i}", [128, 1024], BF16) for i in range(2)]
    vwins = [p.sb(f"vwin{i}", [128, 8, 129], BF16) for i in range(2)]
    gts = [p.sb(f"gts{i}", [128, 4, 3]) for i in range(2)]
    for i in range(2):
        mset(p, "pool", vwins[i], vwins[i][:, :, 128:129], 1.0)
    PcT = [p.sb(f"PcT{i}", [128, 8, 512], BF16) for i in range(2)]
    PsT = [p.sb(f"PsT{i}", [128, 512], BF16) for i in range(3)]
    sc = p.sb("scr_sc", [128, 4, 256])
    scz = p.sb("scr_z", [128, 256])
    acc = p.sb("accO", [128, 4, 128])
    maddf = p.sb("maddf", [128, 256])
    maddT = p.sb("maddT", [128, 2, 512], BF16)
    m8 = p.sb("m8", [128, 16])
    thr = p.sb("thr", [128, 1])
    rden = p.sb("rden", [128, 1])
    wsc = p.sb("wsc", [128, 1])
    ost = [p.sb(f"ost{i}", [128, 512]) for i in range(2)]
    q4v = d["q4T"].rearrange("h d t -> d h t")
    vwv = d["vwin"].rearrange("(n p) d -> p n d", p=128)
    gv = d["gates3"].rearrange("(n p) g -> p n g", p=128)
    nps = 0
    npt = 0

    for Q in range(nq):
        q4 = q4s[Q % 2]; kwin = kwins[Q % 2]; vwin = vwins[Q % 2]; gt = gts[Q % 2]
        p.dma("sp", q4[:], q4v[:, :, Q * 512:(Q + 1) * 512], writes=[q4])
        t0 = max(0, 4 * Q - 4)
        woff = t0 - (4 * Q - 4)
        p.dma("sp", kwin[:, woff * 128:1024], d["kwinT"][:, t0 * 128:(4 * Q + 4) * 128], writes=[kwin])
        p.dma("sp", vwin[:, woff:8, 0:128], vwv[:, t0:4 * Q + 4, :], writes=[vwin])
        p.dma("sp", gt[:], gv[:, 4 * Q:4 * Q + 4, :], writes=[gt])
        nm = Q // 4 + 1
        for hh in range(4):
            pc = PcT[hh % 2]
            for m in range(nm):
                dl = Q - 4 * m
                ps = pS[nps % 2]; nps += 1
                mm(p, ps, ps[:], kcT, kcT[:, m * 128:(m + 1) * 128], q4, q4[:, hh, :], start=True, stop=(dl >= 6))
                if dl < 6:
                    mm(p, ps, ps[:], idb, idb[:], biasC, biasC[:, hh, dl, :], start=False, stop=True)
                    act(p, pc, pc[:, m, :], ps, ps[:], AF.Exp)
                else:
                    act(p, pc, pc[:, m, :], ps, ps[:], AF.Exp, bias=(b31c, b31c[:, hh:hh + 1]))
            for i in range(4):
                pu = pU[(hh * 4 + i) % 2]
                N = 385 if hh == 0 else 257
                for m in range(nm):
                    mm(p, pu, pu[:, 0:N], pc, pc[:, m, i * 128:(i + 1) * 128], RC, RC[:, m, 0:N], start=(m == 0), stop=(m == nm - 1))
                ts(p, "dve", rden, rden[:], pu, pu[:, 256:257], 1e-30, None, op0=ALU.add)
                p.op("dve", lambda e: e.reciprocal(out=rden[:], in_=rden[:]), reads=[rden], writes=[rden])
                if hh == 0:
                    ts(p, "dve", sc, sc[:, i, :], pu, pu[:, 0:256], (rden, rden[:, 0:1]), None, op0=ALU.mult)
                    tt(p, "dve", wsc, wsc[:], rden, rden[:], gt, gt[:, i, 0:1], ALU.mult)
                    ts(p, "dve", acc, acc[:, i, :], pu, pu[:, 257:385], (wsc, wsc[:, 0:1]), None, op0=ALU.mult)
                else:
                    stt(p, "dve", sc, sc[:, i, :], pu, pu[:, 0:256], (rden, rden[:, 0:1]), sc, sc[:, i, :], ALU.mult, ALU.add)
        for i in range(4):
            T = 4 * Q + i
            if 2 * T + 2 < 256:
                mset(p, "dve", sc, sc[:, i, 2 * T + 2:256], -1.0)
            if T >= 1:
                a = 2 * T - 1
                tt(p, "dve", sc, sc[:, i, a:a + 3], sc, sc[:, i, a:a + 3], pmul, pmul[:], ALU.mult)
                tt(p, "dve", sc, sc[:, i, a:a + 3], sc, sc[:, i, a:a + 3], padd, padd[:], ALU.add)
            else:
                tt(p, "dve", sc, sc[:, i, 0:2], sc, sc[:, i, 0:2], pmul, pmul[:, 1:3], ALU.mult)
                tt(p, "dve", sc, sc[:, i, 0:2], sc, sc[:, i, 0:2], padd, padd[:, 1:3], ALU.add)
            mset(p, "dve", sc, sc[:, i, 0:1], 3e6)
            p.op("dve", lambda e, i=i: e.max(out=m8[:, 0:8], in_=sc[:, i, :]), reads=[sc], writes=[m8])
            p.op("dve", lambda e, i=i: e.match_replace(out=scz[:], in_to_replace=m8[:, 0:8], in_values=sc[:, i, :], imm_value=-2.0),
                 reads=[sc, m8], writes=[scz])
            p.op("dve", lambda e: e.max(out=m8[:, 8:16], in_=scz[:]), reads=[scz], writes=[m8])
            ts(p, "dve", thr, thr[:], m8, m8[:, 15:16], 0.0, None, op0=ALU.max)
            ts(p, "dve", maddf, maddf[:], sc, sc[:, i, :], (thr, thr[:, 0:1]), None, op0=ALU.is_ge)
            ts(p, "dve", maddf, maddf[:], maddf, maddf[:], -NEGM, NEGM, op0=ALU.mult, op1=ALU.add)
            for hf in range(2):
                pt = pU[npt % 2]; npt += 1
                tr(p, pt, pt[:, 0:128], maddf, maddf[:, hf * 128:(hf + 1) * 128], idf, idf[:])
                cp(p, "dve", maddT, maddT[:, hf, i * 128:(i + 1) * 128], pt, pt[:, 0:128])
        for kt in range(4 * Q + 4):
            i0 = max(0, kt - 4 * Q)
            c0 = i0 * 128
            near = (4 * Q - kt) <= 7
            ps = pS[nps % 2]; nps += 1
            pst = PsT[nps % 3]
            mm(p, ps, ps[:, c0:512], kslc, kslc[:, kt * 128:(kt + 1) * 128], q4, q4[:, 0, c0:512], start=True, stop=False)
            mm(p, ps, ps[:, c0:512], EXP, EXP[:, kt % 64, :], maddT, maddT[:, kt // 64, c0:512], start=False, stop=(not near))
            if near:
                b0 = (4 * Q - kt + 3) * 128
                mm(p, ps, ps[:, c0:512], idb, idb[:], biasS, biasS[:, b0 + c0:b0 + 512], start=False, stop=True)
                act(p, pst, pst[:, c0:512], ps, ps[:, c0:512], AF.Exp)
            else:
                act(p, pst, pst[:, c0:512], ps, ps[:, c0:512], AF.Exp, bias=(b31c, b31c[:, 0:1]))
            for i in range(i0, 4):
                po = pO[i]
                mm(p, po, po[:, 0:129], pst, pst[:, i * 128:(i + 1) * 128], vslc1, vslc1[:, kt, :], start=(kt == 0), stop=(kt == 4 * Q + i))
        for i in range(4):
            po = pO[i]
            p.op("dve", lambda e, po=po: e.reciprocal(out=rden[:], in_=po[:, 128:129]), reads=[po], writes=[rden])
            tt(p, "dve", wsc, wsc[:], rden, rden[:], gt, gt[:, i, 1:2], ALU.mult)
            stt(p, "dve", acc, acc[:, i, :], po, po[:, 0:128], (wsc, wsc[:, 0:1]), acc, acc[:, i, :], ALU.mult, ALU.add)
        for kt in range(t0, 4 * Q + 4):
            i0 = max(0, kt - 4 * Q)
            i1 = min(3, kt + 4 - 4 * Q)
            c0, c1 = i0 * 128, (i1 + 1) * 128
            slot = kt - (4 * Q - 4)
            ps = pS[nps % 2]; nps += 1
            pst = PsT[nps % 3]
            b0 = (4 * Q - kt + 3) * 128
            mm(p, ps, ps[:, c0:c1], kwin, kwin[:, slot * 128:(slot + 1) * 128], q4, q4[:, 0, c0:c1], start=True, stop=False)
            mm(p, ps, ps[:, c0:c1], idb, idb[:], biasW, biasW[:, b0 + c0:b0 + c1], start=False, stop=True)
            act(p, pst, pst[:, c0:c1], ps, ps[:, c0:c1], AF.Exp)
            for i in range(i0, i1 + 1):
                po = pO[i]
                mm(p, po, po[:, 0:129], pst, pst[:, i * 128:(i + 1) * 128], vwin, vwin[:, slot, :],
                   start=(kt == max(0, 4 * Q + i - 4)), stop=(kt == 4 * Q + i))
        o = ost[Q % 2]
        for i in range(4):
            po = pO[i]
            p.op("dve", lambda e, po=po: e.reciprocal(out=rden[:], in_=po[:, 128:129]), reads=[po], writes=[rden])
            tt(p, "dve", wsc, wsc[:], rden, rden[:], gt, gt[:, i, 2:3], ALU.mult)
            stt(p, "dve", acc, acc[:, i, :], po, po[:, 0:128], (wsc, wsc[:, 0:1]), acc, acc[:, i, :], ALU.mult, ALU.add)
            pt = pU[npt % 2]; npt += 1
            tr(p, pt, pt[:, 0:128], acc, acc[:, i, :], idf, idf[:])
            cp(p, "dve", o, o[:, i * 128:(i + 1) * 128], pt, pt[:, 0:128])
        p.dma("act", out_T[:, Q * 512:(Q + 1) * 512], o[:], reads=[o])


def build_NSA(nq=NQG):
    nc = bass.Bass("TRN2", target_bir_lowering=False)
    d = {k: nc.dram_tensor(k, s, dt, kind="ExternalInput").ap() for k, (s, dt) in NSA_IN.items()}
    out_T = nc.dram_tensor("yattT_o", [128, L], F32, kind="ExternalOutput").ap()
    with ExitStack() as st:
        p = Prog(nc, st)
        emit_NSA(p, d, out_T, nq)
        p.finish()
        p.replay()
        print("NSA instrs", p.ninstr())
    return nc


def nsa_host_inputs(inp, A, j):
    g = j // 4
    heads4 = [j] + [h for h in range(4 * g, 4 * g + 4) if h != j]
    fm = A["fm"]
    m = {}
    m["q4T"] = np.ascontiguousarray(np.stack([fm[h * 128:(h + 1) * 128] for h in heads4]))
    base = 1024
    m["kcmpT"] = np.ascontiguousarray(fm[base + (0 + g) * 128: base + (1 + g) * 128])
    m["vcmpT"] = np.ascontiguousarray(fm[base + (2 + g) * 128: base + (3 + g) * 128])
    m["kslcT"] = np.ascontiguousarray(fm[base + (4 + g) * 128: base + (5 + g) * 128])
    m["kwinT"] = np.ascontiguousarray(fm[base + (6 + g) * 128: base + (7 + g) * 128])
    m["vslc"] = np.ascontiguousarray(A["vtm"][:, g * 128:(g + 1) * 128])
    m["vwin"] = np.ascontiguousarray(A["vtm"][:, 256 + g * 128:256 + (g + 1) * 128])
    m["gates3"] = np.ascontiguousarray(A["gates"].reshape(L, 3, 8)[:, :, j])
    m["w1k"] = inp["cmp_w1_k"][0]; m["w1v"] = inp["cmp_w1_v"][0]; m["w2k"] = inp["cmp_w2_k"][0]; m["w2v"] = inp["cmp_w2_v"][0]
    m["pekT"] = np.ascontiguousarray(inp["cmp_pe_k"][0].T); m["pevT"] = np.ascontiguousarray(inp["cmp_pe_v"][0].T)
    m.update(nsa_tables(inp["rel_bias"], heads4))
    m.update(nsa_consts())
    return m


DS = 1024


def build_C1():
    nc = bass.Bass("TRN2", target_bir_lowering=False)
    I = {}
    def din(name, shape, dt=F32):
        I[name] = nc.dram_tensor(name, shape, dt, kind="ExternalInput").ap()
    din("x", [TPC, D]); din("ypreT", [DS, TPC]); din("yattT", [DS, TPC])
    din("g1row", [128, D]); din("sh2T", [128, 16]); din("sc2T", [128, 16])
    din("n2T", [128, 16]); din("wglu", [DS, DS]); din("bgluT", [128, 8]); din("bsT", [128, 8]); din("baT", [128, 8])
    din("wout", [2 * DS, D]); din("ident", [128, 128])
    O = {}
    O["x1_o"] = nc.dram_tensor("x1_o", [TPC, D], F32, kind="ExternalOutput").ap()
    O["h2T_o"] = nc.dram_tensor("h2T_o", [D, TPC], BF16, kind="ExternalOutput").ap()
    with ExitStack() as st:
        p = Prog(nc, st)
        emit_C1(p, I, O)
        p.finish()
        p.replay()
        print("C1 instrs", p.ninstr())
    return nc


def emit_C1(p, I, O):
    ptr = [p.ps(f"ptr{i}", [128, 1024], BF16) for i in range(2)]
    pg = p.ps("pg", [128, 512])
    pA = p.ps("pA", [128, 512])
    pB = p.ps("pB", [128, 512])
    pq = p.ps("pq", [128, 512])

    idf = p.sb("idf", [128, 128]); idb = p.sb("idb", [128, 128], BF16)
    p.dma("sp", idf[:], I["ident"], writes=[idf])
    cp(p, "dve", idb, idb[:], idf, idf[:])
    g1row = p.sb("g1row", [128, D])
    p.dma("sp", g1row[:], I["g1row"], writes=[g1row])
    sh = p.sb("sh2", [128, 16]); sc2 = p.sb("sc2", [128, 16])
    p.dma("sp", sh[:], I["sh2T"], writes=[sh]); p.dma("sp", sc2[:], I["sc2T"], writes=[sc2])
    ng = p.sb("ng2", [128, 16]); gam = p.sb("gam2", [128, 16])
    p.dma("sp", ng[:], I["n2T"], writes=[ng])
    stt(p, "dve", gam, gam[:], sc2, sc2[:], 1.0, ng, ng[:], ALU.add, ALU.mult)
    bglu = p.sb("bglu", [128, 8]); bsT = p.sb("bsT", [128, 8]); baT = p.sb("baT", [128, 8])
    p.dma("sp", bglu[:], I["bgluT"], writes=[bglu]); p.dma("sp", bsT[:], I["bsT"], writes=[bsT]); p.dma("sp", baT[:], I["baT"], writes=[baT])
    ones = p.sb("ones32", [128, 32])
    mset(p, "dve", ones, ones[:], 1.0)

    woutb = p.sb("woutb", [128, 16, D], BF16)
    wglub = p.sb("wglub", [128, 8, DS], BF16)
    with p.scope():
        stg = [p.sb(f"wstg{i}", [128, D]) for i in range(2)]
        for kc in range(16):
            s = stg[kc % 2]
            p.dma("sp", s[:], I["wout"][kc * 128:(kc + 1) * 128, :], writes=[s])
            cp(p, ("dve", "pool")[kc % 2], woutb, woutb[:, kc, :], s, s[:])
        for kc in range(8):
            s = stg[kc % 2]
            p.dma("sp", s[:, 0:DS], I["wglu"][kc * 128:(kc + 1) * 128, :], writes=[s])
            cp(p, ("dve", "pool")[kc % 2], wglub, wglub[:, kc, :], s, s[:, 0:DS])

    ybuf = p.sb("ybuf", [128, 8, 512])
    zf = p.sb("zf", [128, 8, 512])
    zb = p.sb("zb", [128, 8, 512], BF16)
    sq = p.sb("sqb", [128, 8, 512])
    sig = p.sb("sig", [128, 512])
    s5ob = p.sb("s5ob", [128, 8, 512], BF16)
    attb = p.sb("attb", [128, 8, 512], BF16)
    rs = p.sb("rs_s", [128, 4]); ra = p.sb("rs_a", [128, 4])
    xt = p.sb("xt", [128, D]); x1 = p.sb("x1", [128, D])
    t1 = p.sb("t1c", [128, 512])
    scr = p.sb("sqscr", [128, D], BF16); xn = p.sb("xn", [128, D], BF16)
    ssq = p.sb("ssq", [128, 1]); rstd = p.sb("rstd", [128, 1])
    hT = p.sb("hT", [128, 16, 512], BF16)
    hTk = [p.view(f"hT_{kc}", hT[:, kc, :]) for kc in range(16)]
    ypv = I["ypreT"].rearrange("(c p) t -> p c t", p=128)
    yav = I["yattT"].rearrange("(c p) t -> p c t", p=128)
    h2v = O["h2T_o"].rearrange("(c p) t -> p c t", p=128)

    def rms_rstd(src, dst):
        tt(p, "pool", sq, sq[:], src, src[:], src, src[:], ALU.mult)
        for i in range(4):
            for cc in range(8):
                mm(p, pq, pq[:, 0:32], sq, sq[:, cc, i * 128:(i + 1) * 128], ones, ones[:], start=(cc == 0), stop=(cc == 7))
            ts(p, "dve", dst, dst[:, i:i + 1], pq, pq[:, 0:1], 1.0 / DS, 1e-6, op0=ALU.mult, op1=ALU.add)
        p.op("act", lambda e: e.sqrt(out=dst[:], in_=dst[:]), reads=[dst], writes=[dst])
        p.op("dve", lambda e: e.reciprocal(out=dst[:], in_=dst[:]), reads=[dst], writes=[dst])

    for tg in range(TPC // 512):
        ts_ = slice(tg * 512, (tg + 1) * 512)
        p.dma("sp", ybuf[:], ypv[:, :, ts_], writes=[ybuf])
        act(p, zf, zf[:], ybuf, ybuf[:], AF.Gelu_apprx_tanh)
        cp(p, "pool", zb, zb[:], zf, zf[:])
        for cc in range(8):
            for kc in range(8):
                mm(p, pg, pg[:], wglub, wglub[:, kc, cc * 128:(cc + 1) * 128], zb, zb[:, kc, :], start=(kc == 0), stop=(kc == 7))
            act(p, sig, sig[:], pg, pg[:], AF.Sigmoid, bias=(bglu, bglu[:, cc:cc + 1]))
            tt(p, "dve", zf, zf[:, cc, :], zf, zf[:, cc, :], sig, sig[:], ALU.mult)
        rms_rstd(zf, rs)
        for cc in range(8):
            ts(p, "dve", s5ob, s5ob[:, cc, :], zf, zf[:, cc, :], (bsT, bsT[:, cc:cc + 1]), None, op0=ALU.mult)
        p.dma("sp", ybuf[:], yav[:, :, ts_], writes=[ybuf])
        rms_rstd(ybuf, ra)
        for cc in range(8):
            ts(p, "dve", attb, attb[:, cc, :], ybuf, ybuf[:, cc, :], (baT, baT[:, cc:cc + 1]), None, op0=ALU.mult)
        for i in range(4):
            t = tg * 4 + i
            p.dma("sp", xt[:], I["x"][t * 128:(t + 1) * 128, :], writes=[xt])
            for cg in range(4):
                cs = slice(cg * 512, (cg + 1) * 512)
                for c in range(8):
                    mm(p, pA, pA[:], s5ob, s5ob[:, c, i * 128:(i + 1) * 128], woutb, woutb[:, c, cs], start=(c == 0), stop=(c == 7))
                for c in range(8):
                    mm(p, pB, pB[:], attb, attb[:, c, i * 128:(i + 1) * 128], woutb, woutb[:, 8 + c, cs], start=(c == 0), stop=(c == 7))
                ts(p, "dve", t1, t1[:], pA, pA[:], (rs, rs[:, i:i + 1]), None, op0=ALU.mult)
                stt(p, "dve", t1, t1[:], pB, pB[:], (ra, ra[:, i:i + 1]), t1, t1[:], ALU.mult, ALU.add)
                tt(p, "pool", t1, t1[:], t1, t1[:], g1row, g1row[:, cs], ALU.mult)
                tt(p, "pool", x1, x1[:, cs], t1, t1[:], xt, xt[:, cs], ALU.add)
            p.dma("act", O["x1_o"][t * 128:(t + 1) * 128, :], x1[:], reads=[x1])
            emit_norm_transpose(p, x1, hT, hTk, i * 128, gam, sh, idb, scr, ssq, rstd, xn, ptr)
        p.dma("act", h2v[:, :, ts_], hT[:], reads=hTk)


NE = 16384
EG = 256
NEG_ = NE // EG
GTH_RELAX = 1.0 - 1e-4


def build_C2(npass=4, neg=NEG_):
    nc = bass.Bass("TRN2", target_bir_lowering=False)
    I = {}
    def din(name, shape, dt=F32):
        I[name] = nc.dram_tensor(name, shape, dt, kind="ExternalInput").ap()
    din("x1", [TPC, D]); din("h2T", [D, TPC], BF16); din("g2row", [128, D]); din("fgrow", [128, D])
    din("wq", [D, D]); din("k1T", [128, 128]); din("k2T", [128, 128]); din("pu", [NE, D]); din("pv", [NE, D]); din("ident", [128, 128])
    out = nc.dram_tensor("out", [TPC, D], F32, kind="ExternalOutput").ap()
    with ExitStack() as st:
        p = Prog(nc, st)
        emit_C2(p, I, out, npass, neg)
        p.finish()
        p.replay()
        print("C2 instrs", p.ninstr())
    return nc


def emit_C2(p, I, out, npass=4, neg=NEG_):
    psc = p.ps("psc", [128, 512])
    ptr = p.ps("ptrU", [128, 1024], BF16)
    ptw = p.ps("ptrW", [128, 1024], BF16)
    pv = [p.ps(f"pv{i}", [128, 512]) for i in range(4)]

    idf = p.sb("idf", [128, 128]); idb = p.sb("idb", [128, 128], BF16)
    p.dma("sp", idf[:], I["ident"], writes=[idf])
    cp(p, "dve", idb, idb[:], idf, idf[:])
    k1b = p.sb("k1b", [128, 128], BF16); k2b = p.sb("k2b", [128, 128], BF16)
    kst = p.sb("kst", [128, 128])
    p.dma("sp", kst[:], I["k1T"], writes=[kst]); cp(p, "dve", k1b, k1b[:], kst, kst[:])
    p.dma("sp", kst[:], I["k2T"], writes=[kst]); cp(p, "dve", k2b, k2b[:], kst, kst[:])
    g2row = p.sb("g2row", [128, D]); fgrow = p.sb("fgrow", [128, D])
    p.dma("sp", g2row[:], I["g2row"], writes=[g2row]); p.dma("sp", fgrow[:], I["fgrow"], writes=[fgrow])

    h2T = p.sb("h2T", [128, 16, 512], BF16)
    acc = p.sb("acc", [128, 4, D])
    E2 = p.sb("E2", [128, 4, 8, 128]); Rg = p.sb("Rg", [128, 4, 8, 128]); gth = p.sb("gth", [128, 4, 8])
    h2v = I["h2T"].rearrange("(c p) t -> p c t", p=128)
    wqv = I["wq"].rearrange("(k p) n -> p k n", p=128)
    uv = I["pu"].rearrange("(g a p) d -> g p a d", a=2, p=128)
    vv = I["pv"].rearrange("(g a p) d -> g p a d", a=2, p=128)

    for ps_ in range(npass):
        tsl = slice(ps_ * 512, (ps_ + 1) * 512)
        p.dma("sp", h2T[:], h2v[:, :, tsl], writes=[h2T])
        with p.scope():
            qpT = p.sb("qpT", [128, 16, 512], BF16)
            wqs = [p.sb(f"wqs{i}", [128, 16, 128]) for i in range(2)]
            wqb = [p.sb(f"wqb{i}", [128, 16, 128], BF16) for i in range(2)]
            s12 = p.sb("s12", [128, 2, 128]); scz = p.sb("sczp", [128, 256]); m16 = p.sb("m16", [128, 2, 16])
            cand = p.sb("cand", [128, 16, 16]); c16 = p.sb("c16", [128, 16]); ez = p.sb("ez", [128, 16])
            mm_ = p.sb("mmx", [128, 1]); th = p.sb("thx", [128, 1]); negm = p.sb("negm", [128, 1]); Z = p.sb("Zx", [128, 1]); lnZ = p.sb("lnZ", [128, 1])
            m1 = p.sb("m1x", [128, 1]); m2 = p.sb("m2x", [128, 1]); b1 = p.sb("b1x", [128, 1]); b2 = p.sb("b2x", [128, 1]); bg = p.sb("bgx", [128, 1])
            for ch in range(16):
                s = wqs[ch % 2]; sb_ = wqb[ch % 2]
                p.dma("sp", s[:], wqv[:, :, ch * 128:(ch + 1) * 128], writes=[s])
                cp(p, "pool", sb_, sb_[:], s, s[:])
                for kc in range(16):
                    mm(p, psc, psc[:], sb_, sb_[:, kc, :], h2T, h2T[:, kc, :], start=(kc == 0), stop=(kc == 15))
                cp(p, "act", qpT, qpT[:, ch, :], psc, psc[:])
            for i in range(4):
                for h in range(8):
                    mm(p, psc, psc[:, 0:128], qpT, qpT[:, 2 * h, i * 128:(i + 1) * 128], k1b, k1b[:])
                    mm(p, psc, psc[:, 128:256], qpT, qpT[:, 2 * h + 1, i * 128:(i + 1) * 128], k2b, k2b[:])
                    cp(p, "act", s12, s12[:].rearrange("p a b -> p (a b)"), psc, psc[:, 0:256])
                    for a in range(2):
                        p.op("dve", lambda e, a=a: e.max(out=m16[:, a, 0:8], in_=s12[:, a, :]), reads=[s12], writes=[m16])
                        p.op("dve", lambda e, a=a: e.match_replace(out=scz[:, 0:128], in_to_replace=m16[:, a, 0:8], in_values=s12[:, a, :], imm_value=-1e30),
                             reads=[s12, m16], writes=[scz])
                        p.op("dve", lambda e, a=a: e.max(out=m16[:, a, 8:16], in_=scz[:, 0:128]), reads=[scz], writes=[m16])
                    tt(p, "dve", cand, cand[:], m16, m16[:, 0, :, None].to_broadcast([128, 16, 16]), m16, m16[:, 1, None, :].to_broadcast([128, 16, 16]), ALU.add)
                    cflat = cand[:].rearrange("p a b -> p (a b)")
                    p.op("dve", lambda e: e.max(out=c16[:, 0:8], in_=cflat), reads=[cand], writes=[c16])
                    p.op("dve", lambda e: e.match_replace(out=scz[:], in_to_replace=c16[:, 0:8], in_values=cflat, imm_value=-1e30), reads=[cand, c16], writes=[scz])
                    p.op("dve", lambda e: e.max(out=c16[:, 8:16], in_=scz[:]), reads=[scz], writes=[c16])
                    p.op("dve", lambda e: e.tensor_reduce(out=mm_[:], in_=c16[:, 0:8], axis=AX.X, op=ALU.max), reads=[c16], writes=[mm_])
                    p.op("dve", lambda e: e.tensor_reduce(out=th[:], in_=c16[:, 8:16], axis=AX.X, op=ALU.min), reads=[c16], writes=[th])
                    p.op("dve", lambda e: e.tensor_reduce(out=m1[:], in_=m16[:, 0, 0:8], axis=AX.X, op=ALU.max), reads=[m16], writes=[m1])
                    p.op("dve", lambda e: e.tensor_reduce(out=m2[:], in_=m16[:, 1, 0:8], axis=AX.X, op=ALU.max), reads=[m16], writes=[m2])
                    ts(p, "dve", negm, negm[:], mm_, mm_[:], -1.0, None, op0=ALU.mult)
                    act(p, ez, ez[:], c16, c16[:], AF.Exp, bias=(negm, negm[:, 0:1]), accum=(Z, Z[:]))
                    act(p, lnZ, lnZ[:], Z, Z[:], AF.Ln)
                    ts(p, "dve", b2, b2[:], m2, m2[:], -1.0, None, op0=ALU.mult)
                    stt(p, "dve", b1, b1[:], m1, m1[:], -1.0, lnZ, lnZ[:], ALU.mult, ALU.subtract)
                    tt(p, "dve", bg, bg[:], th, th[:], mm_, mm_[:], ALU.subtract)
                    tt(p, "dve", bg, bg[:], bg, bg[:], lnZ, lnZ[:], ALU.subtract)
                    act(p, E2, E2[:, i, h, :], s12, s12[:, 1, :], AF.Exp, bias=(b2, b2[:, 0:1]))
                    act(p, Rg, Rg[:, i, h, :], s12, s12[:, 0, :], AF.Exp, bias=(b1, b1[:, 0:1]))
                    act(p, gth, gth[:, i, h:h + 1], bg, bg[:], AF.Exp)
            ts(p, "dve", gth, gth[:], gth, gth[:], GTH_RELAX, None, op0=ALU.mult)
        with p.scope():
            ust = p.sb("ust", [128, 2, D]); vst = p.sb("vst", [128, 2, D])
            ub = p.sb("ub", [128, 2, D], BF16); vb = p.sb("vb", [128, 2, D], BF16)
            uT = p.sb("uT", [128, 16, EG], BF16)
            A_ = [p.sb(f"Agelu{k}", [128, 2, 128]) for k in range(2)]
            G_ = [p.sb(f"Ggrid{k}", [128, 8, 2, 128]) for k in range(2)]
            M_ = [p.sb(f"Mgrid{k}", [128, 8, 2, 128]) for k in range(2)]
            Wg_ = [p.sb(f"Wg{k}", [128, 2, 128]) for k in range(2)]
            WA_ = [p.sb(f"WA{k}", [128, 2, 128], BF16) for k in range(2)]
            WAT_ = [p.sb(f"WAT{k}", [128, 2, 128], BF16) for k in range(2)]
            for eg in range(neg):
                p.dma("sp", ust[:], uv[eg], writes=[ust])
                cp(p, "pool", ub, ub[:], ust, ust[:])
                for a in range(2):
                    for half in range(2):
                        for k8 in range(8):
                            kc = half * 8 + k8
                            tr(p, ptr, ptr[:, k8 * 128:(k8 + 1) * 128], ub, ub[:, a, kc * 128:(kc + 1) * 128], idb, idb[:])
                        cp(p, "act", uT, uT[:, half * 8:(half + 1) * 8, a * 128:(a + 1) * 128], ptr, ptr[:].rearrange("p (k e) -> p k e", e=128))
                p.dma("sp", vst[:], vv[eg], writes=[vst])
                cp(p, "pool", vb, vb[:], vst, vst[:])
                for i in range(4):
                    A = A_[i % 2]; G = G_[i % 2]; M = M_[i % 2]; Wg = Wg_[i % 2]; WA = WA_[i % 2]; WAT = WAT_[i % 2]
                    for kc in range(16):
                        mm(p, psc, psc[:, 0:EG], h2T, h2T[:, kc, i * 128:(i + 1) * 128], uT, uT[:, kc, :], start=(kc == 0), stop=(kc == 15))
                    act(p, A, A[:].rearrange("p a b -> p (a b)"), psc, psc[:, 0:EG], AF.Gelu_apprx_tanh)
                    tt(p, "pool", G, G[:], Rg, Rg[:, i, :, 2 * eg:2 * eg + 2, None].to_broadcast([128, 8, 2, 128]),
                       E2, E2[:, i, :, None, :].to_broadcast([128, 8, 2, 128]), ALU.mult)
                    tt(p, "dve", M, M[:], G, G[:], gth, gth[:, i, :, None, None].to_broadcast([128, 8, 2, 128]), ALU.is_ge)
                    tt(p, "pool", M, M[:], M, M[:], G, G[:], ALU.mult)
                    p.op("dve", lambda e, M=M, Wg=Wg: e.tensor_reduce(out=Wg[:], in_=M[:].rearrange("p h a b -> p a b h"), axis=AX.X, op=ALU.add), reads=[M], writes=[Wg])
                    tt(p, "dve", WA, WA[:], A, A[:], Wg, Wg[:], ALU.mult)
                    for a in range(2):
                        tr(p, ptw, ptw[:, a * 128:(a + 1) * 128], WA, WA[:, a, :], idb, idb[:])
                    cp(p, "act", WAT, WAT[:].rearrange("p a b -> p (a b)"), ptw, ptw[:, 0:256])
                    for cg in range(4):
                        for a in range(2):
                            mm(p, pv[cg], pv[cg][:], WAT, WAT[:, a, :], vb, vb[:, a, cg * 512:(cg + 1) * 512], start=(a == 0), stop=(a == 1))
                        if eg == 0:
                            cp(p, "dve", acc, acc[:, i, cg * 512:(cg + 1) * 512], pv[cg], pv[cg][:])
                        else:
                            tt(p, "dve", acc, acc[:, i, cg * 512:(cg + 1) * 512], pv[cg], pv[cg][:], acc, acc[:, i, cg * 512:(cg + 1) * 512], ALU.add)
        with p.scope():
            x1t = p.sb("x1t", [128, D]); x2 = p.sb("x2", [128, D]); scr = p.sb("fscr", [128, D], BF16)
            ssq = p.sb("fssq", [128, 1]); rstd = p.sb("frstd", [128, 1])
            for i in range(4):
                t = ps_ * 4 + i
                p.dma("sp", x1t[:], I["x1"][t * 128:(t + 1) * 128, :], writes=[x1t])
                tt(p, "pool", x2, x2[:], acc, acc[:, i, :], g2row, g2row[:], ALU.mult)
                tt(p, "pool", x2, x2[:], x2, x2[:], x1t, x1t[:], ALU.add)
                act(p, scr, scr[:], x2, x2[:], AF.Square, accum=(ssq, ssq[:]))
                ts(p, "dve", rstd, rstd[:], ssq, ssq[:], 1.0 / D, 1e-6, op0=ALU.mult, op1=ALU.add)
                p.op("act", lambda e: e.sqrt(out=rstd[:], in_=rstd[:]), reads=[rstd], writes=[rstd])
                p.op("dve", lambda e: e.reciprocal(out=rstd[:], in_=rstd[:]), reads=[rstd], writes=[rstd])
                ts(p, "dve", x2, x2[:], x2, x2[:], (rstd, rstd[:, 0:1]), None, op0=ALU.mult)
                tt(p, "pool", x2, x2[:], x2, x2[:], fgrow, fgrow[:], ALU.mult)
                p.dma("act", out[t * 128:(t + 1) * 128, :], x2[:], reads=[x2])


def _ac(a, dt=None):
    return np.ascontiguousarray(a if dt is None else np.asarray(a, dtype=dt))


def _run(nc, maps):
    return run_bass_kernel_spmd(nc, maps, core_ids=list(range(len(maps)))).results


def kernel(**inp):
    f = np.float32
    NCR = 8
    ident = np.eye(128, dtype=f)
    x = inp["x"][0]
    cTl = _ac(inp["c"][0].reshape(16, 128).T)
    T16 = lambda v: _ac(np.asarray(v).reshape(16, 128).T)
    wa = inp["w_ada"][0]
    ba = inp["b_ada"][0]
    m0 = [dict(cT=cTl, wada=_ac(wa[:, c * NC0:(c + 1) * NC0]), badaT=_ac(ba[c * NC0:(c + 1) * NC0].reshape(NC0 // 128, 128).T), ident=ident)
          for c in range(NCR)]
    r0 = _run(build_L0(), m0)
    cond = np.concatenate([np.asarray(r["condT_o"]).T.reshape(-1) for r in r0])
    sh1, sc1, g1, sh2, sc2, g2 = np.split(cond, 6)
    mA = [dict(x=_ac(x[c * TPC:(c + 1) * TPC]), n1T=T16(inp["norm1_g"][0]), sh1T=T16(sh1), sc1T=T16(sc1), w_in=inp["w_in"][0], ident=ident)
          for c in range(NCR)]
    rA = _run(build_A(), mA)
    fm = np.concatenate([np.asarray(r["fm_o"]) for r in rA], axis=1)
    uT = np.concatenate([np.asarray(r["uT_o"]) for r in rA], axis=1)
    vtm = np.concatenate([np.asarray(r["vtm_o"]) for r in rA], axis=0)
    gates = np.concatenate([np.asarray(r["gates_o"]) for r in rA], axis=0)
    del rA
    mS = [dict(s5_host_params(inp, j), uT=_ac(uT[128 * j:128 * (j + 1)])) for j in range(NCR)]
    rS = _run(build_S5(), mS)
    ypreT = np.concatenate([np.asarray(r["yT_o"]) for r in rS], axis=0)
    del rS, mS, uT
    Aout = dict(fm=fm, vtm=vtm, gates=gates)
    mN = [nsa_host_inputs(inp, Aout, j) for j in range(NCR)]
    rN = _run(build_NSA(), mN)
    yattT = np.concatenate([np.asarray(r["yattT_o"]) for r in rN], axis=0)
    del rN, mN, fm, vtm, gates, Aout
    g1row = _ac(np.broadcast_to(g1[None, :], (128, D)), f)
    mC1 = []
    for c in range(NCR):
        sl = slice(c * TPC, (c + 1) * TPC)
        mC1.append(dict(x=_ac(x[sl]), ypreT=_ac(ypreT[:, sl]), yattT=_ac(yattT[:, sl]), g1row=g1row, sh2T=T16(sh2), sc2T=T16(sc2),
                        n2T=T16(inp["norm2_g"][0]), wglu=inp["ssm_w_glu"][0], bgluT=_ac(inp["ssm_b_glu"][0].reshape(8, 128).T),
                        bsT=_ac(inp["beta_ssm"][0].reshape(8, 128).T), baT=_ac(inp["beta_attn"][0].reshape(8, 128).T),
                        wout=inp["w_out"][0], ident=ident))
    rC1 = _run(build_C1(), mC1)
    del mC1, ypreT, yattT
    g2row = _ac(np.broadcast_to(g2[None, :], (128, D)), f)
    fgrow = _ac(np.broadcast_to(inp["final_g"][None, :], (128, D)), f)
    k1T = _ac(inp["peer_k1"][0].T)
    k2T = _ac(inp["peer_k2"][0].T)
    mC2 = [dict(x1=np.asarray(rC1[c]["x1_o"]), h2T=np.asarray(rC1[c]["h2T_o"]), g2row=g2row, fgrow=fgrow, wq=inp["peer_w_q"][0],
                k1T=k1T, k2T=k2T, pu=inp["peer_u"][0], pv=inp["peer_v"][0], ident=ident) for c in range(NCR)]
    rC2 = _run(build_C2(), mC2)
    out = np.concatenate([np.asarray(r["out"]) for r in rC2], axis=0)
    return out.reshape(1, L, D).astype(np.float32)
```

```python
import numpy as np
from contextlib import ExitStack
import concourse.bass as bass
import concourse.mybir as mybir
from concourse.bass_utils import run_bass_kernel_spmd

F32 = mybir.dt.float32
BF16 = mybir.dt.bfloat16
I32 = mybir.dt.int32
AF = mybir.ActivationFunctionType
ALU = mybir.AluOpType
AX = mybir.AxisListType


class Buf:
    __slots__ = ("name", "ap", "lw", "rd")

    def __init__(self, name, ap):
        self.name = name
        self.ap = ap
        self.lw = None
        self.rd = []

    def __getitem__(self, idx):
        return self.ap[idx]


class Prog:
    COMPUTE = ("pe", "act", "dve", "pool")
    ALL = ("pe", "act", "dve", "pool", "sp")
    NDSEM = 16

    def __init__(self, nc, stack):
        self.nc = nc
        self.st = stack
        self.rec = {e: [] for e in self.ALL}
        self.sem = {}
        self.cnt = {}
        for e in self.COMPUTE:
            self.sem[e] = stack.enter_context(nc.semaphore("s_" + e))
            self.cnt[e] = 0
        self.dsem = {}
        self.dcnt = {}
        self.dnext = {}
        for q in ("sp", "act", "pool"):
            self.dsem[q] = [stack.enter_context(nc.semaphore(f"d_{q}{i}")) for i in range(self.NDSEM)]
            self.dcnt[q] = [0] * self.NDSEM
            self.dnext[q] = 0
        self.waited = {e: {} for e in self.ALL}
        self.semobj = {}
        self.nbuf = 0

    def sb(self, name, shape, dt=F32):
        self.nbuf += 1
        t = self.st.enter_context(self.nc.sbuf_tensor(f"sb_{name}_{self.nbuf}", list(shape), dt))
        return Buf(name, t)

    def ps(self, name, shape, dt=F32):
        self.nbuf += 1
        t = self.st.enter_context(self.nc.psum_tensor(f"ps_{name}_{self.nbuf}", list(shape), dt))
        return Buf(name, t)

    def view(self, name, ap):
        return Buf(name, ap)

    def _events(self, reads, writes):
        evs = []
        for b in reads:
            if b.lw is not None:
                evs.append(b.lw)
        for b in writes:
            if b.lw is not None:
                evs.append(b.lw)
            evs.extend(b.rd)
        return evs

    def _emit_waits(self, e, evs):
        need = {}
        for (sk, v) in evs:
            if sk == e and e == "pe":
                continue
            if self.waited[e].get(sk, 0) < v:
                if need.get(sk, 0) < v:
                    need[sk] = v
        for sk, v in need.items():
            self.waited[e][sk] = v
            self.rec[e].append(("w", sk, v))

    def _semof(self, sk):
        if isinstance(sk, str):
            return self.sem[sk]
        q, i = sk
        return self.dsem[q][i]

    def _mark(self, ev, reads, writes):
        for b in writes:
            b.lw = ev
            b.rd = []
        for b in reads:
            if b not in writes:
                b.rd.append(ev)
                if len(b.rd) > 64:
                    last = {}
                    for (sk, v) in b.rd:
                        if last.get(sk, 0) < v:
                            last[sk] = v
                    b.rd = list(last.items())

    def op(self, e, fn, reads=(), writes=()):
        reads = list(reads)
        writes = list(writes)
        self._emit_waits(e, self._events(reads, writes))
        self.cnt[e] += 1
        ev = (e, self.cnt[e])
        self.rec[e].append(("o", fn, e, 1))
        self._mark(ev, reads, writes)
        return ev

    def dma(self, q, out_ap, in_ap, reads=(), writes=(), **kw):
        reads = list(reads)
        writes = list(writes)
        i = self.dnext[q]
        evs = self._events(reads, writes)
        if self.dcnt[q][i]:
            evs.append(((q, i), self.dcnt[q][i]))
        self._emit_waits(q, evs)
        self.dnext[q] = (i + 1) % self.NDSEM
        self.dcnt[q][i] += 16
        ev = ((q, i), self.dcnt[q][i])
        self.rec[q].append(("o", lambda eng: eng.dma_start(out=out_ap, in_=in_ap, **kw), (q, i), 16))
        self._mark(ev, reads, writes)
        return ev

    def dmalike(self, q, fn, reads=(), writes=()):
        reads = list(reads)
        writes = list(writes)
        i = self.dnext[q]
        evs = self._events(reads, writes)
        if self.dcnt[q][i]:
            evs.append(((q, i), self.dcnt[q][i]))
        self._emit_waits(q, evs)
        self.dnext[q] = (i + 1) % self.NDSEM
        self.dcnt[q][i] += 16
        ev = ((q, i), self.dcnt[q][i])
        self.rec[q].append(("o", fn, (q, i), 16))
        self._mark(ev, reads, writes)
        return ev

    def barrier(self):
        evs = self._all_events()
        for e in self.ALL:
            self._emit_waits(e, evs)

    def scope(self):
        prog = self

        class _S:
            def __enter__(self_s):
                self_s.old = prog.st
                self_s.new = ExitStack()
                prog.st = self_s.new
                return self_s

            def __exit__(self_s, *a):
                prog.barrier()
                prog.st = self_s.old
                self_s.new.close()
                return False
        return _S()

    def _all_events(self):
        evs = []
        for q in self.dsem:
            for i in range(self.NDSEM):
                if self.dcnt[q][i]:
                    evs.append(((q, i), self.dcnt[q][i]))
        for e in self.COMPUTE:
            if self.cnt[e]:
                evs.append((e, self.cnt[e]))
        return evs

    def finish(self, waiter="sp"):
        evs = []
        for q in self.dsem:
            for i in range(self.NDSEM):
                if self.dcnt[q][i]:
                    evs.append(((q, i), self.dcnt[q][i]))
        for e in self.COMPUTE:
            if self.cnt[e]:
                evs.append((e, self.cnt[e]))
        self._emit_waits(waiter, evs)

    def replay(self):
        nc = self.nc

        def run(ename):
            def f(eng):
                for item in self.rec[ename]:
                    if item[0] == "w":
                        eng.wait_ge(self._semof(item[1]), item[2])
                    else:
                        ins = item[1](eng)
                        ins.then_inc(self._semof(item[2]), item[3])
            return f

        with nc.Block() as block:
            block.sync(run("sp"))
            block.tensor(run("pe"))
            block.scalar(run("act"))
            block.vector(run("dve"))
            block.gpsimd(run("pool"))

    def ninstr(self):
        return {e: len(self.rec[e]) for e in self.ALL}


def _bufs(*items):
    out = []
    for it in items:
        if it is None:
            continue
        if isinstance(it, Buf):
            out.append(it)
        elif isinstance(it, (list, tuple)):
            out.extend(_bufs(*it))
    return out


def _sc(x):
    if isinstance(x, tuple):
        return x[1], [x[0]]
    return x, []


def mm(p, ob, oap, lb, lap, rb, rap, start=True, stop=True):
    return p.op("pe", lambda e: e.matmul(oap, lhsT=lap, rhs=rap, start=start, stop=stop), reads=_bufs(lb, rb), writes=[ob])


def tr(p, ob, oap, ib, iap, idb, idap):
    return p.op("pe", lambda e: e.transpose(out=oap, in_=iap, identity=idap), reads=[ib, idb], writes=[ob])


def act(p, ob, oap, ib, iap, func, bias=0.0, scale=1.0, accum=None, eng="act"):
    bv, br = _sc(bias)
    sv, sr = _sc(scale)
    kw = {}
    wr = [ob]
    if accum is not None:
        kw["accum_out"] = accum[1]
        wr.append(accum[0])
    return p.op("act", lambda e: e.activation(out=oap, in_=iap, func=func, bias=bv, scale=sv, **kw), reads=[ib] + br + sr, writes=wr)


def tt(p, eng, ob, oap, ab, aap, bb, bap, op):
    return p.op(eng, lambda e: e.tensor_tensor(out=oap, in0=aap, in1=bap, op=op), reads=[ab, bb], writes=[ob])


def ts(p, eng, ob, oap, ib, iap, s1, s2=None, op0=ALU.mult, op1=None, accum=None):
    v1, r1 = _sc(s1)
    v2, r2 = _sc(s2) if s2 is not None else (None, [])
    kw = {}
    wr = [ob]
    if op1 is not None:
        kw["op1"] = op1
    if accum is not None:
        kw["accum_out"] = accum[1]
        wr.append(accum[0])
    return p.op(eng, lambda e: e.tensor_scalar(out=oap, in0=iap, scalar1=v1, scalar2=v2, op0=op0, **kw), reads=[ib] + r1 + r2, writes=wr)


def stt(p, eng, ob, oap, ab, aap, s, bb, bap, op0, op1):
    v, r = _sc(s)
    return p.op(eng, lambda e: e.scalar_tensor_tensor(out=oap, in0=aap, scalar=v, in1=bap, op0=op0, op1=op1), reads=[ab, bb] + r, writes=[ob])


def cp(p, eng, ob, oap, ib, iap):
    if eng == "act":
        return p.op("act", lambda e: e.copy(out=oap, in_=iap), reads=[ib], writes=[ob])
    return p.op(eng, lambda e: e.tensor_copy(out=oap, in_=iap), reads=[ib], writes=[ob])


def mset(p, eng, ob, oap, val):
    return p.op(eng, lambda e: e.memset(oap, val), writes=[ob])


L = 16384
D = 2048
NCORES = 8
TPC = L // NCORES
D_IN = 3608
FM_CHUNKS = list(range(16)) + [16, 17, 18, 19, 20, 21, 24, 25]
TM_COLS = [(22, 2), (26, 2)]
QSCALE = 128.0 ** -0.5


def build_A():
    nc = bass.Bass("TRN2", target_bir_lowering=False)
    x = nc.dram_tensor("x", [TPC, D], F32, kind="ExternalInput").ap()
    cT = None
    n1T = nc.dram_tensor("n1T", [128, 16], F32, kind="ExternalInput").ap()
    wada = nc.dram_tensor("sh1T", [128, 16], F32, kind="ExternalInput").ap()
    bada = nc.dram_tensor("sc1T", [128, 16], F32, kind="ExternalInput").ap()
    w_in = nc.dram_tensor("w_in", [D, D_IN], F32, kind="ExternalInput").ap()
    ident = nc.dram_tensor("ident", [128, 128], F32, kind="ExternalInput").ap()
    uT_o = nc.dram_tensor("uT_o", [1024, TPC], F32, kind="ExternalOutput").ap()
    fm_o = nc.dram_tensor("fm_o", [16 * 128, TPC], BF16, kind="ExternalOutput").ap()
    vtm_o = nc.dram_tensor("vtm_o", [TPC, 512], BF16, kind="ExternalOutput").ap()
    gates_o = nc.dram_tensor("gates_o", [TPC, 24], F32, kind="ExternalOutput").ap()

    with ExitStack() as st:
        p = Prog(nc, st)
        emit_A(p, x, cT, n1T, wada, bada, w_in, ident, uT_o, fm_o, vtm_o, gates_o)
        p.finish()
        p.replay()
        print("phase A instrs", p.ninstr())
    return nc


def emit_cond(p, cT, wada, badaT, ncols, pcond, condT, idf, R=None, dlo=0, nd=None):
    nch = ncols // 128
    if nd is None:
        nd = nch
    ct = p.sb("ct", [128, 16])
    sc = p.sb("silc", [128, 16])
    screp = p.sb("screp", [128, 16, 128])
    bT = p.sb("badaT", [128, nd])
    if R is None:
        R = p.sb("condR", [128, ncols])
    dg = p.sb("conddg", [128, nd, 128])
    slabs = [p.sb(f"wslab{i}", [128, 16, 512]) for i in range(2)]
    p.dma("sp", ct[:], cT, writes=[ct])
    p.dma("sp", bT[:], badaT, writes=[bT])
    act(p, sc, sc[:], ct, ct[:], AF.Silu)
    for kc in range(16):
        cp(p, "dve", screp, screp[:, kc, :], sc, sc[:, kc:kc + 1].to_broadcast([128, 128]))
    wv = wada.rearrange("(k p) n -> p k n", p=128)
    for cg in range(ncols // 512):
        sl = slabs[cg % 2]
        p.dma("sp", sl[:], wv[:, :, cg * 512:(cg + 1) * 512], writes=[sl])
        for kc in range(16):
            mm(p, pcond, pcond[:], screp, screp[:, kc, :], sl, sl[:, kc, :], start=(kc == 0), stop=(kc == 15))
        cp(p, "dve", R, R[:, cg * 512:(cg + 1) * 512], pcond, pcond[:])
    tt(p, "dve", dg, dg[:], R, R[:, dlo * 128:(dlo + nd) * 128].rearrange("p (c k) -> p c k", k=128), idf, idf[:, None, :].to_broadcast([128, nd, 128]), ALU.mult)
    p.op("dve", lambda e: e.tensor_reduce(out=condT[:, 0:nd], in_=dg[:], axis=AX.X, op=ALU.add), reads=[dg], writes=[condT])
    tt(p, "dve", condT, condT[:, 0:nd], condT, condT[:, 0:nd], bT, bT[:], ALU.add)
    return R


def emit_norm_transpose(p, xt, hT, hTk, tcol, gam, sh, idb, scr, ssq, rstd, xn, ptr, evac_engs=("dve", "act")):
    p.op("act", lambda e: e.activation(out=scr[:], in_=xt[:], func=AF.Square, accum_out=ssq[:]), reads=[xt], writes=[scr, ssq])
    p.op("dve", lambda e: e.tensor_scalar(out=rstd[:], in0=ssq[:], scalar1=1.0 / D, scalar2=1e-6, op0=ALU.mult, op1=ALU.add),
         reads=[ssq], writes=[rstd])
    p.op("act", lambda e: e.sqrt(out=rstd[:], in_=rstd[:]), reads=[rstd], writes=[rstd])
    p.op("dve", lambda e: e.reciprocal(out=rstd[:], in_=rstd[:]), reads=[rstd], writes=[rstd])
    p.op("dve", lambda e: e.tensor_scalar(out=xn[:], in0=xt[:], scalar1=rstd[:, 0:1], scalar2=None, op0=ALU.mult), reads=[xt, rstd], writes=[xn])
    for half in range(2):
        pt = ptr[half]
        for k8 in range(8):
            kc = half * 8 + k8
            p.op("pe", lambda e, kc=kc, k8=k8, pt=pt: e.transpose(out=pt[:, k8 * 128:(k8 + 1) * 128], in_=xn[:, kc * 128:(kc + 1) * 128],
                                                                    identity=idb[:]),
                 reads=[xn, idb], writes=[pt])
        for k8 in range(8):
            kc = half * 8 + k8
            eng = evac_engs[half % len(evac_engs)]
            if eng == "act":
                p.op("act", lambda e, kc=kc, k8=k8, pt=pt: e.activation(out=hT[:, kc, tcol:tcol + 128], in_=pt[:, k8 * 128:(k8 + 1) * 128],
                                                                          func=AF.Identity, bias=sh[:, kc:kc + 1], scale=gam[:, kc:kc + 1]),
                     reads=[pt, gam, sh], writes=[hTk[kc]])
            else:
                p.op(eng, lambda e, kc=kc, k8=k8, pt=pt: e.tensor_scalar(out=hT[:, kc, tcol:tcol + 128], in0=pt[:, k8 * 128:(k8 + 1) * 128],
                                                                          scalar1=gam[:, kc:kc + 1], scalar2=sh[:, kc:kc + 1],
                                                                          op0=ALU.mult, op1=ALU.add),
                     reads=[pt, gam, sh], writes=[hTk[kc]])


def emit_A(p, x, cT, n1T, wada, bada, w_in, ident, uT_o, fm_o, vtm_o, gates_o):
    pcond = p.ps("pcond", [128, 512])
    ptr = [p.ps(f"ptr{i}", [128, 1024], BF16) for i in range(2)]
    pmm = [p.ps(f"pmm{i}", [128, 512]) for i in range(4)]

    idf = p.sb("idf", [128, 128])
    idb = p.sb("idb", [128, 128], BF16)
    p.dma("sp", idf[:], ident, writes=[idf])
    p.op("dve", lambda e: e.tensor_copy(out=idb[:], in_=idf[:]), reads=[idf], writes=[idb])

    sh = p.sb("sh1", [128, 16]); sc1 = p.sb("sc1", [128, 16])
    p.dma("sp", sh[:], wada, writes=[sh]); p.dma("sp", sc1[:], bada, writes=[sc1])
    ng = p.sb("ng", [128, 16])
    gam = p.sb("gam", [128, 16])
    p.dma("sp", ng[:], n1T, writes=[ng])
    stt(p, "dve", gam, gam[:], sc1, sc1[:], 1.0, ng, ng[:], ALU.add, ALU.mult)
    import os
    STAGE = 99

    Wb = p.sb("Wb", [128, 16, D_IN], BF16)
    Wbk = [p.view(f"Wb{kc}", Wb[:, kc, :]) for kc in range(16)]
    with p.scope():
        wst = [p.sb(f"wst{i}", [128, D_IN]) for i in range(3)]
        for kc in range(16):
            s = wst[kc % 3]
            p.dma("sp", s[:], w_in[kc * 128:(kc + 1) * 128, :], writes=[s])
            eng = ("dve", "pool")[kc % 2]
            p.op(eng, lambda e, kc=kc, s=s: e.tensor_copy(out=Wb[:, kc, :], in_=s[:]), reads=[s], writes=[Wbk[kc]])

    if STAGE <= 2:
        return
    xts = [p.sb(f"xt{i}", [128, D]) for i in range(2)]
    scr = p.sb("sqscr", [128, D], BF16)
    xn = [p.sb(f"xn{i}", [128, D], BF16) for i in range(2)]
    ssq = [p.sb(f"ssq{i}", [128, 1]) for i in range(2)]
    rstd = [p.sb(f"rstd{i}", [128, 1]) for i in range(2)]
    hTs = [p.sb(f"hT{i}", [128, 16, 512], BF16) for i in range(2)]
    hTks = [[p.view(f"hT{i}_{kc}", hTs[i][:, kc, :]) for kc in range(16)] for i in range(2)]
    ofm = [p.sb(f"ofm{i}", [128, 512], BF16) for i in range(3)]
    ofu = [p.sb(f"ofu{i}", [128, 512]) for i in range(2)]
    ovt = [p.sb(f"ovt{i}", [128, 512], BF16) for i in range(2)]
    ogt = [p.sb(f"ogt{i}", [128, 24]) for i in range(2)]

    nfm = 0
    nu = 0
    nv = 0
    for tg in range(TPC // 512):
        hT = hTs[tg % 2]
        hTk = hTks[tg % 2]
        for ti in range(4):
            t = tg * 4 + ti
            xt = xts[t % 2]
            p.dma("sp", xt[:], x[t * 128:(t + 1) * 128, :], writes=[xt])
            emit_norm_transpose(p, xt, hT, hTk, ti * 128, gam, sh, idb, scr, ssq[t % 2], rstd[t % 2], xn[t % 2], ptr)
        if STAGE <= 3:
            break
        for j, ch in enumerate(FM_CHUNKS):
            pm = pmm[j % 3]
            for kc in range(16):
                p.op("pe", lambda e, kc=kc, ch=ch, pm=pm, hT=hT: e.matmul(pm[:], lhsT=Wb[:, kc, ch * 128:(ch + 1) * 128], rhs=hT[:, kc, :],
                                                                          start=(kc == 0), stop=(kc == 15)),
                     reads=[Wbk[kc], hTk[kc]], writes=[pm])
            if ch < 8:
                o = ofu[nu % 2]
                nu += 1
                p.op("dve", lambda e, o=o, pm=pm: e.tensor_copy(out=o[:], in_=pm[:]), reads=[pm], writes=[o])
                p.dma("act", uT_o[ch * 128:(ch + 1) * 128, tg * 512:(tg + 1) * 512], o[:], reads=[o])
            else:
                o = ofm[nfm % 3]
                nfm += 1
                scale = QSCALE if ch < 16 else 1.0
                p.op("act", lambda e, o=o, pm=pm, scale=scale: e.activation(out=o[:], in_=pm[:], func=AF.Copy, scale=scale),
                     reads=[pm], writes=[o])
                p.dma("act", fm_o[(j - 8) * 128:(j - 7) * 128, tg * 512:(tg + 1) * 512], o[:], reads=[o])
        if STAGE <= 4:
            break
        for ti in range(4):
            t = tg * 4 + ti
            o = ovt[nv % 2]
            og = ogt[nv % 2]
            nv += 1
            for i, (c0, nchk) in enumerate(TM_COLS):
                pm = pmm[3]
                for kc in range(16):
                    p.op("pe", lambda e, kc=kc, c0=c0, nchk=nchk, pm=pm, hT=hT, ti=ti: e.matmul(
                        pm[:, 0:nchk * 128], lhsT=hT[:, kc, ti * 128:(ti + 1) * 128], rhs=Wb[:, kc, c0 * 128:(c0 + nchk) * 128],
                        start=(kc == 0), stop=(kc == 15)), reads=[Wbk[kc], hTk[kc]], writes=[pm])
                p.op("dve", lambda e, o=o, pm=pm, i=i: e.tensor_copy(out=o[:, i * 256:(i + 1) * 256], in_=pm[:, 0:256]), reads=[pm], writes=[o])
            p.dma("act", vtm_o[t * 128:(t + 1) * 128, :], o[:], reads=[o])
            pm = pmm[3]
            for kc in range(16):
                p.op("pe", lambda e, kc=kc, pm=pm, hT=hT, ti=ti: e.matmul(pm[:, 0:24], lhsT=hT[:, kc, ti * 128:(ti + 1) * 128], rhs=Wb[:, kc, 3584:3608],
                                                                        start=(kc == 0), stop=(kc == 15)), reads=[Wbk[kc], hTk[kc]], writes=[pm])
            p.op("act", lambda e, og=og, pm=pm: e.activation(out=og[:], in_=pm[:, 0:24], func=AF.Sigmoid), reads=[pm], writes=[og])
            p.dma("act", gates_o[t * 128:(t + 1) * 128, :], og[:], reads=[og])


NC0 = 1536


def build_L0():
    nc = bass.Bass("TRN2", target_bir_lowering=False)
    cT = nc.dram_tensor("cT", [128, 16], F32, kind="ExternalInput").ap()
    wada = nc.dram_tensor("wada", [D, NC0], F32, kind="ExternalInput").ap()
    badaT = nc.dram_tensor("badaT", [128, NC0 // 128], F32, kind="ExternalInput").ap()
    ident = nc.dram_tensor("ident", [128, 128], F32, kind="ExternalInput").ap()
    condT_o = nc.dram_tensor("condT_o", [128, NC0 // 128], F32, kind="ExternalOutput").ap()
    with ExitStack() as st:
        p = Prog(nc, st)
        pcond = p.ps("pcond", [128, 512])
        idf = p.sb("idf", [128, 128])
        p.dma("sp", idf[:], ident, writes=[idf])
        condT = p.sb("condT", [128, NC0 // 128])
        emit_cond(p, cT, wada, badaT, NC0, pcond, condT, idf)
        p.dma("act", condT_o, condT[:], reads=[condT])
        p.finish()
        p.replay()
    return nc

import math

L = 16384
NCH = L // 128
TWO_PI = 2.0 * math.pi


_SC_N = [0]


def emit_sincos(p, ang, cos_o, sin_o, tmp, eng="dve"):
    (ab, aap), (cb, cap), (sb_, sap), (tb, tap) = ang, cos_o, sin_o, tmp
    shape = list(tb.ap.shape)
    _SC_N[0] += 1
    ni = p.sb(f"sc_ni{_SC_N[0]}", shape, I32)
    nf = p.sb(f"sc_nf{_SC_N[0]}", shape)
    cm = p.sb(f"sc_cm{_SC_N[0]}", shape)
    C1 = 6.28125
    C2 = TWO_PI - C1
    for shift, (ob, oap) in ((0.0, (sb_, sap)), (0.5 * math.pi, (cb, cap))):
        ts(p, eng, tb, tap, ab, aap, shift, None, op0=ALU.add)
        ts(p, eng, nf, nf[:], tb, tap, 1.0 / TWO_PI, None, op0=ALU.mult)
        cp(p, eng, ni, ni[:], nf, nf[:])
        cp(p, eng, nf, nf[:], ni, ni[:])
        stt(p, eng, tb, tap, nf, nf[:], -C1, tb, tap, ALU.mult, ALU.add)
        stt(p, eng, tb, tap, nf, nf[:], -C2, tb, tap, ALU.mult, ALU.add)
        ts(p, eng, cm, cm[:], tb, tap, math.pi, None, op0=ALU.is_gt)
        stt(p, eng, tb, tap, cm, cm[:], -TWO_PI, tb, tap, ALU.mult, ALU.add)
        ts(p, eng, cm, cm[:], tb, tap, -math.pi, None, op0=ALU.is_lt)
        stt(p, eng, tb, tap, cm, cm[:], TWO_PI, tb, tap, ALU.mult, ALU.add)
        ts(p, eng, tb, tap, tb, tap, math.pi, -math.pi, op0=ALU.min, op1=ALU.max)
        act(p, ob, oap, tb, tap, AF.Sin)


NEGPI = [None]


def emit_S5(p, uT_d, prm, yT_o):
    negpi = p.sb("negpi", [128, 1])
    p.op("dve", lambda e: e.memset(negpi[:], -math.pi), writes=[negpi])
    NEGPI[0] = negpi

    def load(name, shape):
        b = p.sb("s5_" + name, shape)
        p.dma("sp", b[:], prm[name], writes=[b])
        return b

    jcol = load("jcol", [128, 1])
    irow = load("irow", [128, 129])
    triT = load("triT", [128, 128])
    maskBD = load("maskBD", [128, 8])
    d_ch = load("d_ch", [128, 1])
    CTre = load("CTre", [128, 4, 128])
    CTim = load("CTim", [128, 4, 128])
    p.op("pool", lambda e: e.tensor_scalar(out=CTim[:], in0=CTim[:], scalar1=-1.0, scalar2=None, op0=ALU.mult), reads=[CTim], writes=[CTim])

    BD = p.sb("BD", [128, 4, 2, 2, 64])
    Er = p.sb("Er", [128, 4, 128])
    Ei = p.sb("Ei", [128, 4, 128])
    Fr = p.sb("Fr", [128, 4, 129])
    Fi = p.sb("Fi", [128, 4, 129])

    with p.scope():
        lr = load("lr_ch", [128, 64]); li = load("li_ch", [128, 64]); ldt = load("ldt_ch", [128, 1])
        bre = load("bre_ch", [128, 64]); bim = load("bim_ch", [128, 64])
        dt = p.sb("dt_ch", [128, 1])
        p.op("act", lambda e: e.activation(out=dt[:], in_=ldt[:], func=AF.Exp), reads=[ldt], writes=[dt])
        lrdt = p.sb("lrdt", [128, 64]); th = p.sb("th", [128, 64]); mag = p.sb("mag", [128, 64])
        cs = p.sb("cs", [128, 64]); sn = p.sb("sn", [128, 64]); tmp = p.sb("tmpc", [128, 64])
        p.op("dve", lambda e: e.tensor_scalar(out=lrdt[:], in0=lr[:], scalar1=dt[:, 0:1], scalar2=None, op0=ALU.mult), reads=[lr, dt], writes=[lrdt])
        p.op("dve", lambda e: e.tensor_scalar(out=th[:], in0=li[:], scalar1=dt[:, 0:1], scalar2=None, op0=ALU.mult), reads=[li, dt], writes=[th])
        p.op("act", lambda e: e.activation(out=mag[:], in_=lrdt[:], func=AF.Exp), reads=[lrdt], writes=[mag])
        emit_sincos(p, (th, th[:]), (cs, cs[:]), (sn, sn[:]), (tmp, tmp[:]))
        nr = p.sb("nr", [128, 64]); ni = p.sb("ni", [128, 64]); den = p.sb("den", [128, 64]); t2 = p.sb("t2", [128, 64])
        wre = p.sb("wre", [128, 64]); wim = p.sb("wim", [128, 64]); bbr = p.sb("bbr", [128, 64]); bbi = p.sb("bbi", [128, 64])
        V = "dve"
        p.op(V, lambda e: e.tensor_tensor(out=nr[:], in0=mag[:], in1=cs[:], op=ALU.mult), reads=[mag, cs], writes=[nr])
        p.op(V, lambda e: e.tensor_scalar(out=nr[:], in0=nr[:], scalar1=-1.0, scalar2=None, op0=ALU.add), reads=[nr], writes=[nr])
        p.op(V, lambda e: e.tensor_tensor(out=ni[:], in0=mag[:], in1=sn[:], op=ALU.mult), reads=[mag, sn], writes=[ni])
        p.op(V, lambda e: e.tensor_tensor(out=den[:], in0=lr[:], in1=lr[:], op=ALU.mult), reads=[lr], writes=[den])
        p.op(V, lambda e: e.tensor_tensor(out=t2[:], in0=li[:], in1=li[:], op=ALU.mult), reads=[li], writes=[t2])
        p.op(V, lambda e: e.tensor_tensor(out=den[:], in0=den[:], in1=t2[:], op=ALU.add), reads=[den, t2], writes=[den])
        p.op(V, lambda e: e.reciprocal(out=den[:], in_=den[:]), reads=[den], writes=[den])
        p.op(V, lambda e: e.tensor_tensor(out=wre[:], in0=nr[:], in1=lr[:], op=ALU.mult), reads=[nr, lr], writes=[wre])
        p.op(V, lambda e: e.tensor_tensor(out=t2[:], in0=ni[:], in1=li[:], op=ALU.mult), reads=[ni, li], writes=[t2])
        p.op(V, lambda e: e.tensor_tensor(out=wre[:], in0=wre[:], in1=t2[:], op=ALU.add), reads=[wre, t2], writes=[wre])
        p.op(V, lambda e: e.tensor_tensor(out=wre[:], in0=wre[:], in1=den[:], op=ALU.mult), reads=[wre, den], writes=[wre])
        p.op(V, lambda e: e.tensor_tensor(out=wim[:], in0=ni[:], in1=lr[:], op=ALU.mult), reads=[ni, lr], writes=[wim])
        p.op(V, lambda e: e.tensor_tensor(out=t2[:], in0=nr[:], in1=li[:], op=ALU.mult), reads=[nr, li], writes=[t2])
        p.op(V, lambda e: e.tensor_tensor(out=wim[:], in0=wim[:], in1=t2[:], op=ALU.subtract), reads=[wim, t2], writes=[wim])
        p.op(V, lambda e: e.tensor_tensor(out=wim[:], in0=wim[:], in1=den[:], op=ALU.mult), reads=[wim, den], writes=[wim])
        p.op(V, lambda e: e.tensor_tensor(out=bbr[:], in0=wre[:], in1=bre[:], op=ALU.mult), reads=[wre, bre], writes=[bbr])
        p.op(V, lambda e: e.tensor_tensor(out=t2[:], in0=wim[:], in1=bim[:], op=ALU.mult), reads=[wim, bim], writes=[t2])
        p.op(V, lambda e: e.tensor_tensor(out=bbr[:], in0=bbr[:], in1=t2[:], op=ALU.subtract), reads=[bbr, t2], writes=[bbr])
        p.op(V, lambda e: e.tensor_tensor(out=bbi[:], in0=wre[:], in1=bim[:], op=ALU.mult), reads=[wre, bim], writes=[bbi])
        p.op(V, lambda e: e.tensor_tensor(out=t2[:], in0=wim[:], in1=bre[:], op=ALU.mult), reads=[wim, bre], writes=[t2])
        p.op(V, lambda e: e.tensor_tensor(out=bbi[:], in0=bbi[:], in1=t2[:], op=ALU.add), reads=[bbi, t2], writes=[bbi])
        mview = maskBD[:].rearrange("c (q g) -> c q g", g=2)
        for ri, bb in ((0, bbr), (1, bbi)):
            for q in range(4):
                for gg in range(2):
                    p.op(V, lambda e, ri=ri, bb=bb, q=q, gg=gg: e.tensor_scalar(out=BD[:, q, ri, gg, :], in0=bb[:], scalar1=maskBD[:, q * 2 + gg:q * 2 + gg + 1],
                                                                               scalar2=None, op0=ALU.mult), reads=[bb, maskBD], writes=[BD])

        lr_r = load("lr_row", [128, 512]); li_r = load("li_row", [128, 512]); ldt_r = load("ldt_row", [128, 512])
        dt_r = p.sb("dt_r", [128, 512]); ang = p.sb("ang_r", [128, 512]); mg = p.sb("mg_r", [128, 512])
        cs_r = p.sb("cs_r", [128, 512]); sn_r = p.sb("sn_r", [128, 512]); tmp_r = p.sb("tmp_r", [128, 512])
        negj = p.sb("negj", [128, 1])
        p.op(V, lambda e: e.tensor_scalar(out=negj[:], in0=jcol[:], scalar1=-1.0, scalar2=None, op0=ALU.mult), reads=[jcol], writes=[negj])
        p.op("act", lambda e: e.activation(out=dt_r[:], in_=ldt_r[:], func=AF.Exp), reads=[ldt_r], writes=[dt_r])
        p.op(V, lambda e: e.tensor_tensor(out=lr_r[:], in0=lr_r[:], in1=dt_r[:], op=ALU.mult), reads=[lr_r, dt_r], writes=[lr_r])
        p.op(V, lambda e: e.tensor_tensor(out=li_r[:], in0=li_r[:], in1=dt_r[:], op=ALU.mult), reads=[li_r, dt_r], writes=[li_r])
        p.op("act", lambda e: e.activation(out=mg[:], in_=lr_r[:], func=AF.Exp, scale=negj[:, 0:1]), reads=[lr_r, negj], writes=[mg])
        p.op(V, lambda e: e.tensor_scalar(out=ang[:], in0=li_r[:], scalar1=jcol[:, 0:1], scalar2=None, op0=ALU.mult), reads=[li_r, jcol], writes=[ang])
        emit_sincos(p, (ang, ang[:]), (cs_r, cs_r[:]), (sn_r, sn_r[:]), (tmp_r, tmp_r[:]))
        p.op(V, lambda e: e.tensor_tensor(out=Er[:], in0=mg[:].rearrange("c (q s) -> c q s", q=4), in1=cs_r[:].rearrange("c (q s) -> c q s", q=4), op=ALU.mult),
             reads=[mg, cs_r], writes=[Er])
        p.op(V, lambda e: e.scalar_tensor_tensor(out=Ei[:], in0=mg[:].rearrange("c (q s) -> c q s", q=4), scalar=-1.0,
                                                 in1=sn_r[:].rearrange("c (q s) -> c q s", q=4), op0=ALU.mult, op1=ALU.mult),
             reads=[mg, sn_r], writes=[Ei])

        lr_s = load("lr_sp", [128, 4]); li_s = load("li_sp", [128, 4]); ldt_s = load("ldt_sp", [128, 4])
        dt_s = p.sb("dt_s", [128, 4])
        p.op("act", lambda e: e.activation(out=dt_s[:], in_=ldt_s[:], func=AF.Exp), reads=[ldt_s], writes=[dt_s])
        p.op(V, lambda e: e.tensor_tensor(out=lr_s[:], in0=lr_s[:], in1=dt_s[:], op=ALU.mult), reads=[lr_s, dt_s], writes=[lr_s])
        p.op(V, lambda e: e.tensor_tensor(out=li_s[:], in0=li_s[:], in1=dt_s[:], op=ALU.mult), reads=[li_s, dt_s], writes=[li_s])
        ang_s = p.sb("ang_s", [128, 4, 129]); mg_s = p.sb("mg_s", [128, 4, 129]); cs_s = p.sb("cs_s", [128, 4, 129]); sn_s = p.sb("sn_s", [128, 4, 129])
        tmp_s = p.sb("tmp_s", [128, 4, 129])
        for q in range(4):
            p.op("act", lambda e, q=q: e.activation(out=mg_s[:, q, :], in_=irow[:], func=AF.Exp, scale=lr_s[:, q:q + 1]), reads=[irow, lr_s], writes=[mg_s])
            p.op(V, lambda e, q=q: e.tensor_scalar(out=ang_s[:, q, :], in0=irow[:], scalar1=li_s[:, q:q + 1], scalar2=None, op0=ALU.mult),
                 reads=[irow, li_s], writes=[ang_s])
        emit_sincos(p, (ang_s, ang_s[:]), (cs_s, cs_s[:]), (sn_s, sn_s[:]), (tmp_s, tmp_s[:]))
        p.op(V, lambda e: e.tensor_tensor(out=Fr[:], in0=mg_s[:], in1=cs_s[:], op=ALU.mult), reads=[mg_s, cs_s], writes=[Fr])
        p.op(V, lambda e: e.tensor_tensor(out=Fi[:], in0=mg_s[:], in1=sn_s[:], op=ALU.mult), reads=[mg_s, sn_s], writes=[Fi])

    with p.scope():
        uT = p.sb("uT_sb", [128, L])
        NLD = 8
        uTv = [p.view(f"uT{i}", uT[:, i * (L // NLD):(i + 1) * (L // NLD)]) for i in range(NLD)]
        for i in range(NLD):
            p.dma("sp", uT[:, i * (L // NLD):(i + 1) * (L // NLD)], uT_d[:, i * (L // NLD):(i + 1) * (L // NLD)], writes=[uTv[i]])
        pX = [p.ps(f"pX{i}", [128, 4, 2, 128]) for i in range(1)]
        pZ = [p.ps(f"pZ{i}", [128, 4, 2, 128]) for i in range(1)]
        pY = [p.ps(f"pY{i}", [128, 128]) for i in range(2)]
        Xs = p.sb("Xs", [128, 4, 2, 128])
        Wt = p.sb("Wt", [128, 4, 2, 128])
        Zs = p.sb("Zs", [128, 4, 2, 128])
        Sr = p.sb("Sr", [128, 4, 128]); Si = p.sb("Si", [128, 4, 128])
        m1 = p.sb("m1", [128, 4, 128]); m2 = p.sb("m2", [128, 4, 128]); m3 = p.sb("m3", [128, 4, 128]); m4 = p.sb("m4", [128, 4, 128])
        cpr = p.sb("cpr", [128, 4]); cpi = p.sb("cpi", [128, 4])
        ca = p.sb("ca", [128, 4]); cb = p.sb("cb", [128, 4])
        p.op("dve", lambda e: e.memset(cpr[:], 0.0), writes=[cpr])
        p.op("dve", lambda e: e.memset(cpi[:], 0.0), writes=[cpi])
        yst = [p.sb(f"yst{i}", [128, 512]) for i in range(2)]
        BDf = BD[:].rearrange("c q r g s -> c (q r g s)")
        for c in range(NCH):
            uc = uT[:, c * 128:(c + 1) * 128]
            ub = uTv[c * 128 // (L // NLD)]
            px = pX[0]; pz = pZ[0]; py = pY[c % 2]
            pxf = px[:].rearrange("c q r s -> c (q r s)")
            for h in range(2):
                p.op("pe", lambda e, h=h, uc=uc, pxf=pxf: e.matmul(pxf[:, h * 512:(h + 1) * 512], lhsT=uc, rhs=BDf[:, h * 512:(h + 1) * 512], start=True, stop=True),
                     reads=[ub, BD], writes=[px])
            p.op("act", lambda e, px=px: e.activation(out=Xs[:], in_=px[:], func=AF.Copy), reads=[px], writes=[Xs])
            p.op("dve", lambda e: e.tensor_tensor(out=m1[:], in0=Xs[:, :, 0, :], in1=Er[:], op=ALU.mult), reads=[Xs, Er], writes=[m1])
            p.op("dve", lambda e: e.tensor_tensor(out=m2[:], in0=Xs[:, :, 1, :], in1=Ei[:], op=ALU.mult), reads=[Xs, Ei], writes=[m2])
            p.op("dve", lambda e: e.tensor_tensor(out=Wt[:, :, 0, :], in0=m1[:], in1=m2[:], op=ALU.subtract), reads=[m1, m2], writes=[Wt])
            p.op("pool", lambda e: e.tensor_tensor(out=m3[:], in0=Xs[:, :, 0, :], in1=Ei[:], op=ALU.mult), reads=[Xs, Ei], writes=[m3])
            p.op("pool", lambda e: e.tensor_tensor(out=m4[:], in0=Xs[:, :, 1, :], in1=Er[:], op=ALU.mult), reads=[Xs, Er], writes=[m4])
            p.op("pool", lambda e: e.tensor_tensor(out=Wt[:, :, 1, :], in0=m3[:], in1=m4[:], op=ALU.add), reads=[m3, m4, Wt], writes=[Wt])
            for q in range(4):
                for ri in range(2):
                    p.op("pe", lambda e, q=q, ri=ri, pz=pz: e.matmul(pz[:, q, ri, :], lhsT=Wt[:, q, ri, :], rhs=triT[:], start=True, stop=True),
                         reads=[Wt, triT], writes=[pz])
            for q in range(4):
                p.op("act", lambda e, q=q, pz=pz: e.activation(out=Zs[:, q, 0, :], in_=pz[:, q, 0, :], func=AF.Identity, bias=cpr[:, q:q + 1], scale=1.0),
                     reads=[pz, cpr], writes=[Zs])
                p.op("act", lambda e, q=q, pz=pz: e.activation(out=Zs[:, q, 1, :], in_=pz[:, q, 1, :], func=AF.Identity, bias=cpi[:, q:q + 1], scale=1.0),
                     reads=[pz, cpi], writes=[Zs])
            if c + 1 < NCH:
                p.op("dve", lambda e: e.tensor_tensor(out=ca[:], in0=Zs[:, :, 0, 127], in1=Fr[:, :, 128], op=ALU.mult), reads=[Zs, Fr], writes=[ca])
                p.op("dve", lambda e: e.tensor_tensor(out=cb[:], in0=Zs[:, :, 1, 127], in1=Fi[:, :, 128], op=ALU.mult), reads=[Zs, Fi], writes=[cb])
                p.op("dve", lambda e: e.tensor_tensor(out=cpr[:], in0=ca[:], in1=cb[:], op=ALU.subtract), reads=[ca, cb], writes=[cpr])
                p.op("dve", lambda e: e.tensor_tensor(out=ca[:], in0=Zs[:, :, 0, 127], in1=Fi[:, :, 128], op=ALU.mult), reads=[Zs, Fi], writes=[ca])
                p.op("dve", lambda e: e.tensor_tensor(out=cb[:], in0=Zs[:, :, 1, 127], in1=Fr[:, :, 128], op=ALU.mult), reads=[Zs, Fr], writes=[cb])
                p.op("dve", lambda e: e.tensor_tensor(out=cpi[:], in0=ca[:], in1=cb[:], op=ALU.add), reads=[ca, cb], writes=[cpi])
            p.op("dve", lambda e: e.tensor_tensor(out=m1[:], in0=Zs[:, :, 0, :], in1=Fr[:, :, 0:128], op=ALU.mult), reads=[Zs, Fr], writes=[m1])
            p.op("dve", lambda e: e.tensor_tensor(out=m2[:], in0=Zs[:, :, 1, :], in1=Fi[:, :, 0:128], op=ALU.mult), reads=[Zs, Fi], writes=[m2])
            p.op("dve", lambda e: e.tensor_tensor(out=Sr[:], in0=m1[:], in1=m2[:], op=ALU.subtract), reads=[m1, m2], writes=[Sr])
            p.op("pool", lambda e: e.tensor_tensor(out=m3[:], in0=Zs[:, :, 0, :], in1=Fi[:, :, 0:128], op=ALU.mult), reads=[Zs, Fi], writes=[m3])
            p.op("pool", lambda e: e.tensor_tensor(out=m4[:], in0=Zs[:, :, 1, :], in1=Fr[:, :, 0:128], op=ALU.mult), reads=[Zs, Fr], writes=[m4])
            p.op("pool", lambda e: e.tensor_tensor(out=Si[:], in0=m3[:], in1=m4[:], op=ALU.add), reads=[m3, m4], writes=[Si])
            n = 0
            for q in range(4):
                for ri, (CT, S) in enumerate(((CTre, Sr), (CTim, Si))):
                    p.op("pe", lambda e, q=q, CT=CT, S=S, py=py, n=n: e.matmul(py[:], lhsT=CT[:, q, :], rhs=S[:, q, :], start=(n == 0), stop=(n == 7)),
                         reads=[CT, S], writes=[py])
                    n += 1
            ys = yst[(c // 4) % 2]
            p.op("dve", lambda e, uc=uc, py=py, ys=ys, c=c: e.scalar_tensor_tensor(out=ys[:, (c % 4) * 128:(c % 4 + 1) * 128], in0=uc, scalar=d_ch[:, 0:1], in1=py[:],
                                                                             op0=ALU.mult, op1=ALU.add), reads=[ub, d_ch, py], writes=[ys])
            if c % 4 == 3:
                p.dma("sp", yT_o[:, (c - 3) * 128:(c + 1) * 128], ys[:], reads=[ys])


def s5_host_params(inp, j):
    f = np.float32
    gs = slice(8 * j, 8 * j + 8)
    lr = inp["ssm_lam_re"][0][gs]; li = inp["ssm_lam_im"][0][gs]; ldt = inp["ssm_log_dt"][0][gs]
    bre = inp["ssm_b_re"][0][gs]; bim = inp["ssm_b_im"][0][gs]
    cre = inp["ssm_c_re"][0][gs]; cim = inp["ssm_c_im"][0][gs]
    d = inp["ssm_d"][0][gs]
    prm = {}
    prm["lr_ch"] = np.repeat(lr, 16, axis=0); prm["li_ch"] = np.repeat(li, 16, axis=0)
    prm["ldt_ch"] = np.repeat(ldt, 16)[:, None]
    prm["bre_ch"] = bre.transpose(0, 2, 1).reshape(128, 64); prm["bim_ch"] = bim.transpose(0, 2, 1).reshape(128, 64)
    prm["d_ch"] = d.reshape(128, 1)
    prm["lr_row"] = np.broadcast_to(lr.reshape(1, 512), (128, 512)); prm["li_row"] = np.broadcast_to(li.reshape(1, 512), (128, 512))
    prm["ldt_row"] = np.broadcast_to(np.repeat(ldt, 64).reshape(1, 512), (128, 512))
    def sp(a):
        return a.reshape(4, 2, 64).transpose(1, 2, 0).reshape(128, 4)
    prm["lr_sp"] = sp(lr); prm["li_sp"] = sp(li); prm["ldt_sp"] = sp(np.repeat(ldt[:, None], 64, axis=1))
    CTre = np.zeros((128, 4, 128), f); CTim = np.zeros((128, 4, 128), f)
    for q in range(4):
        for gg in range(2):
            g = 2 * q + gg
            CTre[gg * 64:(gg + 1) * 64, q, 16 * g:16 * g + 16] = cre[g].T
            CTim[gg * 64:(gg + 1) * 64, q, 16 * g:16 * g + 16] = cim[g].T
    prm["CTre"] = CTre; prm["CTim"] = CTim
    prm["jcol"] = np.arange(128, dtype=f)[:, None]
    prm["irow"] = np.broadcast_to(np.arange(129, dtype=f)[None, :], (128, 129))
    prm["triT"] = np.triu(np.ones((128, 128), f))
    m = np.zeros((128, 8), f)
    for g in range(8):
        m[16 * g:16 * g + 16, g] = 1.0
    prm["maskBD"] = m
    return {k: np.ascontiguousarray(v, dtype=f) for k, v in prm.items()}


S5_SHAPES = dict(lr_ch=[128, 64], li_ch=[128, 64], ldt_ch=[128, 1], bre_ch=[128, 64], bim_ch=[128, 64], d_ch=[128, 1],
                 lr_row=[128, 512], li_row=[128, 512], ldt_row=[128, 512], lr_sp=[128, 4], li_sp=[128, 4], ldt_sp=[128, 4],
                 CTre=[128, 4, 128], CTim=[128, 4, 128], jcol=[128, 1], irow=[128, 129], triT=[128, 128], maskBD=[128, 8])


def build_S5():
    nc = bass.Bass("TRN2", target_bir_lowering=False)
    uT = nc.dram_tensor("uT", [128, L], F32, kind="ExternalInput").ap()
    prm = {k: nc.dram_tensor(k, s, F32, kind="ExternalInput").ap() for k, s in S5_SHAPES.items()}
    yT_o = nc.dram_tensor("yT_o", [128, L], F32, kind="ExternalOutput").ap()
    with ExitStack() as st:
        p = Prog(nc, st)
        emit_S5(p, uT, prm, yT_o)
        p.finish()
        p.replay()
        print("S5 instrs", p.ninstr())
    return nc

import math

L = 16384
NEGM = -30000.0
NQG = L // 512


def t5_bucket_np(dist):
    n = np.maximum(dist, 0)
    me = 16
    scaled = np.log(np.maximum(n, me).astype(np.float32) / np.float32(me)) / np.float32(math.log(1024 / 16))
    large = me + (scaled * np.float32(16)).astype(np.int32)
    return np.where(n < me, n, np.minimum(large, 31))


def nsa_tables(rel_bias, heads4):
    f = np.float32
    own = heads4[0]
    k = np.arange(128)[:, None]
    cols = np.arange(11 * 128)[None, :]
    dist = (cols // 128 - 3) * 128 + (cols % 128) - k
    bw = np.where((dist >= 0) & (dist < 512), rel_bias[t5_bucket_np(dist), own], f(NEGM)).astype(f)
    cols = np.arange(14 * 128)[None, :]
    dist = (cols // 128 - 3) * 128 + (cols % 128) - k
    bs = np.where(dist >= 0, rel_bias[t5_bucket_np(dist), own], f(NEGM)).astype(f)
    bc = np.zeros((128, 4, 6, 512), f)
    n_ = np.arange(128)[:, None]
    q_ = np.arange(512)[None, :]
    for hi, h in enumerate(heads4):
        for dl in range(6):
            dist = 512 * dl + q_ - 16 * n_ - 31
            bc[:, hi, dl, :] = np.where(dist >= 0, rel_bias[t5_bucket_np(dist), h], f(NEGM))
    b31 = np.broadcast_to(rel_bias[31, heads4][None, :], (128, 4)).astype(f)
    return dict(biasW=np.ascontiguousarray(bw), biasS=np.ascontiguousarray(bs), biasC=bc, b31c=np.ascontiguousarray(b31))


def nsa_consts():
    import ml_dtypes
    f = np.float32
    bf = ml_dtypes.bfloat16
    EXP = np.zeros((128, 64, 128), f)
    for kk in range(64):
        EXP[2 * kk, kk, 0:64] = 1.0
        EXP[2 * kk + 1, kk, 64:128] = 1.0
    ov = np.zeros((1024, 256), f)
    for n in range(1023):
        for j in range(max(0, (16 * n) // 64 - 1), min(256, (16 * n + 32) // 64 + 1)):
            o = min(16 * n + 32, 64 * j + 64) - max(16 * n, 64 * j)
            if o > 0:
                ov[n, j] = o / 16.0
    ovx = np.zeros((128, 8, 257), f)
    ovx[:, :, :256] = ov.reshape(8, 128, 256).transpose(1, 0, 2)
    ovx[:, :, 256] = 1.0
    pmul = np.zeros((128, 3), f)
    padd = np.zeros((128, 3), f)
    padd[:64] = [1e6, 2e6, -1.0]
    pmul[64:, 0] = 1.0
    padd[64:] = [0.0, 1e6, 2e6]
    return dict(EXP=EXP.astype(bf), ovx=ovx.astype(bf), pmul=pmul, padd=padd, ident=np.eye(128, dtype=f))


NSA_IN = dict(q4T=([4, 128, L], BF16), kcmpT=([128, L], BF16), vcmpT=([128, L], BF16), kslcT=([128, L], BF16), kwinT=([128, L], BF16),
              vslc=([L, 128], BF16), vwin=([L, 128], BF16), gates3=([L, 3], F32),
              w1k=([4096, 256], F32), w1v=([4096, 256], F32), w2k=([256, 128], F32), w2v=([256, 128], F32),
              pekT=([128, 32], F32), pevT=([128, 32], F32),
              biasW=([128, 11 * 128], F32), biasS=([128, 14 * 128], F32), biasC=([128, 4, 6, 512], F32), b31c=([128, 4], F32),
              EXP=([128, 64, 128], BF16), ovx=([128, 8, 257], BF16), pmul=([128, 3], F32), padd=([128, 3], F32), ident=([128, 128], F32))


def emit_NSA(p, d, out_T, nq=NQG):
    pS = [p.ps(f"pS{i}", [128, 512]) for i in range(2)]
    pU = [p.ps(f"pU{i}", [128, 512]) for i in range(2)]
    pO = [p.ps(f"pO{i}", [128, 512]) for i in range(4)]

    def load(name, shape, dt=F32, q="sp"):
        b = p.sb("n_" + name, shape, dt)
        p.dma(q, b[:], d[name], writes=[b])
        return b

    idf = load("ident", [128, 128])
    idb = p.sb("n_idb", [128, 128], BF16)
    cp(p, "dve", idb, idb[:], idf, idf[:])
    pmul = load("pmul", [128, 3])
    padd = load("padd", [128, 3])
    b31c = load("b31c", [128, 4])
    kcT = p.sb("kcT", [128, 1024], BF16)
    RC = p.sb("RC", [128, 8, 385], BF16)
    p.dma("sp", RC[:, :, 0:257], d["ovx"], writes=[RC])
    biasC = p.sb("biasC", [128, 4, 6, 512], BF16)
    biasS = p.sb("biasS", [128, 14 * 128], BF16)
    biasW = p.sb("biasW", [128, 11 * 128], BF16)

    with p.scope():
        stg = [p.sb(f"tstg{i}", [128, 3072]) for i in range(2)]
        n = 0
        for hh in range(4):
            s = stg[n % 2]; n += 1
            p.dma("sp", s[:], d["biasC"][:, hh].rearrange("p a b -> p (a b)"), writes=[s])
            cp(p, ("dve", "pool")[n % 2], biasC, biasC[:, hh].rearrange("p a b -> p (a b)"), s, s[:])
        s = stg[n % 2]; n += 1
        p.dma("sp", s[:, 0:1792], d["biasS"], writes=[s])
        cp(p, "dve", biasS, biasS[:], s, s[:, 0:1792])
        s = stg[n % 2]; n += 1
        p.dma("sp", s[:, 0:1408], d["biasW"], writes=[s])
        cp(p, "pool", biasW, biasW[:], s, s[:, 0:1408])

        kext = p.sb("kext", [128, L + 32], BF16)
        kview = kext[:].rearrange("d (n s) -> d s n", s=16)
        w1b = p.sb("w1b", [128, 32, 256], BF16)
        w1s = [p.sb(f"w1s{i}", [128, 8, 256]) for i in range(2)]
        w2s = p.sb("w2s", [128, 2, 128])
        w2b = p.sb("w2b", [128, 2, 128], BF16)
        pes = p.sb("pes", [128, 32])
        peRep = p.sb("peRep", [128, 32, 128], BF16)
        biasH = p.sb("biasH", [128, 2])
        hTb = p.sb("hTb", [128, 2, 1024], BF16)
        mset(p, "pool", kext, kext[:, L:L + 32], 0.0)
        for which in range(2):
            kname, w1n, w2n, pen = (("kcmpT", "w1k", "w2k", "pekT"), ("vcmpT", "w1v", "w2v", "pevT"))[which]
            for c4 in range(4):
                p.dma("sp", kext[:, c4 * 4096:(c4 + 1) * 4096], d[kname][:, c4 * 4096:(c4 + 1) * 4096], writes=[kext])
            w1v_ = d[w1n].rearrange("(pos d) h -> d pos h", d=128)
            for c4 in range(4):
                s = w1s[c4 % 2]
                p.dma("sp", s[:], w1v_[:, c4 * 8:(c4 + 1) * 8, :], writes=[s])
                cp(p, ("dve", "pool")[c4 % 2], w1b, w1b[:, c4 * 8:(c4 + 1) * 8, :], s, s[:])
            p.dma("sp", w2s[:], d[w2n].rearrange("(c h) d -> h c d", h=128), writes=[w2s])
            cp(p, "dve", w2b, w2b[:], w2s, w2s[:])
            p.dma("sp", pes[:], d[pen], writes=[pes])
            for pos in range(32):
                cp(p, "dve", peRep, peRep[:, pos, :], pes, pes[:, pos:pos + 1].to_broadcast([128, 128]))
            for hc in range(2):
                ps = pS[hc % 2]
                for pos in range(32):
                    mm(p, ps, ps[:, 0:128], w1b, w1b[:, pos, hc * 128:(hc + 1) * 128], peRep, peRep[:, pos, :], start=(pos == 0), stop=(pos == 31))
                act(p, biasH, biasH[:, hc:hc + 1], ps, ps[:, 0:1], AF.Copy)
            for hc in range(2):
                for nh in range(2):
                    ps = pS[(hc * 2 + nh) % 2]
                    for pos in range(32):
                        n0 = pos // 16 + nh * 512
                        mm(p, ps, ps[:], w1b, w1b[:, pos, hc * 128:(hc + 1) * 128], kext, kview[:, pos % 16, n0:n0 + 512], start=(pos == 0), stop=(pos == 31))
                    act(p, hTb, hTb[:, hc, nh * 512:(nh + 1) * 512], ps, ps[:], AF.Gelu_apprx_tanh, bias=(biasH, biasH[:, hc:hc + 1]))
            if which == 0:
                for nh in range(2):
                    ps = pS[nh % 2]
                    for hc in range(2):
                        mm(p, ps, ps[:], w2b, w2b[:, hc, :], hTb, hTb[:, hc, nh * 512:(nh + 1) * 512], start=(hc == 0), stop=(hc == 1))
                    cp(p, "act", kcT, kcT[:, nh * 512:(nh + 1) * 512], ps, ps[:])
                mset(p, "dve", kcT, kcT[:, 1023:1024], 0.0)
            else:
                for m in range(8):
                    ps = pS[m % 2]
                    for hc in range(2):
                        mm(p, ps, ps[:, 0:128], hTb, hTb[:, hc, m * 128:(m + 1) * 128], w2b, w2b[:, hc, :], start=(hc == 0), stop=(hc == 1))
                    cp(p, "act", RC, RC[:, m, 257:385], ps, ps[:, 0:128])

    EXP = p.sb("EXPm", [128, 64, 128], BF16)
    p.dma("sp", EXP[:], d["EXP"], writes=[EXP])
    kslc = p.sb("kslc", [128, L], BF16)
    vslc1 = p.sb("vslc1", [128, 128, 129], BF16)
    mset(p, "pool", vslc1, vslc1[:, :, 128:129], 1.0)
    nld = max(1, (nq * 512) // 2048)
    vsv = d["vslc"].rearrange("(n p) d -> p n d", p=128)
    for c in range(nld):
        p.dma("sp", kslc[:, c * 2048:(c + 1) * 2048], d["kslcT"][:, c * 2048:(c + 1) * 2048], writes=[kslc])
        p.dma("sp", vslc1[:, c * 16:(c + 1) * 16, 0:128], vsv[:, c * 16:(c + 1) * 16, :], writes=[vslc1])
    q4s = [p.sb(f"q4_{i}", [128, 4, 512], BF16) for i in range(2)]
    kwins = [p.sb(f"kwin{i}", [128, 1024], BF16) for i in range(2)]
    vwins = [p.sb(f"vwin{i}", [128, 8, 129], BF16) for i in range(2)]
    gts = [p.sb(f"gts{i}", [128, 4, 3]) for i in range(2)]
    for i in range(2):
        mset(p, "pool", vwins[i], vwins[i][:, :, 128:129], 1.0)
    PcT = [p.sb(f"PcT{i}", [128, 8, 512], BF16) for i in range(2)]
    PsT = [p.sb(f"PsT{i}", [128, 512], BF16) for i in range(3)]
    sc = p.sb("scr_sc", [128, 4, 256])
    scz = p.sb("scr_z", [128, 256])
    acc = p.sb("accO", [128, 4, 128])
    maddf = p.sb("maddf", [128, 256])
    maddT = p.sb("maddT", [128, 2, 512], BF16)
    m8 = p.sb("m8", [128, 16])
    thr = p.sb("thr", [128, 1])
    rden = p.sb("rden", [128, 1])
    wsc = p.sb("wsc", [128, 1])
    ost = [p.sb(f"ost{i}", [128, 512]) for i in range(2)]
    q4v = d["q4T"].rearrange("h d t -> d h t")
    vwv = d["vwin"].rearrange("(n p) d -> p n d", p=128)
    gv = d["gates3"].rearrange("(n p) g -> p n g", p=128)
    nps = 0
    npt = 0

    for Q in range(nq):
        q4 = q4s[Q % 2]; kwin = kwins[Q % 2]; vwin = vwins[Q % 2]; gt = gts[Q % 2]
        p.dma("sp", q4[:], q4v[:, :, Q * 512:(Q + 1) * 512], writes=[q4])
        t0 = max(0, 4 * Q - 4)
        woff = t0 - (4 * Q - 4)
        p.dma("sp", kwin[:, woff * 128:1024], d["kwinT"][:, t0 * 128:(4 * Q + 4) * 128], writes=[kwin])
        p.dma("sp", vwin[:, woff:8, 0:128], vwv[:, t0:4 * Q + 4, :], writes=[vwin])
        p.dma("sp", gt[:], gv[:, 4 * Q:4 * Q + 4, :], writes=[gt])
        nm = Q // 4 + 1
        for hh in range(4):
            pc = PcT[hh % 2]
            for m in range(nm):
                dl = Q - 4 * m
                ps = pS[nps % 2]; nps += 1
                mm(p, ps, ps[:], kcT, kcT[:, m * 128:(m + 1) * 128], q4, q4[:, hh, :], start=True, stop=(dl >= 6))
                if dl < 6:
                    mm(p, ps, ps[:], idb, idb[:], biasC, biasC[:, hh, dl, :], start=False, stop=True)
                    act(p, pc, pc[:, m, :], ps, ps[:], AF.Exp)
                else:
                    act(p, pc, pc[:, m, :], ps, ps[:], AF.Exp, bias=(b31c, b31c[:, hh:hh + 1]))
            for i in range(4):
                pu = pU[(hh * 4 + i) % 2]
                N = 385 if hh == 0 else 257
                for m in range(nm):
                    mm(p, pu, pu[:, 0:N], pc, pc[:, m, i * 128:(i + 1) * 128], RC, RC[:, m, 0:N], start=(m == 0), stop=(m == nm - 1))
                ts(p, "dve", rden, rden[:], pu, pu[:, 256:257], 1e-30, None, op0=ALU.add)
                p.op("dve", lambda e: e.reciprocal(out=rden[:], in_=rden[:]), reads=[rden], writes=[rden])
                if hh == 0:
                    ts(p, "dve", sc, sc[:, i, :], pu, pu[:, 0:256], (rden, rden[:, 0:1]), None, op0=ALU.mult)
                    tt(p, "dve", wsc, wsc[:], rden, rden[:], gt, gt[:, i, 0:1], ALU.mult)
                    ts(p, "dve", acc, acc[:, i, :], pu, pu[:, 257:385], (wsc, wsc[:, 0:1]), None, op0=ALU.mult)
                else:
                    stt(p, "dve", sc, sc[:, i, :], pu, pu[:, 0:256], (rden, rden[:, 0:1]), sc, sc[:, i, :], ALU.mult, ALU.add)
        for i in range(4):
            T = 4 * Q + i
            if 2 * T + 2 < 256:
                mset(p, "dve", sc, sc[:, i, 2 * T + 2:256], -1.0)
            if T >= 1:
                a = 2 * T - 1
                tt(p, "dve", sc, sc[:, i, a:a + 3], sc, sc[:, i, a:a + 3], pmul, pmul[:], ALU.mult)
                tt(p, "dve", sc, sc[:, i, a:a + 3], sc, sc[:, i, a:a + 3], padd, padd[:], ALU.add)
            else:
                tt(p, "dve", sc, sc[:, i, 0:2], sc, sc[:, i, 0:2], pmul, pmul[:, 1:3], ALU.mult)
                tt(p, "dve", sc, sc[:, i, 0:2], sc, sc[:, i, 0:2], padd, padd[:, 1:3], ALU.add)
            mset(p, "dve", sc, sc[:, i, 0:1], 3e6)
            p.op("dve", lambda e, i=i: e.max(out=m8[:, 0:8], in_=sc[:, i, :]), reads=[sc], writes=[m8])
            p.op("dve", lambda e, i=i: e.match_replace(out=scz[:], in_to_replace=m8[:, 0:8], in_values=sc[:, i, :], imm_value=-2.0),
                 reads=[sc, m8], writes=[scz])
            p.op("dve", lambda e: e.max(out=m8[:, 8:16], in_=scz[:]), reads=[scz], writes=[m8])
            ts(p, "dve", thr, thr[:], m8, m8[:, 15:16], 0.0, None, op0=ALU.max)
            ts(p, "dve", maddf, maddf[:], sc, sc[:, i, :], (thr, thr[:, 0:1]), None, op0=ALU.is_ge)
            ts(p, "dve", maddf, maddf[:], maddf, maddf[:], -NEGM, NEGM, op0=ALU.mult, op1=ALU.add)
            for hf in range(2):
                pt = pU[npt % 2]; npt += 1
                tr(p, pt, pt[:, 0:128], maddf, maddf[:, hf * 128:(hf + 1) * 128], idf, idf[:])
                cp(p, "dve", maddT, maddT[:, hf, i * 128:(i + 1) * 128], pt, pt[:, 0:128])
        for kt in range(4 * Q + 4):
            i0 = max(0, kt - 4 * Q)
            c0 = i0 * 128
            near = (4 * Q - kt) <= 7
            ps = pS[nps % 2]; nps += 1
            pst = PsT[nps % 3]
            mm(p, ps, ps[:, c0:512], kslc, kslc[:, kt * 128:(kt + 1) * 128], q4, q4[:, 0, c0:512], start=True, stop=False)
            mm(p, ps, ps[:, c0:512], EXP, EXP[:, kt % 64, :], maddT, maddT[:, kt // 64, c0:512], start=False, stop=(not near))
            if near:
                b0 = (4 * Q - kt + 3) * 128
                mm(p, ps, ps[:, c0:512], idb, idb[:], biasS, biasS[:, b0 + c0:b0 + 512], start=False, stop=True)
                act(p, pst, pst[:, c0:512], ps, ps[:, c0:512], AF.Exp)
            else:
                act(p, pst, pst[:, c0:512], ps, ps[:, c0:512], AF.Exp, bias=(b31c, b31c[:, 0:1]))
            for i in range(i0, 4):
                po = pO[i]
                mm(p, po, po[:, 0:129], pst, pst[:, i * 128:(i + 1) * 128], vslc1, vslc1[:, kt, :], start=(kt == 0), stop=(kt == 4 * Q + i))
        for i in range(4):
            po = pO[i]
            p.op("dve", lambda e, po=po: e.reciprocal(out=rden[:], in_=po[:, 128:129]), reads=[po], writes=[rden])
            tt(p, "dve", wsc, wsc[:], rden, rden[:], gt, gt[:, i, 1:2], ALU.mult)
            stt(p, "dve", acc, acc[:, i, :], po, po[:, 0:128], (wsc, wsc[:, 0:1]), acc, acc[:, i, :], ALU.mult, ALU.add)
        for kt in range(t0, 4 * Q + 4):
            i0 = max(0, kt - 4 * Q)
            i1 = min(3, kt + 4 - 4 * Q)
            c0, c1 = i0 * 128, (i1 + 1) * 128
            slot = kt - (4 * Q - 4)
            ps = pS[nps % 2]; nps += 1
            pst = PsT[nps % 3]
            b0 = (4 * Q - kt + 3) * 128
            mm(p, ps, ps[:, c0:c1], kwin, kwin[:, slot * 128:(slot + 1) * 128], q4, q4[:, 0, c0:c1], start=True, stop=False)
            mm(p, ps, ps[:, c0:c1], idb, idb[:], biasW, biasW[:, b0 + c0:b0 + c1], start=False, stop=True)
            act(p, pst, pst[:, c0:c1], ps, ps[:, c0:c1], AF.Exp)
            for i in range(i0, i1 + 1):
                po = pO[i]
                mm(p, po, po[:, 0:129], pst, pst[:, i * 128:(i + 1) * 128], vwin, vwin[:, slot, :],
                   start=(kt == max(0, 4 * Q + i - 4)), stop=(kt == 4 * Q + i))
        o = ost[Q % 2]
        for i in range(4):
            po = pO[i]
            p.op("dve", lambda e, po=po: e.reciprocal(out=rden[:], in_=po[:, 128:129]), reads=[po], writes=[rden])
            tt(p, "dve", wsc, wsc[:], rden, rden[:], gt, gt[:, i, 2:3], ALU.mult)
            stt(p, "dve", acc, acc[:, i, :], po, po[:, 0:128], (wsc, wsc[:, 0:1]), acc, acc[:, i, :], ALU.mult, ALU.add)
            pt = pU[npt % 2]; npt += 1
            tr(p, pt, pt[:, 0:128], acc, acc[:, i, :], idf, idf[:])
            cp(p, "dve", o, o[:, i * 128:(i + 1) * 128], pt, pt[:, 0:128])
        p.dma("act", out_T[:, Q * 512:(Q + 1) * 512], o[:], reads=[o])


def build_NSA(nq=NQG):
    nc = bass.Bass("TRN2", target_bir_lowering=False)
    d = {k: nc.dram_tensor(k, s, dt, kind="ExternalInput").ap() for k, (s, dt) in NSA_IN.items()}
    out_T = nc.dram_tensor("yattT_o", [128, L], F32, kind="ExternalOutput").ap()
    with ExitStack() as st:
        p = Prog(nc, st)
        emit_NSA(p, d, out_T, nq)
        p.finish()
        p.replay()
        print("NSA instrs", p.ninstr())
    return nc


def nsa_host_inputs(inp, A, j):
    g = j // 4
    heads4 = [j] + [h for h in range(4 * g, 4 * g + 4) if h != j]
    fm = A["fm"]
    m = {}
    m["q4T"] = np.ascontiguousarray(np.stack([fm[h * 128:(h + 1) * 128] for h in heads4]))
    base = 1024
    m["kcmpT"] = np.ascontiguousarray(fm[base + (0 + g) * 128: base + (1 + g) * 128])
    m["vcmpT"] = np.ascontiguousarray(fm[base + (2 + g) * 128: base + (3 + g) * 128])
    m["kslcT"] = np.ascontiguousarray(fm[base + (4 + g) * 128: base + (5 + g) * 128])
    m["kwinT"] = np.ascontiguousarray(fm[base + (6 + g) * 128: base + (7 + g) * 128])
    m["vslc"] = np.ascontiguousarray(A["vtm"][:, g * 128:(g + 1) * 128])
    m["vwin"] = np.ascontiguousarray(A["vtm"][:, 256 + g * 128:256 + (g + 1) * 128])
    m["gates3"] = np.ascontiguousarray(A["gates"].reshape(L, 3, 8)[:, :, j])
    m["w1k"] = inp["cmp_w1_k"][0]; m["w1v"] = inp["cmp_w1_v"][0]; m["w2k"] = inp["cmp_w2_k"][0]; m["w2v"] = inp["cmp_w2_v"][0]
    m["pekT"] = np.ascontiguousarray(inp["cmp_pe_k"][0].T); m["pevT"] = np.ascontiguousarray(inp["cmp_pe_v"][0].T)
    m.update(nsa_tables(inp["rel_bias"], heads4))
    m.update(nsa_consts())
    return m


DS = 1024


def build_C1():
    nc = bass.Bass("TRN2", target_bir_lowering=False)
    I = {}
    def din(name, shape, dt=F32):
        I[name] = nc.dram_tensor(name, shape, dt, kind="ExternalInput").ap()
    din("x", [TPC, D]); din("ypreT", [DS, TPC]); din("yattT", [DS, TPC])
    din("g1row", [128, D]); din("sh2T", [128, 16]); din("sc2T", [128, 16])
    din("n2T", [128, 16]); din("wglu", [DS, DS]); din("bgluT", [128, 8]); din("bsT", [128, 8]); din("baT", [128, 8])
    din("wout", [2 * DS, D]); din("ident", [128, 128])
    O = {}
    O["x1_o"] = nc.dram_tensor("x1_o", [TPC, D], F32, kind="ExternalOutput").ap()
    O["h2T_o"] = nc.dram_tensor("h2T_o", [D, TPC], BF16, kind="ExternalOutput").ap()
    with ExitStack() as st:
        p = Prog(nc, st)
        emit_C1(p, I, O)
        p.finish()
        p.replay()
        print("C1 instrs", p.ninstr())
    return nc


def emit_C1(p, I, O):
    ptr = [p.ps(f"ptr{i}", [128, 1024], BF16) for i in range(2)]
    pg = p.ps("pg", [128, 512])
    pA = p.ps("pA", [128, 512])
    pB = p.ps("pB", [128, 512])
    pq = p.ps("pq", [128, 512])

    idf = p.sb("idf", [128, 128]); idb = p.sb("idb", [128, 128], BF16)
    p.dma("sp", idf[:], I["ident"], writes=[idf])
    cp(p, "dve", idb, idb[:], idf, idf[:])
    g1row = p.sb("g1row", [128, D])
    p.dma("sp", g1row[:], I["g1row"], writes=[g1row])
    sh = p.sb("sh2", [128, 16]); sc2 = p.sb("sc2", [128, 16])
    p.dma("sp", sh[:], I["sh2T"], writes=[sh]); p.dma("sp", sc2[:], I["sc2T"], writes=[sc2])
    ng = p.sb("ng2", [128, 16]); gam = p.sb("gam2", [128, 16])
    p.dma("sp", ng[:], I["n2T"], writes=[ng])
    stt(p, "dve", gam, gam[:], sc2, sc2[:], 1.0, ng, ng[:], ALU.add, ALU.mult)
    bglu = p.sb("bglu", [128, 8]); bsT = p.sb("bsT", [128, 8]); baT = p.sb("baT", [128, 8])
    p.dma("sp", bglu[:], I["bgluT"], writes=[bglu]); p.dma("sp", bsT[:], I["bsT"], writes=[bsT]); p.dma("sp", baT[:], I["baT"], writes=[baT])
    ones = p.sb("ones32", [128, 32])
    mset(p, "dve", ones, ones[:], 1.0)

    woutb = p.sb("woutb", [128, 16, D], BF16)
    wglub = p.sb("wglub", [128, 8, DS], BF16)
    with p.scope():
        stg = [p.sb(f"wstg{i}", [128, D]) for i in range(2)]
        for kc in range(16):
            s = stg[kc % 2]
            p.dma("sp", s[:], I["wout"][kc * 128:(kc + 1) * 128, :], writes=[s])
            cp(p, ("dve", "pool")[kc % 2], woutb, woutb[:, kc, :], s, s[:])
        for kc in range(8):
            s = stg[kc % 2]
            p.dma("sp", s[:, 0:DS], I["wglu"][kc * 128:(kc + 1) * 128, :], writes=[s])
            cp(p, ("dve", "pool")[kc % 2], wglub, wglub[:, kc, :], s, s[:, 0:DS])

    ybuf = p.sb("ybuf", [128, 8, 512])
    zf = p.sb("zf", [128, 8, 512])
    zb = p.sb("zb", [128, 8, 512], BF16)
    sq = p.sb("sqb", [128, 8, 512])
    sig = p.sb("sig", [128, 512])
    s5ob = p.sb("s5ob", [128, 8, 512], BF16)
    attb = p.sb("attb", [128, 8, 512], BF16)
    rs = p.sb("rs_s", [128, 4]); ra = p.sb("rs_a", [128, 4])
    xt = p.sb("xt", [128, D]); x1 = p.sb("x1", [128, D])
    t1 = p.sb("t1c", [128, 512])
    scr = p.sb("sqscr", [128, D], BF16); xn = p.sb("xn", [128, D], BF16)
    ssq = p.sb("ssq", [128, 1]); rstd = p.sb("rstd", [128, 1])
    hT = p.sb("hT", [128, 16, 512], BF16)
    hTk = [p.view(f"hT_{kc}", hT[:, kc, :]) for kc in range(16)]
    ypv = I["ypreT"].rearrange("(c p) t -> p c t", p=128)
    yav = I["yattT"].rearrange("(c p) t -> p c t", p=128)
    h2v = O["h2T_o"].rearrange("(c p) t -> p c t", p=128)

    def rms_rstd(src, dst):
        tt(p, "pool", sq, sq[:], src, src[:], src, src[:], ALU.mult)
        for i in range(4):
            for cc in range(8):
                mm(p, pq, pq[:, 0:32], sq, sq[:, cc, i * 128:(i + 1) * 128], ones, ones[:], start=(cc == 0), stop=(cc == 7))
            ts(p, "dve", dst, dst[:, i:i + 1], pq, pq[:, 0:1], 1.0 / DS, 1e-6, op0=ALU.mult, op1=ALU.add)
        p.op("act", lambda e: e.sqrt(out=dst[:], in_=dst[:]), reads=[dst], writes=[dst])
        p.op("dve", lambda e: e.reciprocal(out=dst[:], in_=dst[:]), reads=[dst], writes=[dst])

    for tg in range(TPC // 512):
        ts_ = slice(tg * 512, (tg + 1) * 512)
        p.dma("sp", ybuf[:], ypv[:, :, ts_], writes=[ybuf])
        act(p, zf, zf[:], ybuf, ybuf[:], AF.Gelu_apprx_tanh)
        cp(p, "pool", zb, zb[:], zf, zf[:])
        for cc in range(8):
            for kc in range(8):
                mm(p, pg, pg[:], wglub, wglub[:, kc, cc * 128:(cc + 1) * 128], zb, zb[:, kc, :], start=(kc == 0), stop=(kc == 7))
            act(p, sig, sig[:], pg, pg[:], AF.Sigmoid, bias=(bglu, bglu[:, cc:cc + 1]))
            tt(p, "dve", zf, zf[:, cc, :], zf, zf[:, cc, :], sig, sig[:], ALU.mult)
        rms_rstd(zf, rs)
        for cc in range(8):
            ts(p, "dve", s5ob, s5ob[:, cc, :], zf, zf[:, cc, :], (bsT, bsT[:, cc:cc + 1]), None, op0=ALU.mult)
        p.dma("sp", ybuf[:], yav[:, :, ts_], writes=[ybuf])
        rms_rstd(ybuf, ra)
        for cc in range(8):
            ts(p, "dve", attb, attb[:, cc, :], ybuf, ybuf[:, cc, :], (baT, baT[:, cc:cc + 1]), None, op0=ALU.mult)
        for i in range(4):
            t = tg * 4 + i
            p.dma("sp", xt[:], I["x"][t * 128:(t + 1) * 128, :], writes=[xt])
            for cg in range(4):
                cs = slice(cg * 512, (cg + 1) * 512)
                for c in range(8):
                    mm(p, pA, pA[:], s5ob, s5ob[:, c, i * 128:(i + 1) * 128], woutb, woutb[:, c, cs], start=(c == 0), stop=(c == 7))
                for c in range(8):
                    mm(p, pB, pB[:], attb, attb[:, c, i * 128:(i + 1) * 128], woutb, woutb[:, 8 + c, cs], start=(c == 0), stop=(c == 7))
                ts(p, "dve", t1, t1[:], pA, pA[:], (rs, rs[:, i:i + 1]), None, op0=ALU.mult)
                stt(p, "dve", t1, t1[:], pB, pB[:], (ra, ra[:, i:i + 1]), t1, t1[:], ALU.mult, ALU.add)
                tt(p, "pool", t1, t1[:], t1, t1[:], g1row, g1row[:, cs], ALU.mult)
                tt(p, "pool", x1, x1[:, cs], t1, t1[:], xt, xt[:, cs], ALU.add)
            p.dma("act", O["x1_o"][t * 128:(t + 1) * 128, :], x1[:], reads=[x1])
            emit_norm_transpose(p, x1, hT, hTk, i * 128, gam, sh, idb, scr, ssq, rstd, xn, ptr)
        p.dma("act", h2v[:, :, ts_], hT[:], reads=hTk)


NE = 16384
EG = 256
NEG_ = NE // EG
GTH_RELAX = 1.0 - 1e-4


def build_C2(npass=4, neg=NEG_):
    nc = bass.Bass("TRN2", target_bir_lowering=False)
    I = {}
    def din(name, shape, dt=F32):
        I[name] = nc.dram_tensor(name, shape, dt, kind="ExternalInput").ap()
    din("x1", [TPC, D]); din("h2T", [D, TPC], BF16); din("g2row", [128, D]); din("fgrow", [128, D])
    din("wq", [D, D]); din("k1T", [128, 128]); din("k2T", [128, 128]); din("pu", [NE, D]); din("pv", [NE, D]); din("ident", [128, 128])
    out = nc.dram_tensor("out", [TPC, D], F32, kind="ExternalOutput").ap()
    with ExitStack() as st:
        p = Prog(nc, st)
        emit_C2(p, I, out, npass, neg)
        p.finish()
        p.replay()
        print("C2 instrs", p.ninstr())
    return nc


def emit_C2(p, I, out, npass=4, neg=NEG_):
    psc = p.ps("psc", [128, 512])
    ptr = p.ps("ptrU", [128, 1024], BF16)
    ptw = p.ps("ptrW", [128, 1024], BF16)
    pv = [p.ps(f"pv{i}", [128, 512]) for i in range(4)]

    idf = p.sb("idf", [128, 128]); idb = p.sb("idb", [128, 128], BF16)
    p.dma("sp", idf[:], I["ident"], writes=[idf])
    cp(p, "dve", idb, idb[:], idf, idf[:])
    k1b = p.sb("k1b", [128, 128], BF16); k2b = p.sb("k2b", [128, 128], BF16)
    kst = p.sb("kst", [128, 128])
    p.dma("sp", kst[:], I["k1T"], writes=[kst]); cp(p, "dve", k1b, k1b[:], kst, kst[:])
    p.dma("sp", kst[:], I["k2T"], writes=[kst]); cp(p, "dve", k2b, k2b[:], kst, kst[:])
    g2row = p.sb("g2row", [128, D]); fgrow = p.sb("fgrow", [128, D])
    p.dma("sp", g2row[:], I["g2row"], writes=[g2row]); p.dma("sp", fgrow[:], I["fgrow"], writes=[fgrow])

    h2T = p.sb("h2T", [128, 16, 512], BF16)
    acc = p.sb("acc", [128, 4, D])
    E2 = p.sb("E2", [128, 4, 8, 128]); Rg = p.sb("Rg", [128, 4, 8, 128]); gth = p.sb("gth", [128, 4, 8])
    h2v = I["h2T"].rearrange("(c p) t -> p c t", p=128)
    wqv = I["wq"].rearrange("(k p) n -> p k n", p=128)
    uv = I["pu"].rearrange("(g a p) d -> g p a d", a=2, p=128)
    vv = I["pv"].rearrange("(g a p) d -> g p a d", a=2, p=128)

    for ps_ in range(npass):
        tsl = slice(ps_ * 512, (ps_ + 1) * 512)
        p.dma("sp", h2T[:], h2v[:, :, tsl], writes=[h2T])
        with p.scope():
            qpT = p.sb("qpT", [128, 16, 512], BF16)
            wqs = [p.sb(f"wqs{i}", [128, 16, 128]) for i in range(2)]
            wqb = [p.sb(f"wqb{i}", [128, 16, 128], BF16) for i in range(2)]
            s12 = p.sb("s12", [128, 2, 128]); scz = p.sb("sczp", [128, 256]); m16 = p.sb("m16", [128, 2, 16])
            cand = p.sb("cand", [128, 16, 16]); c16 = p.sb("c16", [128, 16]); ez = p.sb("ez", [128, 16])
            mm_ = p.sb("mmx", [128, 1]); th = p.sb("thx", [128, 1]); negm = p.sb("negm", [128, 1]); Z = p.sb("Zx", [128, 1]); lnZ = p.sb("lnZ", [128, 1])
            m1 = p.sb("m1x", [128, 1]); m2 = p.sb("m2x", [128, 1]); b1 = p.sb("b1x", [128, 1]); b2 = p.sb("b2x", [128, 1]); bg = p.sb("bgx", [128, 1])
            for ch in range(16):
                s = wqs[ch % 2]; sb_ = wqb[ch % 2]
                p.dma("sp", s[:], wqv[:, :, ch * 128:(ch + 1) * 128], writes=[s])
                cp(p, "pool", sb_, sb_[:], s, s[:])
                for kc in range(16):
                    mm(p, psc, psc[:], sb_, sb_[:, kc, :], h2T, h2T[:, kc, :], start=(kc == 0), stop=(kc == 15))
                cp(p, "act", qpT, qpT[:, ch, :], psc, psc[:])
            for i in range(4):
                for h in range(8):
                    mm(p, psc, psc[:, 0:128], qpT, qpT[:, 2 * h, i * 128:(i + 1) * 128], k1b, k1b[:])
                    mm(p, psc, psc[:, 128:256], qpT, qpT[:, 2 * h + 1, i * 128:(i + 1) * 128], k2b, k2b[:])
                    cp(p, "act", s12, s12[:].rearrange("p a b -> p (a b)"), psc, psc[:, 0:256])
                    for a in range(2):
                        p.op("dve", lambda e, a=a: e.max(out=m16[:, a, 0:8], in_=s12[:, a, :]), reads=[s12], writes=[m16])
                        p.op("dve", lambda e, a=a: e.match_replace(out=scz[:, 0:128], in_to_replace=m16[:, a, 0:8], in_values=s12[:, a, :], imm_value=-1e30),
                             reads=[s12, m16], writes=[scz])
                        p.op("dve", lambda e, a=a: e.max(out=m16[:, a, 8:16], in_=scz[:, 0:128]), reads=[scz], writes=[m16])
                    tt(p, "dve", cand, cand[:], m16, m16[:, 0, :, None].to_broadcast([128, 16, 16]), m16, m16[:, 1, None, :].to_broadcast([128, 16, 16]), ALU.add)
                    cflat = cand[:].rearrange("p a b -> p (a b)")
                    p.op("dve", lambda e: e.max(out=c16[:, 0:8], in_=cflat), reads=[cand], writes=[c16])
                    p.op("dve", lambda e: e.match_replace(out=scz[:], in_to_replace=c16[:, 0:8], in_values=cflat, imm_value=-1e30), reads=[cand, c16], writes=[scz])
                    p.op("dve", lambda e: e.max(out=c16[:, 8:16], in_=scz[:]), reads=[scz], writes=[c16])
                    p.op("dve", lambda e: e.tensor_reduce(out=mm_[:], in_=c16[:, 0:8], axis=AX.X, op=ALU.max), reads=[c16], writes=[mm_])
                    p.op("dve", lambda e: e.tensor_reduce(out=th[:], in_=c16[:, 8:16], axis=AX.X, op=ALU.min), reads=[c16], writes=[th])
                    p.op("dve", lambda e: e.tensor_reduce(out=m1[:], in_=m16[:, 0, 0:8], axis=AX.X, op=ALU.max), reads=[m16], writes=[m1])
                    p.op("dve", lambda e: e.tensor_reduce(out=m2[:], in_=m16[:, 1, 0:8], axis=AX.X, op=ALU.max), reads=[m16], writes=[m2])
                    ts(p, "dve", negm, negm[:], mm_, mm_[:], -1.0, None, op0=ALU.mult)
                    act(p, ez, ez[:], c16, c16[:], AF.Exp, bias=(negm, negm[:, 0:1]), accum=(Z, Z[:]))
                    act(p, lnZ, lnZ[:], Z, Z[:], AF.Ln)
                    ts(p, "dve", b2, b2[:], m2, m2[:], -1.0, None, op0=ALU.mult)
                    stt(p, "dve", b1, b1[:], m1, m1[:], -1.0, lnZ, lnZ[:], ALU.mult, ALU.subtract)
                    tt(p, "dve", bg, bg[:], th, th[:], mm_, mm_[:], ALU.subtract)
                    tt(p, "dve", bg, bg[:], bg, bg[:], lnZ, lnZ[:], ALU.subtract)
                    act(p, E2, E2[:, i, h, :], s12, s12[:, 1, :], AF.Exp, bias=(b2, b2[:, 0:1]))
                    act(p, Rg, Rg[:, i, h, :], s12, s12[:, 0, :], AF.Exp, bias=(b1, b1[:, 0:1]))
                    act(p, gth, gth[:, i, h:h + 1], bg, bg[:], AF.Exp)
            ts(p, "dve", gth, gth[:], gth, gth[:], GTH_RELAX, None, op0=ALU.mult)
        with p.scope():
            ust = p.sb("ust", [128, 2, D]); vst = p.sb("vst", [128, 2, D])
            ub = p.sb("ub", [128, 2, D], BF16); vb = p.sb("vb", [128, 2, D], BF16)
            uT = p.sb("uT", [128, 16, EG], BF16)
            A_ = [p.sb(f"Agelu{k}", [128, 2, 128]) for k in range(2)]
            G_ = [p.sb(f"Ggrid{k}", [128, 8, 2, 128]) for k in range(2)]
            M_ = [p.sb(f"Mgrid{k}", [128, 8, 2, 128]) for k in range(2)]
            Wg_ = [p.sb(f"Wg{k}", [128, 2, 128]) for k in range(2)]
            WA_ = [p.sb(f"WA{k}", [128, 2, 128], BF16) for k in range(2)]
            WAT_ = [p.sb(f"WAT{k}", [128, 2, 128], BF16) for k in range(2)]
            for eg in range(neg):
                p.dma("sp", ust[:], uv[eg], writes=[ust])
                cp(p, "act", ub, ub[:], ust, ust[:])
                for a in range(2):
                    for half in range(2):
                        for k8 in range(8):
                            kc = half * 8 + k8
                            tr(p, ptr, ptr[:, k8 * 128:(k8 + 1) * 128], ub, ub[:, a, kc * 128:(kc + 1) * 128], idb, idb[:])
                        cp(p, "act", uT, uT[:, half * 8:(half + 1) * 8, a * 128:(a + 1) * 128], ptr, ptr[:].rearrange("p (k e) -> p k e", e=128))
                p.dma("sp", vst[:], vv[eg], writes=[vst])
                cp(p, "act", vb, vb[:], vst, vst[:])
                for i in range(4):
                    A = A_[i % 2]; G = G_[i % 2]; M = M_[i % 2]; Wg = Wg_[i % 2]; WA = WA_[i % 2]; WAT = WAT_[i % 2]
                    for kc in range(16):
                        mm(p, psc, psc[:, 0:EG], h2T, h2T[:, kc, i * 128:(i + 1) * 128], uT, uT[:, kc, :], start=(kc == 0), stop=(kc == 15))
                    act(p, A, A[:].rearrange("p a b -> p (a b)"), psc, psc[:, 0:EG], AF.Gelu_apprx_tanh)
                    tt(p, "dve", G, G[:], Rg, Rg[:, i, :, 2 * eg:2 * eg + 2, None].to_broadcast([128, 8, 2, 128]),
                       E2, E2[:, i, :, None, :].to_broadcast([128, 8, 2, 128]), ALU.mult)
                    tt(p, "dve", M, M[:], G, G[:], gth, gth[:, i, :, None, None].to_broadcast([128, 8, 2, 128]), ALU.is_ge)
                    tt(p, "pool", M, M[:], M, M[:], G, G[:], ALU.mult)
                    p.op("dve", lambda e, M=M, Wg=Wg: e.tensor_reduce(out=Wg[:], in_=M[:].rearrange("p h a b -> p a b h"), axis=AX.X, op=ALU.add), reads=[M], writes=[Wg])
                    tt(p, "dve", WA, WA[:], A, A[:], Wg, Wg[:], ALU.mult)
                    for a in range(2):
                        tr(p, ptw, ptw[:, a * 128:(a + 1) * 128], WA, WA[:, a, :], idb, idb[:])
                    cp(p, "act", WAT, WAT[:].rearrange("p a b -> p (a b)"), ptw, ptw[:, 0:256])
                    for cg in range(4):
                        for a in range(2):
                            mm(p, pv[cg], pv[cg][:], WAT, WAT[:, a, :], vb, vb[:, a, cg * 512:(cg + 1) * 512], start=(a == 0), stop=(a == 1))
                        if eg == 0:
                            cp(p, "dve", acc, acc[:, i, cg * 512:(cg + 1) * 512], pv[cg], pv[cg][:])
                        else:
                            tt(p, "dve", acc, acc[:, i, cg * 512:(cg + 1) * 512], pv[cg], pv[cg][:], acc, acc[:, i, cg * 512:(cg + 1) * 512], ALU.add)
        with p.scope():
            x1t = p.sb("x1t", [128, D]); x2 = p.sb("x2", [128, D]); scr = p.sb("fscr", [128, D], BF16)
            ssq = p.sb("fssq", [128, 1]); rstd = p.sb("frstd", [128, 1])
            for i in range(4):
                t = ps_ * 4 + i
                p.dma("sp", x1t[:], I["x1"][t * 128:(t + 1) * 128, :], writes=[x1t])
                tt(p, "pool", x2, x2[:], acc, acc[:, i, :], g2row, g2row[:], ALU.mult)
                tt(p, "pool", x2, x2[:], x2, x2[:], x1t, x1t[:], ALU.add)
                act(p, scr, scr[:], x2, x2[:], AF.Square, accum=(ssq, ssq[:]))
                ts(p, "dve", rstd, rstd[:], ssq, ssq[:], 1.0 / D, 1e-6, op0=ALU.mult, op1=ALU.add)
                p.op("act", lambda e: e.sqrt(out=rstd[:], in_=rstd[:]), reads=[rstd], writes=[rstd])
                p.op("dve", lambda e: e.reciprocal(out=rstd[:], in_=rstd[:]), reads=[rstd], writes=[rstd])
                ts(p, "dve", x2, x2[:], x2, x2[:], (rstd, rstd[:, 0:1]), None, op0=ALU.mult)
                tt(p, "pool", x2, x2[:], x2, x2[:], fgrow, fgrow[:], ALU.mult)
                p.dma("act", out[t * 128:(t + 1) * 128, :], x2[:], reads=[x2])


def _ac(a, dt=None):
    return np.ascontiguousarray(a if dt is None else np.asarray(a, dtype=dt))


def _run(nc, maps):
    return run_bass_kernel_spmd(nc, maps, core_ids=list(range(len(maps)))).results


def kernel(**inp):
    f = np.float32
    NCR = 8
    ident = np.eye(128, dtype=f)
    x = inp["x"][0]
    cTl = _ac(inp["c"][0].reshape(16, 128).T)
    T16 = lambda v: _ac(np.asarray(v).reshape(16, 128).T)
    wa = inp["w_ada"][0]
    ba = inp["b_ada"][0]
    m0 = [dict(cT=cTl, wada=_ac(wa[:, c * NC0:(c + 1) * NC0]), badaT=_ac(ba[c * NC0:(c + 1) * NC0].reshape(NC0 // 128, 128).T), ident=ident)
          for c in range(NCR)]
    r0 = _run(build_L0(), m0)
    cond = np.concatenate([np.asarray(r["condT_o"]).T.reshape(-1) for r in r0])
    sh1, sc1, g1, sh2, sc2, g2 = np.split(cond, 6)
    mA = [dict(x=_ac(x[c * TPC:(c + 1) * TPC]), n1T=T16(inp["norm1_g"][0]), sh1T=T16(sh1), sc1T=T16(sc1), w_in=inp["w_in"][0], ident=ident)
          for c in range(NCR)]
    rA = _run(build_A(), mA)
    fm = np.concatenate([np.asarray(r["fm_o"]) for r in rA], axis=1)
    uT = np.concatenate([np.asarray(r["uT_o"]) for r in rA], axis=1)
    vtm = np.concatenate([np.asarray(r["vtm_o"]) for r in rA], axis=0)
    gates = np.concatenate([np.asarray(r["gates_o"]) for r in rA], axis=0)
    del rA
    mS = [dict(s5_host_params(inp, j), uT=_ac(uT[128 * j:128 * (j + 1)])) for j in range(NCR)]
    rS = _run(build_S5(), mS)
    ypreT = np.concatenate([np.asarray(r["yT_o"]) for r in rS], axis=0)
    del rS, mS, uT
    Aout = dict(fm=fm, vtm=vtm, gates=gates)
    mN = [nsa_host_inputs(inp, Aout, j) for j in range(NCR)]
    rN = _run(build_NSA(), mN)
    yattT = np.concatenate([np.asarray(r["yattT_o"]) for r in rN], axis=0)
    del rN, mN, fm, vtm, gates, Aout
    g1row = _ac(np.broadcast_to(g1[None, :], (128, D)), f)
    mC1 = []
    for c in range(NCR):
        sl = slice(c * TPC, (c + 1) * TPC)
        mC1.append(dict(x=_ac(x[sl]), ypreT=_ac(ypreT[:, sl]), yattT=_ac(yattT[:, sl]), g1row=g1row, sh2T=T16(sh2), sc2T=T16(sc2),
                        n2T=T16(inp["norm2_g"][0]), wglu=inp["ssm_w_glu"][0], bgluT=_ac(inp["ssm_b_glu"][0].reshape(8, 128).T),
                        bsT=_ac(inp["beta_ssm"][0].reshape(8, 128).T), baT=_ac(inp["beta_attn"][0].reshape(8, 128).T),
                        wout=inp["w_out"][0], ident=ident))
    rC1 = _run(build_C1(), mC1)
    del mC1, ypreT, yattT
    g2row = _ac(np.broadcast_to(g2[None, :], (128, D)), f)
    fgrow = _ac(np.broadcast_to(inp["final_g"][None, :], (128, D)), f)
    k1T = _ac(inp["peer_k1"][0].T)
    k2T = _ac(inp["peer_k2"][0].T)
    mC2 = [dict(x1=np.asarray(rC1[c]["x1_o"]), h2T=np.asarray(rC1[c]["h2T_o"]), g2row=g2row, fgrow=fgrow, wq=inp["peer_w_q"][0],
                k1T=k1T, k2T=k2T, pu=inp["peer_u"][0], pv=inp["peer_v"][0], ident=ident) for c in range(NCR)]
    rC2 = _run(build_C2(), mC2)
    out = np.concatenate([np.asarray(r["out"]) for r in rC2], axis=0)
    return out.reshape(1, L, D).astype(np.float32)
```
